# Optimizing a Trainium2 kernel written in Bass

```python
import math
import jax, jax.numpy as jnp
from jax import lax
import numpy as np

D_MODEL = 1024
BATCH = 8
SEQ = 4096
DEPTH = 4

CHUNK = 64
HEAD_DIM = 64
EPS = 1e-6
ROPE_THETA = 10000.0
A_HEADS = 4
A_LEFT_CHUNKS = 8
A_BAND = (A_LEFT_CHUNKS + 1) * CHUNK
REL_CLIP = 128
B_HEADS = 4
IDX_HEADS = 4
IDX_DIM = 64
TOPK_MAX = 256
Q_BLOCK = 128
C_WIDTH = 512
C_BLOCKS = 8
C_BLOCK_DIM = C_WIDTH // C_BLOCKS
C_CONV = 4
LRU_C = 8.0
N_BRANCH = 3
A_WIDTH = A_HEADS * HEAD_DIM
B_WIDTH = B_HEADS * HEAD_DIM
MIX_WIDTH = A_WIDTH + B_WIDTH + C_WIDTH
D_FF = -(-8 * D_MODEL // (3 * 256)) * 256
IN_SPLITS = (A_WIDTH, A_WIDTH, A_WIDTH,
             B_WIDTH, B_WIDTH, B_WIDTH,
             IDX_HEADS * IDX_DIM, IDX_DIM, IDX_HEADS,
             C_WIDTH, C_WIDTH,
             N_BRANCH * D_MODEL)
D_IN = sum(IN_SPLITS)

kernel_name = "hybrid_chunk_attn_dsa_rglru_gated"


def rms_norm(x, g):
    x32 = x.astype(jnp.float32)
    y = x32 * lax.rsqrt(jnp.mean(x32 * x32, axis=-1, keepdims=True) + EPS)
    return (y * g.astype(jnp.float32)).astype(x.dtype)


def rope_tables(seq):
    pos = jnp.arange(seq, dtype=jnp.float32)
    inv = 1.0 / (ROPE_THETA ** (jnp.arange(0, HEAD_DIM, 2, dtype=jnp.float32) / HEAD_DIM))
    ang = pos[:, None] * inv[None, :]
    ang = jnp.concatenate([ang, ang], axis=-1)
    return jnp.cos(ang), jnp.sin(ang)


def apply_rope(x, cos, sin):
    shape = (cos.shape[0],) + (1,) * (x.ndim - 3) + (cos.shape[1],)
    c, s = cos.reshape(shape), sin.reshape(shape)
    x32 = x.astype(jnp.float32)
    x1, x2 = jnp.split(x32, 2, axis=-1)
    rot = jnp.concatenate([-x2, x1], axis=-1)
    return (x32 * c + rot * s).astype(x.dtype)


def split_cols(z):
    offs, acc = [], 0
    for w in IN_SPLITS[:-1]:
        acc += w
        offs.append(acc)
    return jnp.split(z, offs, axis=-1)


def chunk_relpos_attention(q, k, v, rel_bias, rel_idx):
    b, s, h, d = q.shape
    n_c = s // CHUNK
    qc = q.reshape(b, n_c, CHUNK, h, d)
    pad = ((0, 0), (A_LEFT_CHUNKS, 0), (0, 0), (0, 0), (0, 0))
    kp = jnp.pad(k.reshape(b, n_c, CHUNK, h, d), pad)
    vp = jnp.pad(v.reshape(b, n_c, CHUNK, h, d), pad)
    kb = jnp.concatenate([kp[:, j:j + n_c] for j in range(A_LEFT_CHUNKS + 1)], axis=2)
    vb = jnp.concatenate([vp[:, j:j + n_c] for j in range(A_LEFT_CHUNKS + 1)], axis=2)
    sc = jnp.einsum('bcqhd,bckhd->bhcqk', qc, kb).astype(jnp.float32) * (d ** -0.5)
    bias = rel_bias.astype(jnp.float32)[:, rel_idx]
    sc = sc + bias[:, None]
    key_chunk = jnp.arange(n_c)[:, None] - A_LEFT_CHUNKS + jnp.arange(A_BAND)[None, :] // CHUNK
    valid = key_chunk >= 0
    sc = jnp.where(valid[None, None, :, None, :], sc, -jnp.inf)
    p = jax.nn.softmax(sc, axis=-1).astype(v.dtype)
    o = jnp.einsum('bhcqk,bckhd->bcqhd', p, vb)
    return o.reshape(b, s, h * d)


def indexer_sparse_attention(q, k, v, qi, ki, wi):
    b, s, h, d = q.shape
    topk = min(TOPK_MAX, s // 4)
    n_blk = s // Q_BLOCK
    key_chunk = jnp.arange(s) // CHUNK
    bidx = jnp.arange(b)[:, None, None]
    scale = d ** -0.5

    def to_blocks(t):
        return jnp.moveaxis(t.reshape((b, n_blk, Q_BLOCK) + t.shape[2:]), 1, 0)

    def block(args):
        blk, qb, qib, wib = args
        q_chunk = (blk * Q_BLOCK + jnp.arange(Q_BLOCK)) // CHUNK
        admissible = key_chunk[None, :] <= q_chunk[:, None]
        dots = jnp.einsum('bqhe,bse->bqhs', qib, ki).astype(jnp.float32)
        score = jnp.einsum('bqhs,bqh->bqs', jax.nn.relu(dots), wib.astype(jnp.float32))
        score = jnp.where(admissible[None], score, -jnp.inf)
        _, idx = lax.top_k(score, topk)
        sel_valid = (idx // CHUNK) <= q_chunk[None, :, None]
        ks = k[bidx, idx]
        vs = v[bidx, idx]
        att = jnp.einsum('bqhd,bqkhd->bhqk', qb, ks).astype(jnp.float32) * scale
        att = jnp.where(sel_valid[:, None], att, -jnp.inf)
        p = jax.nn.softmax(att, axis=-1).astype(v.dtype)
        return jnp.einsum('bhqk,bqkhd->bqhd', p, vs)

    out = lax.map(block, (jnp.arange(n_blk), to_blocks(q), to_blocks(qi), to_blocks(wi)))
    return jnp.moveaxis(out, 0, 1).reshape(b, s, h * d)


def rglru_branch(xc, yc, conv_w, conv_b, wa, ba, wx, bx, lam):
    b, s, c = xc.shape
    xp = jnp.pad(xc, ((0, 0), (C_CONV - 1, 0), (0, 0)))
    u = conv_b + sum(xp[:, j:j + s] * conv_w[j] for j in range(C_CONV))
    ub = u.reshape(b, s, C_BLOCKS, C_BLOCK_DIM)
    r = jax.nn.sigmoid(jnp.einsum('bsgi,gij->bsgj', ub, wa).reshape(b, s, c) + ba)
    i = jax.nn.sigmoid(jnp.einsum('bsgi,gij->bsgj', ub, wx).reshape(b, s, c) + bx)
    log_a = -LRU_C * r.astype(jnp.float32) * jax.nn.softplus(-lam.astype(jnp.float32))
    a = jnp.exp(log_a)
    gated_in = jnp.sqrt(-jnp.expm1(2.0 * log_a)) * (i * u).astype(jnp.float32)

    def combine(left, right):
        a1, b1 = left
        a2, b2 = right
        return a1 * a2, a2 * b1 + b2

    _, hs = lax.associative_scan(combine, (a, gated_in), axis=1)
    return hs.astype(xc.dtype) * jax.nn.gelu(yc)


def setup_inputs(seed: int = 0) -> dict:
    key = jax.random.key(seed)
    ks = jax.random.split(key, 20)
    f32 = jnp.float32
    nrm = lambda k, shp: jax.random.normal(k, shp, f32)
    x = nrm(ks[0], (BATCH, SEQ, D_MODEL))
    g_mix = 1.0 + 0.02 * nrm(ks[1], (DEPTH, D_MODEL))
    w_in = nrm(ks[2], (DEPTH, D_MODEL, D_IN)) * D_MODEL ** -0.5
    qk_gain_a = 1.0 + 0.02 * nrm(ks[3], (DEPTH, 2, HEAD_DIM))
    rel_bias = 0.1 * nrm(ks[4], (DEPTH, A_HEADS, 2 * REL_CLIP + 1))
    qk_gain_b = 1.0 + 0.02 * nrm(ks[5], (DEPTH, 2, HEAD_DIM))
    g_idx_k = 1.0 + 0.02 * nrm(ks[6], (DEPTH, IDX_DIM))
    conv_w = nrm(ks[7], (DEPTH, C_CONV, C_WIDTH)) * C_CONV ** -0.5
    conv_b = 0.01 * nrm(ks[8], (DEPTH, C_WIDTH))
    lru_wa = nrm(ks[9], (DEPTH, C_BLOCKS, C_BLOCK_DIM, C_BLOCK_DIM)) * C_BLOCK_DIM ** -0.5
    lru_ba = 0.01 * nrm(ks[10], (DEPTH, C_WIDTH))
    lru_wx = nrm(ks[11], (DEPTH, C_BLOCKS, C_BLOCK_DIM, C_BLOCK_DIM)) * C_BLOCK_DIM ** -0.5
    lru_bx = 0.01 * nrm(ks[12], (DEPTH, C_WIDTH))
    a_c = jax.random.uniform(ks[13], (DEPTH, C_WIDTH), f32, 0.9, 0.999)
    a0 = a_c ** (1.0 / LRU_C)
    lru_lambda = jnp.log(a0) - jnp.log1p(-a0)
    b_gate = 0.01 * nrm(ks[14], (DEPTH, N_BRANCH * D_MODEL))
    row_scale = jnp.concatenate([jnp.full((A_WIDTH,), A_WIDTH ** -0.5, f32),
                                 jnp.full((B_WIDTH,), B_WIDTH ** -0.5, f32),
                                 jnp.full((C_WIDTH,), C_WIDTH ** -0.5, f32)])
    w_branch = nrm(ks[15], (DEPTH, MIX_WIDTH, D_MODEL)) * row_scale[None, :, None]
    w_out = nrm(ks[16], (DEPTH, D_MODEL, D_MODEL)) * D_MODEL ** -0.5
    g_ffn = 1.0 + 0.02 * nrm(ks[17], (DEPTH, D_MODEL))
    w_ffn_in = nrm(ks[18], (DEPTH, D_MODEL, 2 * D_FF)) * D_MODEL ** -0.5
    w_ffn_out = nrm(ks[19], (DEPTH, D_FF, D_MODEL)) * D_FF ** -0.5
    return {"x": x, "g_mix": g_mix, "w_in": w_in, "qk_gain_a": qk_gain_a, "rel_bias": rel_bias,
            "qk_gain_b": qk_gain_b, "g_idx_k": g_idx_k, "conv_w": conv_w, "conv_b": conv_b,
            "lru_wa": lru_wa, "lru_ba": lru_ba, "lru_wx": lru_wx, "lru_bx": lru_bx,
            "lru_lambda": lru_lambda, "b_gate": b_gate, "w_branch": w_branch, "w_out": w_out,
            "g_ffn": g_ffn, "w_ffn_in": w_ffn_in, "w_ffn_out": w_ffn_out}


def reference(x, g_mix, w_in, qk_gain_a, rel_bias, qk_gain_b, g_idx_k, conv_w, conv_b,
              lru_wa, lru_ba, lru_wx, lru_bx, lru_lambda, b_gate, w_branch, w_out,
              g_ffn, w_ffn_in, w_ffn_out):
    b, s, _ = x.shape
    cos, sin = rope_tables(s)
    q_in_band = A_LEFT_CHUNKS * CHUNK + jnp.arange(CHUNK)
    rel_idx = jnp.clip(q_in_band[:, None] - jnp.arange(A_BAND)[None, :], -REL_CLIP, REL_CLIP) + REL_CLIP
    for l in range(DEPTH):
        h = rms_norm(x, g_mix[l])
        z = h @ w_in[l]
        aq, ak, av, bq, bk, bv, iq, ik, iw, cx, cy, gt = split_cols(z)
        hd = lambda t, n: t.reshape(b, s, n, HEAD_DIM)
        y_a = chunk_relpos_attention(rms_norm(hd(aq, A_HEADS), qk_gain_a[l, 0]),
                                     rms_norm(hd(ak, A_HEADS), qk_gain_a[l, 1]),
                                     hd(av, A_HEADS), rel_bias[l], rel_idx)
        q_b = apply_rope(rms_norm(hd(bq, B_HEADS), qk_gain_b[l, 0]), cos, sin)
        k_b = apply_rope(rms_norm(hd(bk, B_HEADS), qk_gain_b[l, 1]), cos, sin)
        q_i = apply_rope(iq.reshape(b, s, IDX_HEADS, IDX_DIM), cos, sin)
        k_i = apply_rope(rms_norm(ik, g_idx_k[l]), cos, sin)
        w_i = iw * (IDX_HEADS ** -0.5 * IDX_DIM ** -0.5)
        y_b = indexer_sparse_attention(q_b, k_b, hd(bv, B_HEADS), q_i, k_i, w_i)
        y_c = rglru_branch(cx, cy, conv_w[l], conv_b[l], lru_wa[l], lru_ba[l],
                           lru_wx[l], lru_bx[l], lru_lambda[l])
        gates = jax.nn.sigmoid(gt + b_gate[l]).reshape(b, s, N_BRANCH, D_MODEL)
        wb = w_branch[l]
        merged = (gates[:, :, 0] * (y_a @ wb[:A_WIDTH])
                  + gates[:, :, 1] * (y_b @ wb[A_WIDTH:A_WIDTH + B_WIDTH])
                  + gates[:, :, 2] * (y_c @ wb[A_WIDTH + B_WIDTH:]))
        x = x + merged @ w_out[l]
        gu = rms_norm(x, g_ffn[l]) @ w_ffn_in[l]
        g_, u_ = jnp.split(gu, 2, axis=-1)
        x = x + (jax.nn.silu(g_) * u_) @ w_ffn_out[l]
    return x
```

```python
import math
from contextlib import ExitStack
import numpy as np
import concourse.bass as bass
import concourse.mybir as mybir
from concourse.bass_utils import run_bass_kernel_spmd

F32 = mybir.dt.float32
BF16 = mybir.dt.bfloat16
AF = mybir.ActivationFunctionType
ALU = mybir.AluOpType
AX = mybir.AxisListType

D = 1024; S = 4096; L = 4; DIN = 5956; DFF = 2816
NT = S // 128; NB = S // 512
EPS = 1e-6
NBIS = 16
C_AQ, C_AK, C_AV, C_BQ, C_BK, C_BV, C_IQ, C_IK, C_IW, C_CX, C_CY, C_GT = (
    0, 256, 512, 768, 1024, 1280, 1536, 1792, 1856, 1860, 2372, 2884)
SP_GMIX, SP_GFFN, SP_BG, SP_GAQ, SP_GAK, SP_GBQ, SP_GBK, SP_GIK, SP_CW, SP_CB, SP_BA, SP_BX, SP_LAM = (
    0, 8, 16, 40, 41, 42, 43, 44, 45, 61, 65, 69, 73)
NSP = 77


class Tok:
    __slots__ = ("w", "r")

    def __init__(self):
        self.w = None
        self.r = {}


class Ring:
    def __init__(self, items):
        self.items = items
        self.i = -1

    def next(self):
        self.i = (self.i + 1) % len(self.items)
        return self.items[self.i]


class Slot:
    def __init__(self, t):
        self.t = t
        self.tok = Tok()


class K:
    INC = {"pe": 1, "act": 1, "dve": 1, "pool": 1, "dsp": 16, "dpool": 16, "dact": 16}

    def __init__(self, nc, es):
        self.nc = nc
        self.sem = {f"{n}@{l}": es.enter_context(nc.semaphore(f"s_{n}_{l}")) for n in self.INC for l in range(L)}
        self.cnt = {n: 0 for n in self.sem}
        self.ep = 0
        self.streams = {e: [] for e in ("pe", "act", "dve", "pool", "sp")}
        self.waited = {e: {} for e in self.streams}
        self.ninstr = 0

    def op(self, stream, fn, reads=(), writes=(), counter=None):
        counter = f"{counter or stream}@{self.ep}"
        deps = {}
        for t in reads:
            if t.w is not None and deps.get(t.w[0], 0) < t.w[1]:
                deps[t.w[0]] = t.w[1]
        for t in writes:
            if t.w is not None and deps.get(t.w[0], 0) < t.w[1]:
                deps[t.w[0]] = t.w[1]
            for c, s in t.r.items():
                if deps.get(c, 0) < s:
                    deps[c] = s
        wd = self.waited[stream]
        waits = []
        for c, s in deps.items():
            if c[:3] == "pe@" and counter[:3] == "pe@":
                continue
            if wd.get(c, 0) >= s:
                continue
            wd[c] = s
            waits.append((c, s))
        self.cnt[counter] += 1
        seq = self.cnt[counter]
        for t in reads:
            if t.r.get(counter, 0) < seq:
                t.r[counter] = seq
        for t in writes:
            t.w = (counter, seq)
            t.r = {}
        self.streams[stream].append((waits, fn, counter))
        self.ninstr += 1

    def wait_all_dma(self):
        waits = [(c, self.cnt[c]) for c in self.cnt if c[0] == "d" and c[:3] != "dve" and self.cnt[c] > 0]
        self.streams["sp"].append((waits, None, None))

    def flush(self):
        nc = self.nc
        streams = self.streams
        self.streams = {e: [] for e in streams}
        sem, INC = self.sem, self.INC

        def mk(lst):
            def f(eng):
                for waits, fn, counter in lst:
                    for c, s in waits:
                        eng.wait_ge(sem[c], s * INC[c.split("@")[0]])
                    if fn is not None:
                        fn(eng).then_inc(sem[counter], INC[counter.split("@")[0]])
            return f

        with nc.Block() as block:
            block.tensor(mk(streams["pe"]))
            block.scalar(mk(streams["act"]))
            block.vector(mk(streams["dve"]))
            block.gpsimd(mk(streams["pool"]))
            block.sync(mk(streams["sp"]))

    def dma(self, q, out, in_, reads, writes):
        self.op(q, lambda e: e.dma_start(out=out, in_=in_), reads, writes, counter="d" + q)

    def mm(self, out, lhsT, rhs, start, stop, reads, writes):
        self.op("pe", lambda e: e.matmul(out, lhsT, rhs, start=start, stop=stop), reads, writes)

    def tr(self, out, in_, ident, reads, writes):
        self.op("pe", lambda e: e.transpose(out, in_, ident), reads, writes)

    def act(self, out, in_, func, reads, writes, bias=None, scale=None):
        kw = {}
        if bias is not None:
            kw["bias"] = bias
        if scale is not None:
            kw["scale"] = scale
        self.op("act", lambda e: e.activation(out=out, in_=in_, func=func, **kw), reads, writes)

    def tt(self, eng, out, in0, in1, op, reads, writes):
        self.op(eng, lambda e: e.tensor_tensor(out=out, in0=in0, in1=in1, op=op), reads, writes)

    def ts(self, eng, out, in0, s1, s2, op0, op1, reads, writes, accum_out=None):
        if op1 is None:
            self.op(eng, lambda e: e.tensor_scalar(out=out, in0=in0, scalar1=s1, scalar2=None, op0=op0),
                    reads, writes)
        elif accum_out is None:
            self.op(eng, lambda e: e.tensor_scalar(out=out, in0=in0, scalar1=s1, scalar2=s2, op0=op0, op1=op1),
                    reads, writes)
        else:
            self.op(eng, lambda e: e.tensor_scalar(out=out, in0=in0, scalar1=s1, scalar2=s2, op0=op0, op1=op1,
                                                   accum_out=accum_out), reads, writes)

    def stt(self, eng, out, in0, scalar, in1, op0, op1, reads, writes):
        self.op(eng, lambda e: e.scalar_tensor_tensor(out=out, in0=in0, scalar=scalar, in1=in1, op0=op0, op1=op1),
                reads, writes)

    def recip(self, out, in_, reads, writes):
        self.op("dve", lambda e: e.reciprocal(out=out, in_=in_), reads, writes)

    def memset(self, eng, ap, val, writes):
        self.op(eng, lambda e: e.memset(ap, val), (), writes)

    def copy(self, eng, out, in_, reads, writes):
        self.op(eng, lambda e: e.tensor_copy(out=out, in_=in_), reads, writes)


OPT = {}


class _Stop(Exception):
    pass


def build(nlayers=L, dbg=False, stop=None):
    try:
        return _build(nlayers, dbg, stop)
    except _Stop as e:
        nc, k, es = e.args
        k.wait_all_dma()
        k.flush()
        return nc, k


def _build(nlayers, dbg, stop):
    nc = bass.Bass("TRN2", target_bir_lowering=False)
    es = ExitStack()

    def din(name, shape, dt=F32):
        return nc.dram_tensor(name, list(shape), dt, kind="ExternalInput").ap()

    kind_dbg = "ExternalOutput" if dbg else "Internal"

    def dscr(name, shape, dt):
        return nc.dram_tensor(name, list(shape), dt, kind=kind_dbg).ap()

    xT_in = din("xT", [D, S])
    w_in = din("w_in", [L, D, DIN])
    w_br = din("w_branch", [L, D, D])
    w_out = din("w_out", [L, D, D])
    w_f1 = din("w_ffn_in", [L, D, 2 * DFF])
    w_f2 = din("w_ffn_out", [L, DFF, D])
    spar = din("spar", [L, 128, NSP])
    lrubd = din("lrubd", [L, 2, 4, 128, 128])
    abias = din("abias", [L, 128, 4, 5, 128])
    cs_d = din("cs", [128, 2, S])
    cmat = din("cmat", [4, 128, 128])
    amask = din("amask", [128, 5, 128])
    pow2 = din("pow2", [128, NBIS + 1])
    yT = nc.dram_tensor("yT", [D, S], F32, kind="ExternalOutput").ap()

    XT = dscr("XT", [D, S], F32)
    QA = dscr("QA", [256, S], BF16)
    KA = dscr("KA", [256, S], BF16)
    QB = dscr("QB", [256, S], BF16)
    KB = dscr("KB", [256, S], BF16)
    QI = dscr("QI", [256, S], BF16)
    KI = dscr("KI", [64, S], BF16)
    VAB = dscr("VAB", [S, 512], BF16)
    WI = dscr("WI", [S, 4], F32)
    YM = dscr("YM", [D, S], BF16)
    dtok = {n: Tok() for n in ("XT", "QA", "KA", "QB", "KB", "QI", "KI", "VAB", "WI", "YM", "xin", "yT")}
    YMt = [Tok() for _ in range(3)]

    k = K(nc, es)

    PS2 = [es.enter_context(nc.psum_tensor(f"ps2_{i}", [128, 1024], F32)) for i in range(3)]
    PSTs = [es.enter_context(nc.psum_tensor(f"pst{i}", [128, 1024], BF16)) for i in range(2)]

    class Bank:
        def __init__(self, t, c0):
            self.t, self.c0, self.tok = t, c0, Tok()

        def ap(self, p0=0, p1=128, a=0, b=512):
            return self.t[p0:p1, self.c0 + a:self.c0 + b]

    banks = []
    for t in PS2:
        banks.append(Bank(t, 0))
        banks.append(Bank(t, 512))
    pstoks = [Tok(), Tok()]

    uid = [0]

    def sbuf(name, shape, dt, stack=es):
        uid[0] += 1
        return stack.enter_context(nc.sbuf_tensor(f"{name}_{uid[0]}", list(shape), dt))

    cm = sbuf("cm", [128, 4, 128], BF16)
    cmt = Tok()
    epsc = sbuf("epsc", [128, 1], F32)
    epst = Tok()
    spt = sbuf("spt", [128, NSP], F32)
    sptok = Tok()
    clam = sbuf("clam", [128, 4], F32)
    clamtok = Tok()
    k.dma("pool", cm[:], cmat.rearrange("m p n -> p m n"), [], [cmt])
    k.memset("dve", epsc[:], EPS, [epst])
    ONES, ONESBLK, ROTM, IDENT = (cm[:, i, :] for i in range(4))

    def xview(ap2d, tb):
        return ap2d.rearrange("(c p) t -> p c t", p=128)[:, :, tb * 512:(tb + 1) * 512]

    def wview(w2d, c0, n):
        return w2d.rearrange("(c p) n -> p c n", p=128)[:, :, c0:c0 + n]

    def make_hT(X, Xtok, gcol, sq, sqtok, sd, sdtok, rstd, rstok, hdst, htok, bank):
        k.act(sq[:], X[:], AF.Square, [Xtok], [sqtok])
        for c in range(8):
            k.mm(bank.ap(), ONES, sq[:, c, :], c == 0, c == 7, [sqtok, cmt], [bank.tok])
        k.act(sd[:], bank.ap(), AF.Sqrt, [bank.tok, epst], [sdtok], bias=epsc[:, 0:1], scale=1.0 / D)
        k.recip(rstd[:], sd[:], [sdtok], [rstok])
        for c in range(8):
            k.stt("dve", hdst(c), X[:, c, :], spt[:, gcol + c:gcol + c + 1], rstd[:],
                  ALU.mult, ALU.mult, [Xtok, sptok, rstok], [htok])

    for l in range(nlayers):
        xsrc, xsrct = (xT_in, dtok["xin"]) if l == 0 else (XT, dtok["XT"])
        last = l == nlayers - 1
        k.ep = l
        k.dma("sp", spt[:], spar[l], [], [sptok])

        with ExitStack() as ph:
            hT = sbuf("hT", [128, 8, S], BF16, ph); hTt = Tok()
            with ExitStack() as ph1:
                xs = Ring([Slot(sbuf(f"xs{i}", [128, 8, 512], F32, ph1)) for i in range(2)])
                sq = sbuf("sq", [128, 8, 512], BF16, ph1); sqt = Tok()
                sd = sbuf("sd", [128, 512], F32, ph1); sdt = Tok()
                rstd = sbuf("rstd", [128, 512], F32, ph1); rst = Tok()
                for tb in range(NB):
                    X = xs.next()
                    k.dma("sp", X.t[:], xview(xsrc, tb), [xsrct], [X.tok])
                    make_hT(X.t, X.tok, SP_GMIX, sq, sqt, sd, sdt, rstd, rst,
                            lambda c, tb=tb: hT[:, c, tb * 512:(tb + 1) * 512], hTt, banks[tb % 6])
                k.flush()
                if stop == 'A1':
                    raise _Stop(nc, k, es)

            wr = Ring([Slot(sbuf(f"wr{i}", [128, 8, 128], BF16, ph)) for i in range(3)])
            ob = Ring([Slot(sbuf(f"ob{i}", [128, S], BF16, ph)) for i in range(2)])
            csr = Ring([Slot(sbuf(f"csr{i}", [128, 2, 512], F32, ph)) for i in range(2)])
            sqb = Ring([Slot(sbuf(f"sqb{i}", [128, 512], BF16, ph)) for i in range(2)])
            sd2 = Ring([Slot(sbuf(f"sd2{i}", [128, 512], F32, ph)) for i in range(2)])
            rs2 = Ring([Slot(sbuf(f"rs2{i}", [128, 512], F32, ph)) for i in range(2)])
            qnb = Ring([Slot(sbuf(f"qnb{i}", [128, 512], BF16, ph)) for i in range(2)])
            t1r = Ring([Slot(sbuf(f"t1r{i}", [128, 512], F32, ph)) for i in range(2)])
            t2r = Ring([Slot(sbuf(f"t2r{i}", [128, 512], F32, ph)) for i in range(2)])
            bk = Ring(banks)

            def load_w(c0, m):
                W = wr.next()
                k.dma("pool", W.t[:, :, 0:m], wview(w_in[l], c0, m), [], [W.tok])
                return W

            def proj_mm(W, m, tb):
                b = bk.next()
                for c in range(8):
                    k.mm(b.ap(0, m), W.t[:, c, 0:m], hT[:, c, tb * 512:(tb + 1) * 512], c == 0, c == 7,
                         [W.tok, hTt], [b.tok])
                return b

            def qk_chunk(c0, m, dst, dstt, row0, gcol, norm, rope):
                W = load_w(c0, m)
                O = ob.next()
                for tb in range(NB):
                    b = proj_mm(W, m, tb)
                    osl = O.t[0:m, tb * 512:(tb + 1) * 512]
                    if norm:
                        SQ = sqb.next(); SD = sd2.next(); RS = rs2.next()
                        k.act(SQ.t[0:m, :], b.ap(0, m), AF.Square, [b.tok], [SQ.tok])
                        b2 = bk.next()
                        k.mm(b2.ap(0, m), cm[0:m, 1, 0:m], SQ.t[0:m, :], True, True, [SQ.tok, cmt], [b2.tok])
                        k.act(SD.t[0:m, :], b2.ap(0, m), AF.Sqrt, [b2.tok, epst], [SD.tok],
                              bias=epsc[0:m, 0:1], scale=1.0 / 64)
                        k.recip(RS.t[0:m, :], SD.t[0:m, :], [SD.tok], [RS.tok])
                        if not rope:
                            k.stt("dve", osl, b.ap(0, m), spt[0:m, gcol:gcol + 1], RS.t[0:m, :],
                                  ALU.mult, ALU.mult, [b.tok, sptok, RS.tok], [O.tok])
                            continue
                        QN = qnb.next()
                        k.stt("dve", QN.t[0:m, :], b.ap(0, m), spt[0:m, gcol:gcol + 1], RS.t[0:m, :],
                              ALU.mult, ALU.mult, [b.tok, sptok, RS.tok], [QN.tok])
                    else:
                        QN = qnb.next()
                        k.act(QN.t[0:m, :], b.ap(0, m), AF.Copy, [b.tok], [QN.tok])
                    CS = csr.next()
                    k.dma("sp", CS.t[:], cs_d[:, :, tb * 512:(tb + 1) * 512], [], [CS.tok])
                    b3 = bk.next()
                    k.mm(b3.ap(0, m), cm[0:m, 2, 0:m], QN.t[0:m, :], True, True, [QN.tok, cmt], [b3.tok])
                    T1 = t1r.next(); T2 = t2r.next()
                    k.tt("dve", T1.t[0:m, :], QN.t[0:m, :], CS.t[0:m, 0, :], ALU.mult, [QN.tok, CS.tok], [T1.tok])
                    k.tt("dve", T2.t[0:m, :], b3.ap(0, m), CS.t[0:m, 1, :], ALU.mult, [b3.tok, CS.tok], [T2.tok])
                    k.tt("pool", osl, T1.t[0:m, :], T2.t[0:m, :], ALU.add, [T1.tok, T2.tok], [O.tok])
                k.dma("sp", dst[row0:row0 + m, :], O.t[0:m, :], [O.tok], [dstt])

            for ch in range(2):
                qk_chunk(C_AQ + ch * 128, 128, QA, dtok["QA"], ch * 128, SP_GAQ, True, False)
                qk_chunk(C_AK + ch * 128, 128, KA, dtok["KA"], ch * 128, SP_GAK, True, False)
                qk_chunk(C_BQ + ch * 128, 128, QB, dtok["QB"], ch * 128, SP_GBQ, True, True)
                qk_chunk(C_BK + ch * 128, 128, KB, dtok["KB"], ch * 128, SP_GBK, True, True)
                qk_chunk(C_IQ + ch * 128, 128, QI, dtok["QI"], ch * 128, 0, False, True)
            qk_chunk(C_IK, 64, KI, dtok["KI"], 0, SP_GIK, True, True)

            wv = sbuf("wv", [128, 8, 512], BF16, ph); wvt = Tok()
            wiw = sbuf("wiw", [128, 8, 4], BF16, ph); wiwt = Tok()
            k.dma("pool", wv[:, :, 0:256], wview(w_in[l], C_AV, 256), [], [wvt])
            k.dma("pool", wv[:, :, 256:512], wview(w_in[l], C_BV, 256), [], [wvt])
            k.dma("pool", wiw[:], wview(w_in[l], C_IW, 4), [], [wiwt])
            vb = Ring([Slot(sbuf(f"vb{i}", [128, 512], BF16, ph)) for i in range(2)])
            wib = sbuf("wib", [128, NT, 4], F32, ph); wibt = Tok()
            for tt_ in range(NT):
                b = bk.next()
                for c in range(8):
                    k.mm(b.ap(), hT[:, c, tt_ * 128:(tt_ + 1) * 128], wv[:, c, :], c == 0, c == 7,
                         [hTt, wvt], [b.tok])
                V = vb.next()
                k.act(V.t[:], b.ap(), AF.Copy, [b.tok], [V.tok])
                k.dma("sp", VAB[tt_ * 128:(tt_ + 1) * 128, :], V.t[:], [V.tok], [dtok["VAB"]])
                b = bk.next()
                for c in range(8):
                    k.mm(b.ap(0, 128, 0, 4), hT[:, c, tt_ * 128:(tt_ + 1) * 128], wiw[:, c, :], c == 0, c == 7,
                         [hTt, wiwt], [b.tok])
                k.ts("dve", wib[:, tt_, :], b.ap(0, 128, 0, 4), 0.0625, None, ALU.mult, None, [b.tok], [wibt])
            k.dma("sp", WI.rearrange("(t p) c -> p t c", p=128), wib[:], [wibt], [dtok["WI"]])

            k.act(clam[:], spt[:, SP_LAM:SP_LAM + 4], AF.Exp, [sptok], [clamtok], scale=-1.0)
            k.act(clam[:], clam[:], AF.Ln, [clamtok], [clamtok], bias=1.0)
            k.ts("dve", clam[:], clam[:], -8.0, None, ALU.mult, None, [clamtok], [clamtok])
            cxb = sbuf("cxb", [128, 3 + S], F32, ph)
            cxt = [Tok() for _ in range(NB + 1)]
            k.memset("dve", cxb[:, 0:3], 0.0, [cxt[NB]])
            wbd = Ring([Slot(sbuf(f"wbd{i}", [128, 2, 128], BF16, ph)) for i in range(2)])
            f32r = {n: Ring([Slot(sbuf(f"c{n}{i}", [128, 512], F32, ph)) for i in range(2)])
                    for n in ("gy", "u", "r", "i", "a", "m", "bb", "hs")}
            ubr = Ring([Slot(sbuf(f"ub{i}", [128, 512], BF16, ph)) for i in range(2)])
            for cc in range(4):
                WX = load_w(C_CX + cc * 128, 128)
                WY = load_w(C_CY + cc * 128, 128)
                BD = wbd.next()
                k.dma("pool", BD.t[:], lrubd[l, :, cc].rearrange("m p n -> p m n"), [], [BD.tok])
                O = ob.next()
                prev_hs = None
                for tb in range(NB):
                    b = proj_mm(WX, 128, tb)
                    k.act(cxb[:, 3 + tb * 512:3 + (tb + 1) * 512], b.ap(), AF.Copy, [b.tok], [cxt[tb]])
                    b = proj_mm(WY, 128, tb)
                    GY = f32r["gy"].next()
                    k.act(GY.t[:], b.ap(), AF.Gelu, [b.tok], [GY.tok])
                    U = f32r["u"].next()
                    rd = [cxt[tb], cxt[tb - 1] if tb > 0 else cxt[NB], sptok]
                    cw = lambda j: spt[:, SP_CW + j * 4 + cc:SP_CW + j * 4 + cc + 1]
                    k.ts("dve", U.t[:], cxb[:, tb * 512:tb * 512 + 512], cw(0),
                         spt[:, SP_CB + cc:SP_CB + cc + 1], ALU.mult, ALU.add, rd, [U.tok])
                    for j in range(1, 4):
                        k.stt("dve", U.t[:], cxb[:, tb * 512 + j:tb * 512 + j + 512], cw(j), U.t[:],
                              ALU.mult, ALU.add, rd + [U.tok], [U.tok])
                    UB = ubr.next()
                    k.act(UB.t[:], U.t[:], AF.Copy, [U.tok], [UB.tok])
                    bR = bk.next()
                    k.mm(bR.ap(), BD.t[:, 0, :], UB.t[:], True, True, [BD.tok, UB.tok], [bR.tok])
                    bI = bk.next()
                    k.mm(bI.ap(), BD.t[:, 1, :], UB.t[:], True, True, [BD.tok, UB.tok], [bI.tok])
                    R = f32r["r"].next(); I_ = f32r["i"].next(); A = f32r["a"].next(); M = f32r["m"].next()
                    k.act(R.t[:], bR.ap(), AF.Sigmoid, [bR.tok, sptok], [R.tok], bias=spt[:, SP_BA + cc:SP_BA + cc + 1])
                    k.act(I_.t[:], bI.ap(), AF.Sigmoid, [bI.tok, sptok], [I_.tok], bias=spt[:, SP_BX + cc:SP_BX + cc + 1])
                    k.act(A.t[:], R.t[:], AF.Exp, [R.tok, clamtok], [A.tok], scale=clam[:, cc:cc + 1])
                    k.act(M.t[:], A.t[:], AF.Square, [A.tok], [M.tok])
                    k.act(M.t[:], M.t[:], AF.Sqrt, [M.tok], [M.tok], bias=1.0, scale=-1.0)
                    BB = f32r["bb"].next()
                    k.tt("pool", BB.t[:], I_.t[:], U.t[:], ALU.mult, [I_.tok, U.tok], [BB.tok])
                    k.tt("pool", BB.t[:], BB.t[:], M.t[:], ALU.mult, [BB.tok, M.tok], [BB.tok])
                    HS = f32r["hs"].next()
                    if prev_hs is None:
                        k.op("dve", lambda e, HS=HS, A=A, BB=BB: e.tensor_tensor_scan(
                            out=HS.t[:], data0=A.t[:], data1=BB.t[:], initial=0.0, op0=ALU.mult, op1=ALU.add),
                            [A.tok, BB.tok], [HS.tok])
                    else:
                        k.op("dve", lambda e, HS=HS, A=A, BB=BB, PH=prev_hs: e.tensor_tensor_scan(
                            out=HS.t[:], data0=A.t[:], data1=BB.t[:], initial=PH.t[:, 511:512],
                            op0=ALU.mult, op1=ALU.add), [A.tok, BB.tok, prev_hs.tok], [HS.tok])
                    prev_hs = HS
                    k.tt("pool", O.t[:, tb * 512:(tb + 1) * 512], HS.t[:], GY.t[:], ALU.mult,
                         [HS.tok, GY.tok], [O.tok])
                k.dma("sp", YM[512 + cc * 128:512 + (cc + 1) * 128, :], O.t[:], [O.tok], [YMt[2]])
            k.flush()
            if stop == 'A':
                raise _Stop(nc, k, es)

        def finalize_attn(t, acc, rdr, ytr, yor, row0, ymtok, pst_i):
            RD = rdr.next()
            a3 = acc.t[:, acc.c0:acc.c0 + 260].rearrange("p (h e) -> p h e", e=65)
            k.recip(RD.t[:], a3[:, :, 64:65], [acc.tok], [RD.tok])
            YT = ytr.next()
            for h in range(4):
                k.ts("dve", YT.t[:, h * 64:(h + 1) * 64], acc.ap(0, 128, h * 65, h * 65 + 64), RD.t[:, h, :], None,
                     ALU.mult, None, [acc.tok, RD.tok], [YT.tok])
            half = pst_i[0] % 2
            pst_i[0] += 1
            for c in range(2):
                k.tr(PSTs[half][:, c * 128:(c + 1) * 128], YT.t[:, c * 128:(c + 1) * 128], IDENT,
                     [YT.tok, cmt], [pstoks[half]])
            YO = yor.next()
            k.act(YO.t[:], PSTs[half][:, 0:256], AF.Copy, [pstoks[half]], [YO.tok])
            k.dma("sp", YM[row0:row0 + 256, t * 128:(t + 1) * 128].rearrange("(c p) q -> p c q", p=128),
                  YO.t[:].rearrange("p (c q) -> p c q", c=2), [YO.tok], [ymtok])

        with ExitStack() as ph:
            KAs = sbuf("KAs", [128, 2, S], BF16, ph); kat = Tok()
            QAs = sbuf("QAs", [128, 2, S], BF16, ph); qat = Tok()
            VA4 = sbuf("VA4", [128, NT, 4, 65], BF16, ph); vat = Tok()
            EB = sbuf("EB", [128, 4, 5, 128], F32, ph); ebt = Tok()
            AM = sbuf("AM", [128, 5, 128], F32, ph); amt = Tok()
            k.dma("sp", KAs[:], KA.rearrange("(c p) t -> p c t", p=128), [dtok["KA"]], [kat])
            k.dma("sp", QAs[:], QA.rearrange("(c p) t -> p c t", p=128), [dtok["QA"]], [qat])
            for h in range(4):
                k.dma("sp", VA4[:, :, h, 0:64], VAB.rearrange("(t p) c -> p t c", p=128)[:, :, h * 64:(h + 1) * 64],
                      [dtok["VAB"]], [vat])
            k.memset("pool", VA4[:, :, :, 64:65], 1.0, [vat])
            k.dma("sp", EB[:], abias[l], [], [ebt])
            k.dma("sp", AM[:], amask, [], [amt])
            k.act(EB[:], EB[:], AF.Exp, [ebt], [ebt])
            for h in range(4):
                k.tt("dve", EB[:, h], EB[:, h], AM[:], ALU.mult, [ebt, amt], [ebt])
            Er = Ring([Slot(sbuf(f"Ea{i}", [128, 640], F32, ph)) for i in range(2)])
            Emr = Ring([Slot(sbuf(f"Ema{i}", [128, 640], BF16, ph)) for i in range(3)])
            rdr = Ring([Slot(sbuf(f"rda{i}", [128, 4, 1], F32, ph)) for i in range(2)])
            ytr = Ring([Slot(sbuf(f"yta{i}", [128, 256], BF16, ph)) for i in range(2)])
            yor = Ring([Slot(sbuf(f"yoa{i}", [128, 256], BF16, ph)) for i in range(2)])
            stb = Ring([(PS2[0], banks[0], banks[1]), (PS2[1], banks[2], banks[3])])
            accb = Ring([banks[4], banks[5]])
            pst_i = [0]

            def a_stage(t, h):
                ch, pb = h // 2, (h % 2) * 64
                js = [j for j in range(5) if t - 4 + j >= 0]
                j0 = js[0]
                PT_, ba, bb_ = stb.next()
                for j in js:
                    k.mm(PT_[:, j * 128:(j + 1) * 128], KAs[pb:pb + 64, ch, (t - 4 + j) * 128:(t - 3 + j) * 128],
                         QAs[pb:pb + 64, ch, t * 128:(t + 1) * 128], True, True,
                         [kat, qat], [ba.tok if j < 4 else bb_.tok])
                E = Er.next(); Em = Emr.next()
                k.act(E.t[:, j0 * 128:640], PT_[:, j0 * 128:640], AF.Exp, [ba.tok, bb_.tok], [E.tok], scale=0.125)
                k.tt("dve", Em.t[:, j0 * 128:640], E.t[:, j0 * 128:640],
                     EB[:, h, j0:5, :].rearrange("p j q -> p (j q)"), ALU.mult, [E.tok, ebt], [Em.tok])
                return Em, js

            items = [(t, h) for t in range(NT) for h in range(4)]
            pend = a_stage(*items[0])
            acc = None
            for i, (t, h) in enumerate(items):
                Em, js = pend
                if i + 1 < len(items):
                    pend = a_stage(*items[i + 1])
                if h == 0:
                    acc = accb.next()
                for j in js:
                    k.mm(acc.ap(0, 128, h * 65, h * 65 + 65), Em.t[:, j * 128:(j + 1) * 128], VA4[:, t - 4 + j, h, :],
                         j == js[0], j == 4, [vat, Em.tok], [acc.tok])
                if h == 3:
                    finalize_attn(t, acc, rdr, ytr, yor, 0, YMt[0], pst_i)
            k.flush()
            if stop == 'B1':
                raise _Stop(nc, k, es)

        with ExitStack() as ph:
            KI2 = sbuf("KI2", [128, S], BF16, ph); kit = Tok()
            QIs = sbuf("QIs", [128, 2, S], BF16, ph); qit = Tok()
            KBs = sbuf("KBs", [128, 2, S], BF16, ph); kbt = Tok()
            QBs = sbuf("QBs", [128, 2, S], BF16, ph); qbt = Tok()
            VB4 = sbuf("VB4", [128, NT, 4, 65], BF16, ph); vbt = Tok()
            WIs = sbuf("WIs", [128, NT, 4], F32, ph); wit = Tok()
            P2 = sbuf("P2", [128, NBIS + 1], F32, ph); p2t = Tok()
            k.dma("sp", KI2[0:64, :], KI, [dtok["KI"]], [kit])
            k.dma("sp", KI2[64:128, :], KI, [dtok["KI"]], [kit])
            k.dma("sp", QIs[:], QI.rearrange("(c p) t -> p c t", p=128), [dtok["QI"]], [qit])
            k.dma("sp", KBs[:], KB.rearrange("(c p) t -> p c t", p=128), [dtok["KB"]], [kbt])
            k.dma("sp", QBs[:], QB.rearrange("(c p) t -> p c t", p=128), [dtok["QB"]], [qbt])
            for h in range(4):
                k.dma("sp", VB4[:, :, h, 0:64],
                      VAB.rearrange("(t p) c -> p t c", p=128)[:, :, 256 + h * 64:256 + (h + 1) * 64],
                      [dtok["VAB"]], [vbt])
            k.memset("pool", VB4[:, :, :, 64:65], 1.0, [vbt])
            k.dma("sp", WIs[:], WI.rearrange("(t p) c -> p t c", p=128), [dtok["WI"]], [wit])
            k.dma("sp", P2[:], pow2, [], [p2t])
            scr = Ring([Slot(sbuf(f"sc{i}", [128, S], F32, ph)) for i in range(2)])
            rlr = Ring([Slot(sbuf(f"rl{i}", [128, 512], F32, ph)) for i in range(3)])
            mkr = Ring([Slot(sbuf(f"mk{i}", [128, S], BF16, ph)) for i in range(2)])
            mTall = Ring([Slot(sbuf(f"mTa{i}", [128, S], BF16, ph)) for i in range(2)])
            Er = Ring([Slot(sbuf(f"Eb{i}", [128, 512], BF16, ph)) for i in range(3)])
            Emr = Ring([Slot(sbuf(f"Emb{i}", [128, 512], BF16, ph)) for i in range(3)])
            junk = sbuf("junk", [128, S], BF16, ph); jt = Tok()
            smr = Ring([Slot(sbuf(f"sm{i}", [128, 8 + NBIS + 1], F32, ph)) for i in range(2)])
            rdr = Ring([Slot(sbuf(f"rdb{i}", [128, 4, 1], F32, ph)) for i in range(2)])
            ytr = Ring([Slot(sbuf(f"ytb{i}", [128, 256], BF16, ph)) for i in range(2)])
            yor = Ring([Slot(sbuf(f"yob{i}", [128, 256], BF16, ph)) for i in range(2)])
            dbk = Ring([banks[0], banks[1]])
            sbk = Ring([banks[2], banks[3]])
            accb = Ring([banks[4], banks[5]])
            pst_i = [0]

            def prep(t):
                nk = 128 * (t + 1)
                SC = scr.next()
                nblk = (nk + 511) // 512
                for kb_ in range(nblk):
                    w = min(512, nk - kb_ * 512)
                    cs_ = slice(kb_ * 512, kb_ * 512 + w)
                    for h in range(4):
                        ch, pb = h // 2, (h % 2) * 64
                        b = dbk.next()
                        k.mm(b.ap(0, 128, 0, w), QIs[pb:pb + 64, ch, t * 128:(t + 1) * 128], KI2[pb:pb + 64, cs_],
                             True, True, [qit, kit], [b.tok])
                        wsc = WIs[:, t, h:h + 1]
                        if h == 0:
                            k.ts("dve", SC.t[:, cs_], b.ap(0, 128, 0, w), 0.0, wsc, ALU.max, ALU.mult,
                                 [b.tok, wit], [SC.tok])
                        else:
                            RL = rlr.next()
                            k.act(RL.t[:, 0:w], b.ap(0, 128, 0, w), AF.Relu, [b.tok], [RL.tok])
                            k.act(RL.t[:, 0:w], RL.t[:, 0:w], AF.Copy, [RL.tok, wit], [RL.tok], scale=wsc)
                            k.tt("pool", SC.t[:, cs_], SC.t[:, cs_], RL.t[:, 0:w], ALU.add,
                                 [RL.tok, SC.tok], [SC.tok])
                SM = smr.next()
                mx, mn, rng, mid, cnt, dd, thr = (SM.t[:, i:i + 1] for i in range(7))
                steps = SM.t[:, 8:8 + NBIS + 1]
                bis = t >= 2 and not OPT.get('b2_nobis')
                if bis:
                    k.op("dve", lambda e: e.tensor_reduce(out=mx, in_=SC.t[:, 0:nk], axis=AX.X, op=ALU.max),
                         [SC.tok], [SM.tok])
                k.op("dve", lambda e: e.tensor_reduce(out=mn, in_=SC.t[:, 0:nk], axis=AX.X, op=ALU.min),
                     [SC.tok], [SM.tok])
                k.memset("dve", SC.t[0:64, nk - 64:nk], -1.0e30, [SC.tok])
                if bis:
                    k.tt("dve", rng, mx, mn, ALU.subtract, [SM.tok], [SM.tok])
                    k.ts("dve", steps, P2[:], rng, None, ALU.mult, None, [p2t, SM.tok], [SM.tok])
                    k.tt("dve", mid, mn, steps[:, 0:1], ALU.add, [SM.tok], [SM.tok])
                    for it in range(NBIS):
                        k.ts("dve", junk[:, 0:nk], SC.t[:, 0:nk], mid, 0.0, ALU.is_ge, ALU.add,
                             [SC.tok, SM.tok], [jt, SM.tok], accum_out=cnt)
                        k.ts("dve", dd, cnt, 255.5, 0.5, ALU.is_ge, ALU.subtract, [SM.tok], [SM.tok])
                        k.stt("dve", mid, dd, steps[:, it:it + 1], mid, ALU.mult, ALU.add, [SM.tok], [SM.tok])
                    k.tt("dve", thr, mid, steps[:, NBIS:NBIS + 1], ALU.subtract, [SM.tok], [SM.tok])
                else:
                    k.copy("dve", thr, mn, [SM.tok], [SM.tok])
                MK = mkr.next()
                k.ts("dve", MK.t[:, 0:nk], SC.t[:, 0:nk], thr, None, ALU.is_ge, None, [SC.tok, SM.tok], [MK.tok])
                ngrp = (t + 1 + 3) // 4
                MT = mTall.next()
                for g in range(ngrp):
                    jts = list(range(g * 4, min(t + 1, g * 4 + 4)))
                    half = pst_i[0] % 2
                    pst_i[0] += 1
                    for jj, jt_ in enumerate(jts):
                        k.tr(PSTs[half][:, jj * 128:(jj + 1) * 128],
                             MK.t[:, jt_ * 128:(jt_ + 1) * 128], IDENT, [MK.tok, cmt], [pstoks[half]])
                    n = len(jts) * 128
                    k.act(MT.t[:, g * 512:g * 512 + n], PSTs[half][:, 0:n], AF.Copy,
                          [pstoks[half]], [MT.tok])
                return MT

            def b_stage(t, h, g, MT):
                ch, pb = h // 2, (h % 2) * 64
                jts = list(range(g * 4, min(t + 1, g * 4 + 4)))
                n = len(jts) * 128
                b = sbk.next()
                for jj, jt_ in enumerate(jts):
                    k.mm(b.ap(0, 128, jj * 128, (jj + 1) * 128), KBs[pb:pb + 64, ch, jt_ * 128:(jt_ + 1) * 128],
                         QBs[pb:pb + 64, ch, t * 128:(t + 1) * 128], True, True, [kbt, qbt], [b.tok])
                E = Er.next(); Em = Emr.next()
                k.act(E.t[:, 0:n], b.ap(0, 128, 0, n), AF.Exp, [b.tok], [E.tok], scale=0.125)
                k.tt("pool", Em.t[:, 0:n], E.t[:, 0:n], MT.t[:, g * 512:g * 512 + n], ALU.mult,
                     [E.tok, MT.tok], [Em.tok])
                return Em, jts

            ntile = OPT.get('b2_tmax', NT)
            MTs = {0: prep(0)}
            for t in range(ntile):
                if t + 1 < ntile:
                    MTs[t + 1] = prep(t + 1)
                if OPT.get('b2_noattn'):
                    continue
                MT = MTs.pop(t)
                ngrp = (t + 1 + 3) // 4
                items = [(h, g) for h in range(4) for g in range(ngrp)]
                pend = b_stage(t, *items[0], MT)
                acc = accb.next()
                for i, (h, g) in enumerate(items):
                    Em, jts = pend
                    if i + 1 < len(items):
                        pend = b_stage(t, *items[i + 1], MT)
                    for jj, jt_ in enumerate(jts):
                        k.mm(acc.ap(0, 128, h * 65, h * 65 + 65), Em.t[:, jj * 128:(jj + 1) * 128], VB4[:, jt_, h, :],
                             jt_ == 0, jt_ == t, [vbt, Em.tok], [acc.tok])
                finalize_attn(t, acc, rdr, ytr, yor, 256, YMt[1], pst_i)
            k.flush()
            if stop == 'B2':
                raise _Stop(nc, k, es)

        with ExitStack() as ph:
            wg = sbuf("wg", [128, 8, 3072], BF16, ph); wgt = Tok()
            wb = sbuf("wb", [128, 8, 1024], BF16, ph); wbt = Tok()
            wo = sbuf("wo", [128, 8, 1024], BF16, ph); wot = Tok()
            for c in range(8):
                k.dma("pool", wg[:, c, :], w_in[l, c * 128:(c + 1) * 128, C_GT:C_GT + 3072], [], [wgt])
            k.dma("pool", wb[:], wview(w_br[l], 0, 1024), [], [wbt])
            k.dma("pool", wo[:], wview(w_out[l], 0, 1024), [], [wot])
            X = Slot(sbuf("xc", [128, 8, 512], F32, ph))
            sq = sbuf("sqc", [128, 8, 512], BF16, ph); sqt = Tok()
            sd = sbuf("sdc", [128, 512], F32, ph); sdt = Tok()
            rstd = sbuf("rstdc", [128, 512], F32, ph); rst = Tok()
            hTb = sbuf("hTb", [128, 8, 512], BF16, ph); hbt = Tok()
            ym = sbuf("ymc", [128, 8, 512], BF16, ph); ymt = Tok()
            mg = sbuf("mg", [128, 8, 512], BF16, ph); mgt = [Tok() for _ in range(8)]
            sgr = Ring([Slot(sbuf(f"sg{i}", [128, 512], F32, ph)) for i in range(3)])
            tmr = Ring([Slot(sbuf(f"tm{i}", [128, 512], F32, ph)) for i in range(3)])
            acr = Ring([Slot(sbuf(f"ac{i}", [128, 512], F32, ph)) for i in range(2)])
            xo = Ring([Slot(sbuf(f"xo{i}", [128, 512], F32, ph)) for i in range(3)])
            bk = Ring(banks)
            KR = [(0, 2), (2, 4), (4, 8)]
            for tb in range(NB):
                k.dma("sp", X.t[:], xview(xsrc, tb), [xsrct], [X.tok])
                k.dma("sp", ym[:], xview(YM, tb), YMt, [ymt])
                make_hT(X.t, X.tok, SP_GMIX, sq, sqt, sd, sdt, rstd, rst, lambda c: hTb[:, c, :], hbt, bk.next())
                for dc in range(8):
                    AC = acr.next()
                    for br in range(3):
                        bg = bk.next()
                        for c in range(8):
                            k.mm(bg.ap(), wg[:, c, br * 1024 + dc * 128:br * 1024 + (dc + 1) * 128], hTb[:, c, :],
                                 c == 0, c == 7, [wgt, hbt], [bg.tok])
                        SG = sgr.next()
                        bcol = SP_BG + br * 8 + dc
                        k.act(SG.t[:], bg.ap(), AF.Sigmoid, [bg.tok, sptok], [SG.tok], bias=spt[:, bcol:bcol + 1])
                        bb_ = bk.next()
                        k0, k1 = KR[br]
                        for c in range(k0, k1):
                            k.mm(bb_.ap(), wb[:, c, dc * 128:(dc + 1) * 128], ym[:, c, :], c == k0, c == k1 - 1,
                                 [wbt, ymt], [bb_.tok])
                        if br == 0:
                            k.tt("dve", AC.t[:], SG.t[:], bb_.ap(), ALU.mult, [SG.tok, bb_.tok], [AC.tok])
                        else:
                            TM = tmr.next()
                            k.tt("dve", TM.t[:], SG.t[:], bb_.ap(), ALU.mult, [SG.tok, bb_.tok], [TM.tok])
                            if br == 1:
                                k.tt("pool", AC.t[:], AC.t[:], TM.t[:], ALU.add, [AC.tok, TM.tok], [AC.tok])
                            else:
                                k.tt("pool", mg[:, dc, :], AC.t[:], TM.t[:], ALU.add, [AC.tok, TM.tok], [mgt[dc]])
                for oc in range(8):
                    bo = bk.next()
                    for c in range(8):
                        k.mm(bo.ap(), wo[:, c, oc * 128:(oc + 1) * 128], mg[:, c, :], c == 0, c == 7,
                             [wot, mgt[c]], [bo.tok])
                    XO = xo.next()
                    k.tt("dve", XO.t[:], X.t[:, oc, :], bo.ap(), ALU.add, [X.tok, bo.tok], [XO.tok])
                    k.dma("sp", XT[oc * 128:(oc + 1) * 128, tb * 512:(tb + 1) * 512], XO.t[:], [XO.tok], [dtok["XT"]])
            k.flush()
            if stop == 'C':
                raise _Stop(nc, k, es)

        with ExitStack() as ph:
            w1 = sbuf("w1", [128, 8, 2 * DFF], BF16, ph); w1t = Tok()
            w2 = sbuf("w2", [128, 22, 1024], BF16, ph); w2t = Tok()
            for c in range(8):
                k.dma("pool", w1[:, c, :], w_f1[l, c * 128:(c + 1) * 128, :], [], [w1t])
            for c in range(22):
                k.dma("pool", w2[:, c, :], w_f2[l, c * 128:(c + 1) * 128, :], [], [w2t])
            X = Slot(sbuf("xd", [128, 8, 512], F32, ph))
            sq = sbuf("sqd", [128, 8, 512], BF16, ph); sqt = Tok()
            sd = sbuf("sdd", [128, 512], F32, ph); sdt = Tok()
            rstd = sbuf("rstdd", [128, 512], F32, ph); rst = Tok()
            hTb = sbuf("hTd", [128, 8, 512], BF16, ph); hbt = Tok()
            av = sbuf("av", [128, 22, 512], BF16, ph); avt = [Tok() for _ in range(22)]
            sgr = Ring([Slot(sbuf(f"sl{i}", [128, 512], F32, ph)) for i in range(3)])
            xo = Ring([Slot(sbuf(f"xod{i}", [128, 512], F32, ph)) for i in range(3)])
            bk = Ring(banks)
            xdst, xdstt = (yT, dtok["yT"]) if last else (XT, dtok["XT"])
            for tb in range(NB):
                k.dma("sp", X.t[:], xview(XT, tb), [dtok["XT"]], [X.tok])
                make_hT(X.t, X.tok, SP_GFFN, sq, sqt, sd, sdt, rstd, rst, lambda c: hTb[:, c, :], hbt, bk.next())
                for fc in range(22):
                    bg = bk.next()
                    for c in range(8):
                        k.mm(bg.ap(), w1[:, c, fc * 128:(fc + 1) * 128], hTb[:, c, :], c == 0, c == 7,
                             [w1t, hbt], [bg.tok])
                    bu = bk.next()
                    for c in range(8):
                        k.mm(bu.ap(), w1[:, c, DFF + fc * 128:DFF + (fc + 1) * 128], hTb[:, c, :], c == 0, c == 7,
                             [w1t, hbt], [bu.tok])
                    SG = sgr.next()
                    k.act(SG.t[:], bg.ap(), AF.Silu, [bg.tok], [SG.tok])
                    k.tt("dve", av[:, fc, :], SG.t[:], bu.ap(), ALU.mult, [SG.tok, bu.tok], [avt[fc]])
                for oc in range(8):
                    bo = bk.next()
                    for c in range(22):
                        k.mm(bo.ap(), w2[:, c, oc * 128:(oc + 1) * 128], av[:, c, :], c == 0, c == 21,
                             [w2t, avt[c]], [bo.tok])
                    XO = xo.next()
                    k.tt("dve", XO.t[:], X.t[:, oc, :], bo.ap(), ALU.add, [X.tok, bo.tok], [XO.tok])
                    k.dma("sp", xdst[oc * 128:(oc + 1) * 128, tb * 512:(tb + 1) * 512], XO.t[:], [XO.tok], [xdstt])
            if last:
                k.wait_all_dma()
            k.flush()
            if stop == 'D':
                raise _Stop(nc, k, es)
    es.close()
    return nc, k


def host_consts():
    pos = np.arange(S, dtype=np.float32)
    inv = (1.0 / (np.float32(10000.0) ** (np.arange(0, 64, 2, dtype=np.float32) / np.float32(64)))).astype(np.float32)
    ang = pos[:, None] * inv[None, :]
    ang = np.concatenate([ang, ang], axis=-1)
    cosT = np.cos(ang).astype(np.float32).T
    sinT = np.sin(ang).astype(np.float32).T
    cs = np.zeros((128, 2, S), np.float32)
    cs[0:64, 0], cs[64:128, 0] = cosT, cosT
    cs[0:64, 1], cs[64:128, 1] = sinT, sinT
    ones = np.ones((128, 128), np.float32)
    onesblk = np.zeros((128, 128), np.float32)
    onesblk[0:64, 0:64] = 1.0
    onesblk[64:128, 64:128] = 1.0
    rotm = np.zeros((128, 128), np.float32)
    for hb in (0, 64):
        for m in range(32):
            rotm[hb + m + 32, hb + m] = -1.0
            rotm[hb + m, hb + m + 32] = 1.0
    ident = np.eye(128, dtype=np.float32)
    cmat = np.stack([ones, onesblk, rotm, ident]).astype(np.float32)
    kk = np.arange(128)[:, None, None]
    jj = np.arange(5)[None, :, None]
    qq = np.arange(128)[None, None, :]
    cq = (qq >= 64).astype(np.int64)
    ck = 2 * jj - 8 + (kk >= 64)
    amask = ((ck >= cq - 8) & (ck <= cq)).astype(np.float32)
    relidx = np.clip(128 * (4 - jj) + qq - kk, -128, 128) + 128
    pow2 = np.tile((2.0 ** -(np.arange(NBIS + 1) + 1.0)).astype(np.float32)[None, :], (128, 1))
    return cs, cmat, amask, relidx, pow2


def host_pack(inp):
    cs, cmat, amask, relidx, pow2 = host_consts()
    f = lambda a: np.ascontiguousarray(np.asarray(a, dtype=np.float32))
    spar = np.zeros((L, 128, NSP), np.float32)
    p = np.arange(128)
    for l in range(L):
        spar[l, :, SP_GMIX:SP_GMIX + 8] = f(inp["g_mix"])[l].reshape(8, 128).T
        spar[l, :, SP_GFFN:SP_GFFN + 8] = f(inp["g_ffn"])[l].reshape(8, 128).T
        spar[l, :, SP_BG:SP_BG + 24] = f(inp["b_gate"])[l].reshape(24, 128).T
        spar[l, :, SP_GAQ] = f(inp["qk_gain_a"])[l, 0][p % 64]
        spar[l, :, SP_GAK] = f(inp["qk_gain_a"])[l, 1][p % 64]
        spar[l, :, SP_GBQ] = f(inp["qk_gain_b"])[l, 0][p % 64]
        spar[l, :, SP_GBK] = f(inp["qk_gain_b"])[l, 1][p % 64]
        spar[l, :, SP_GIK] = f(inp["g_idx_k"])[l][p % 64]
        cw = f(inp["conv_w"])[l]
        for j in range(4):
            spar[l, :, SP_CW + j * 4:SP_CW + j * 4 + 4] = cw[j].reshape(4, 128).T
        spar[l, :, SP_CB:SP_CB + 4] = f(inp["conv_b"])[l].reshape(4, 128).T
        spar[l, :, SP_BA:SP_BA + 4] = f(inp["lru_ba"])[l].reshape(4, 128).T
        spar[l, :, SP_BX:SP_BX + 4] = f(inp["lru_bx"])[l].reshape(4, 128).T
        spar[l, :, SP_LAM:SP_LAM + 4] = f(inp["lru_lambda"])[l].reshape(4, 128).T
    lrubd = np.zeros((L, 2, 4, 128, 128), np.float32)
    for m, nm in enumerate(("lru_wa", "lru_wx")):
        wsrc = f(inp[nm])
        for cc in range(4):
            lrubd[:, m, cc, 0:64, 0:64] = wsrc[:, 2 * cc]
            lrubd[:, m, cc, 64:128, 64:128] = wsrc[:, 2 * cc + 1]
    rb = f(inp["rel_bias"])
    ab = rb[:, :, relidx]
    abias = np.ascontiguousarray(ab.transpose(0, 2, 1, 3, 4))
    shared = {"w_in": f(inp["w_in"]), "w_branch": f(inp["w_branch"]), "w_out": f(inp["w_out"]),
              "w_ffn_in": f(inp["w_ffn_in"]), "w_ffn_out": f(inp["w_ffn_out"]),
              "spar": spar, "lrubd": lrubd, "abias": abias, "cs": cs, "cmat": cmat,
              "amask": amask, "pow2": pow2}
    return shared


_CACHE = {}


def kernel(**inputs):
    x = np.asarray(inputs["x"], dtype=np.float32)
    shared = host_pack(inputs)
    if "nc" not in _CACHE:
        _CACHE["nc"] = build()[0]
    nc = _CACHE["nc"]
    in_maps = []
    for b in range(8):
        m = dict(shared)
        m["xT"] = np.ascontiguousarray(x[b].T)
        in_maps.append(m)
    res = run_bass_kernel_spmd(nc, in_maps, core_ids=list(range(8)))
    out = np.stack([np.ascontiguousarray(r["yT"].T) for r in res.results], axis=0)
    return out.astype(np.float32)
```

```python
import math
from contextlib import ExitStack
import numpy as np
import concourse.bass as bass
import concourse.mybir as mybir
from concourse.bass_utils import run_bass_kernel_spmd

F32 = mybir.dt.float32
BF16 = mybir.dt.bfloat16
AF = mybir.ActivationFunctionType
ALU = mybir.AluOpType
AX = mybir.AxisListType

D = 1024; S = 4096; L = 4; DIN = 5956; DFF = 2816
NT = S // 128; NB = S // 512
EPS = 1e-6
NBIS = 14
C_AQ, C_AK, C_AV, C_BQ, C_BK, C_BV, C_IQ, C_IK, C_IW, C_CX, C_CY, C_GT = (
    0, 256, 512, 768, 1024, 1280, 1536, 1792, 1856, 1860, 2372, 2884)
SP_GMIX, SP_GFFN, SP_BG, SP_GAQ, SP_GAK, SP_GBQ, SP_GBK, SP_GIK, SP_CW, SP_CB, SP_BA, SP_BX, SP_LAM = (
    0, 8, 16, 40, 41, 42, 43, 44, 45, 61, 65, 69, 73)
NSP = 77


class Tok:
    __slots__ = ("w", "r")

    def __init__(self):
        self.w = None
        self.r = {}


class Ring:
    def __init__(self, items):
        self.items = items
        self.i = -1

    def next(self):
        self.i = (self.i + 1) % len(self.items)
        return self.items[self.i]


class Slot:
    def __init__(self, t):
        self.t = t
        self.tok = Tok()


class K:
    INC = {"pe": 1, "act": 1, "dve": 1, "pool": 1, "dsp": 16, "dpool": 16, "dact": 16}

    def __init__(self, nc, es):
        self.nc = nc
        self.sem = {f"{n}@{l}": es.enter_context(nc.semaphore(f"s_{n}_{l}")) for n in self.INC for l in range(L)}
        self.cnt = {n: 0 for n in self.sem}
        self.ep = 0
        self.streams = {e: [] for e in ("pe", "act", "dve", "pool", "sp")}
        self.waited = {e: {} for e in self.streams}
        self.ninstr = 0

    def op(self, stream, fn, reads=(), writes=(), counter=None):
        counter = f"{counter or stream}@{self.ep}"
        deps = {}
        for t in reads:
            if t.w is not None and deps.get(t.w[0], 0) < t.w[1]:
                deps[t.w[0]] = t.w[1]
        for t in writes:
            if t.w is not None and deps.get(t.w[0], 0) < t.w[1]:
                deps[t.w[0]] = t.w[1]
            for c, s in t.r.items():
                if deps.get(c, 0) < s:
                    deps[c] = s
        wd = self.waited[stream]
        waits = []
        for c, s in deps.items():
            if c[:3] == "pe@" and counter[:3] == "pe@":
                continue
            if wd.get(c, 0) >= s:
                continue
            wd[c] = s
            waits.append((c, s))
        self.cnt[counter] += 1
        seq = self.cnt[counter]
        for t in reads:
            if t.r.get(counter, 0) < seq:
                t.r[counter] = seq
        for t in writes:
            t.w = (counter, seq)
            t.r = {}
        self.streams[stream].append((waits, fn, counter))
        self.ninstr += 1

    def wait_all_dma(self):
        waits = [(c, self.cnt[c]) for c in self.cnt if c[0] == "d" and c[:3] != "dve" and self.cnt[c] > 0]
        self.streams["sp"].append((waits, None, None))

    def flush(self):
        nc = self.nc
        streams = self.streams
        self.streams = {e: [] for e in streams}
        sem, INC = self.sem, self.INC

        def mk(lst):
            def f(eng):
                for waits, fn, counter in lst:
                    for c, s in waits:
                        eng.wait_ge(sem[c], s * INC[c.split("@")[0]])
                    if fn is not None:
                        fn(eng).then_inc(sem[counter], INC[counter.split("@")[0]])
            return f

        with nc.Block() as block:
            block.tensor(mk(streams["pe"]))
            block.scalar(mk(streams["act"]))
            block.vector(mk(streams["dve"]))
            block.gpsimd(mk(streams["pool"]))
            block.sync(mk(streams["sp"]))

    def dma(self, q, out, in_, reads, writes):
        self.op(q, lambda e: e.dma_start(out=out, in_=in_), reads, writes, counter="d" + q)

    def mm(self, out, lhsT, rhs, start, stop, reads, writes):
        self.op("pe", lambda e: e.matmul(out, lhsT, rhs, start=start, stop=stop), reads, writes)

    def tr(self, out, in_, ident, reads, writes):
        self.op("pe", lambda e: e.transpose(out, in_, ident), reads, writes)

    def act(self, out, in_, func, reads, writes, bias=None, scale=None):
        kw = {}
        if bias is not None:
            kw["bias"] = bias
        if scale is not None:
            kw["scale"] = scale
        self.op("act", lambda e: e.activation(out=out, in_=in_, func=func, **kw), reads, writes)

    def tt(self, eng, out, in0, in1, op, reads, writes):
        self.op(eng, lambda e: e.tensor_tensor(out=out, in0=in0, in1=in1, op=op), reads, writes)

    def ts(self, eng, out, in0, s1, s2, op0, op1, reads, writes, accum_out=None):
        if op1 is None:
            self.op(eng, lambda e: e.tensor_scalar(out=out, in0=in0, scalar1=s1, scalar2=None, op0=op0),
                    reads, writes)
        elif accum_out is None:
            self.op(eng, lambda e: e.tensor_scalar(out=out, in0=in0, scalar1=s1, scalar2=s2, op0=op0, op1=op1),
                    reads, writes)
        else:
            self.op(eng, lambda e: e.tensor_scalar(out=out, in0=in0, scalar1=s1, scalar2=s2, op0=op0, op1=op1,
                                                   accum_out=accum_out), reads, writes)

    def stt(self, eng, out, in0, scalar, in1, op0, op1, reads, writes):
        self.op(eng, lambda e: e.scalar_tensor_tensor(out=out, in0=in0, scalar=scalar, in1=in1, op0=op0, op1=op1),
                reads, writes)

    def recip(self, out, in_, reads, writes):
        self.op("dve", lambda e: e.reciprocal(out=out, in_=in_), reads, writes)

    def memset(self, eng, ap, val, writes):
        self.op(eng, lambda e: e.memset(ap, val), (), writes)

    def copy(self, eng, out, in_, reads, writes):
        self.op(eng, lambda e: e.tensor_copy(out=out, in_=in_), reads, writes)


OPT = {}


class _Stop(Exception):
    pass


def build(nlayers=L, dbg=False, stop=None):
    try:
        return _build(nlayers, dbg, stop)
    except _Stop as e:
        nc, k, es = e.args
        k.wait_all_dma()
        k.flush()
        return nc, k


def _build(nlayers, dbg, stop):
    nc = bass.Bass("TRN2", target_bir_lowering=False)
    es = ExitStack()

    def din(name, shape, dt=F32):
        return nc.dram_tensor(name, list(shape), dt, kind="ExternalInput").ap()

    kind_dbg = "ExternalOutput" if dbg else "Internal"

    def dscr(name, shape, dt):
        return nc.dram_tensor(name, list(shape), dt, kind=kind_dbg).ap()

    xT_in = din("xT", [D, S])
    w_in = din("w_in", [L, D, DIN])
    w_br = din("w_branch", [L, D, D])
    w_out = din("w_out", [L, D, D])
    w_f1 = din("w_ffn_in", [L, D, 2 * DFF])
    w_f2 = din("w_ffn_out", [L, DFF, D])
    spar = din("spar", [L, 128, NSP])
    lrubd = din("lrubd", [L, 2, 4, 128, 128])
    abias = din("abias", [L, 128, 4, 5, 128])
    cs_d = din("cs", [128, 2, S])
    cmat = din("cmat", [4, 128, 128])
    amask = din("amask", [128, 5, 128])
    pow2 = din("pow2", [128, NBIS + 1])
    yT = nc.dram_tensor("yT", [D, S], F32, kind="ExternalOutput").ap()

    XT = dscr("XT", [D, S], F32)
    QA = dscr("QA", [256, S], BF16)
    KA = dscr("KA", [256, S], BF16)
    QB = dscr("QB", [256, S], BF16)
    KB = dscr("KB", [256, S], BF16)
    QI = dscr("QI", [256, S], BF16)
    KI = dscr("KI", [64, S], BF16)
    VAB = dscr("VAB", [S, 512], BF16)
    WI = dscr("WI", [S, 4], F32)
    YM = dscr("YM", [D, S], BF16)
    dtok = {n: Tok() for n in ("XT", "QA", "KA", "QB", "KB", "QI", "KI", "VAB", "WI", "YM", "xin", "yT")}
    YMt = [Tok() for _ in range(3)]

    k = K(nc, es)

    PS2 = [es.enter_context(nc.psum_tensor(f"ps2_{i}", [128, 1024], F32)) for i in range(3)]
    PSTs = [es.enter_context(nc.psum_tensor(f"pst{i}", [128, 1024], BF16)) for i in range(2)]

    class Bank:
        def __init__(self, t, c0):
            self.t, self.c0, self.tok = t, c0, Tok()

        def ap(self, p0=0, p1=128, a=0, b=512):
            return self.t[p0:p1, self.c0 + a:self.c0 + b]

    banks = []
    for t in PS2:
        banks.append(Bank(t, 0))
        banks.append(Bank(t, 512))
    pstoks = [Tok(), Tok()]

    uid = [0]

    def sbuf(name, shape, dt, stack=es):
        uid[0] += 1
        return stack.enter_context(nc.sbuf_tensor(f"{name}_{uid[0]}", list(shape), dt))

    cm = sbuf("cm", [128, 4, 128], BF16)
    cmt = Tok()
    epsc = sbuf("epsc", [128, 1], F32)
    epst = Tok()
    spt = sbuf("spt", [128, NSP], F32)
    sptok = Tok()
    clam = sbuf("clam", [128, 4], F32)
    clamtok = Tok()
    k.dma("pool", cm[:], cmat.rearrange("m p n -> p m n"), [], [cmt])
    k.memset("dve", epsc[:], EPS, [epst])
    ONES, ONESBLK, ROTM, IDENT = (cm[:, i, :] for i in range(4))

    def xview(ap2d, tb):
        return ap2d.rearrange("(c p) t -> p c t", p=128)[:, :, tb * 512:(tb + 1) * 512]

    def wview(w2d, c0, n):
        return w2d.rearrange("(c p) n -> p c n", p=128)[:, :, c0:c0 + n]

    def make_hT(X, Xtok, gcol, sq, sqtok, sd, sdtok, rstd, rstok, hdst, htok, bank):
        k.act(sq[:], X[:], AF.Square, [Xtok], [sqtok])
        for c in range(8):
            k.mm(bank.ap(), ONES, sq[:, c, :], c == 0, c == 7, [sqtok, cmt], [bank.tok])
        k.act(sd[:], bank.ap(), AF.Sqrt, [bank.tok, epst], [sdtok], bias=epsc[:, 0:1], scale=1.0 / D)
        k.recip(rstd[:], sd[:], [sdtok], [rstok])
        for c in range(8):
            k.stt("dve", hdst(c), X[:, c, :], spt[:, gcol + c:gcol + c + 1], rstd[:],
                  ALU.mult, ALU.mult, [Xtok, sptok, rstok], [htok])

    for l in range(nlayers):
        xsrc, xsrct = (xT_in, dtok["xin"]) if l == 0 else (XT, dtok["XT"])
        last = l == nlayers - 1
        k.ep = l
        k.dma("sp", spt[:], spar[l], [], [sptok])

        with ExitStack() as ph:
            hT = sbuf("hT", [128, 8, S], BF16, ph); hTt = Tok()
            with ExitStack() as ph1:
                xs = Ring([Slot(sbuf(f"xs{i}", [128, 8, 512], F32, ph1)) for i in range(2)])
                sq = sbuf("sq", [128, 8, 512], BF16, ph1); sqt = Tok()
                sd = sbuf("sd", [128, 512], F32, ph1); sdt = Tok()
                rstd = sbuf("rstd", [128, 512], F32, ph1); rst = Tok()
                for tb in range(NB):
                    X = xs.next()
                    k.dma("sp", X.t[:], xview(xsrc, tb), [xsrct], [X.tok])
                    make_hT(X.t, X.tok, SP_GMIX, sq, sqt, sd, sdt, rstd, rst,
                            lambda c, tb=tb: hT[:, c, tb * 512:(tb + 1) * 512], hTt, banks[tb % 6])
                k.flush()
                if stop == 'A1':
                    raise _Stop(nc, k, es)

            wr = Ring([Slot(sbuf(f"wr{i}", [128, 8, 128], BF16, ph)) for i in range(3)])
            ob = Ring([Slot(sbuf(f"ob{i}", [128, S], BF16, ph)) for i in range(2)])
            csr = Ring([Slot(sbuf(f"csr{i}", [128, 2, 512], F32, ph)) for i in range(2)])
            sqb = Ring([Slot(sbuf(f"sqb{i}", [128, 512], BF16, ph)) for i in range(2)])
            sd2 = Ring([Slot(sbuf(f"sd2{i}", [128, 512], F32, ph)) for i in range(2)])
            rs2 = Ring([Slot(sbuf(f"rs2{i}", [128, 512], F32, ph)) for i in range(2)])
            qnb = Ring([Slot(sbuf(f"qnb{i}", [128, 512], BF16, ph)) for i in range(2)])
            t1r = Ring([Slot(sbuf(f"t1r{i}", [128, 512], F32, ph)) for i in range(2)])
            t2r = Ring([Slot(sbuf(f"t2r{i}", [128, 512], F32, ph)) for i in range(2)])
            bk = Ring(banks)

            def load_w(c0, m):
                W = wr.next()
                k.dma("pool", W.t[:, :, 0:m], wview(w_in[l], c0, m), [], [W.tok])
                return W

            def proj_mm(W, m, tb):
                b = bk.next()
                for c in range(8):
                    k.mm(b.ap(0, m), W.t[:, c, 0:m], hT[:, c, tb * 512:(tb + 1) * 512], c == 0, c == 7,
                         [W.tok, hTt], [b.tok])
                return b

            def qk_chunk(c0, m, dst, dstt, row0, gcol, norm, rope):
                W = load_w(c0, m)
                O = ob.next()
                for tb in range(NB):
                    b = proj_mm(W, m, tb)
                    osl = O.t[0:m, tb * 512:(tb + 1) * 512]
                    if norm:
                        SQ = sqb.next(); SD = sd2.next(); RS = rs2.next()
                        k.act(SQ.t[0:m, :], b.ap(0, m), AF.Square, [b.tok], [SQ.tok])
                        b2 = bk.next()
                        k.mm(b2.ap(0, m), cm[0:m, 1, 0:m], SQ.t[0:m, :], True, True, [SQ.tok, cmt], [b2.tok])
                        k.act(SD.t[0:m, :], b2.ap(0, m), AF.Sqrt, [b2.tok, epst], [SD.tok],
                              bias=epsc[0:m, 0:1], scale=1.0 / 64)
                        k.recip(RS.t[0:m, :], SD.t[0:m, :], [SD.tok], [RS.tok])
                        if not rope:
                            k.stt("dve", osl, b.ap(0, m), spt[0:m, gcol:gcol + 1], RS.t[0:m, :],
                                  ALU.mult, ALU.mult, [b.tok, sptok, RS.tok], [O.tok])
                            continue
                        QN = qnb.next()
                        k.stt("dve", QN.t[0:m, :], b.ap(0, m), spt[0:m, gcol:gcol + 1], RS.t[0:m, :],
                              ALU.mult, ALU.mult, [b.tok, sptok, RS.tok], [QN.tok])
                    else:
                        QN = qnb.next()
                        k.act(QN.t[0:m, :], b.ap(0, m), AF.Copy, [b.tok], [QN.tok])
                    CS = csr.next()
                    k.dma("sp", CS.t[:], cs_d[:, :, tb * 512:(tb + 1) * 512], [], [CS.tok])
                    b3 = bk.next()
                    k.mm(b3.ap(0, m), cm[0:m, 2, 0:m], QN.t[0:m, :], True, True, [QN.tok, cmt], [b3.tok])
                    T1 = t1r.next(); T2 = t2r.next()
                    k.tt("dve", T1.t[0:m, :], QN.t[0:m, :], CS.t[0:m, 0, :], ALU.mult, [QN.tok, CS.tok], [T1.tok])
                    k.tt("dve", T2.t[0:m, :], b3.ap(0, m), CS.t[0:m, 1, :], ALU.mult, [b3.tok, CS.tok], [T2.tok])
                    k.tt("pool", osl, T1.t[0:m, :], T2.t[0:m, :], ALU.add, [T1.tok, T2.tok], [O.tok])
                k.dma("sp", dst[row0:row0 + m, :], O.t[0:m, :], [O.tok], [dstt])

            for ch in range(2):
                qk_chunk(C_AQ + ch * 128, 128, QA, dtok["QA"], ch * 128, SP_GAQ, True, False)
                qk_chunk(C_AK + ch * 128, 128, KA, dtok["KA"], ch * 128, SP_GAK, True, False)
                qk_chunk(C_BQ + ch * 128, 128, QB, dtok["QB"], ch * 128, SP_GBQ, True, True)
                qk_chunk(C_BK + ch * 128, 128, KB, dtok["KB"], ch * 128, SP_GBK, True, True)
                qk_chunk(C_IQ + ch * 128, 128, QI, dtok["QI"], ch * 128, 0, False, True)
            qk_chunk(C_IK, 64, KI, dtok["KI"], 0, SP_GIK, True, True)

            wv = sbuf("wv", [128, 8, 512], BF16, ph); wvt = Tok()
            wiw = sbuf("wiw", [128, 8, 4], BF16, ph); wiwt = Tok()
            k.dma("pool", wv[:, :, 0:256], wview(w_in[l], C_AV, 256), [], [wvt])
            k.dma("pool", wv[:, :, 256:512], wview(w_in[l], C_BV, 256), [], [wvt])
            k.dma("pool", wiw[:], wview(w_in[l], C_IW, 4), [], [wiwt])
            vb = Ring([Slot(sbuf(f"vb{i}", [128, 512], BF16, ph)) for i in range(2)])
            wib = sbuf("wib", [128, NT, 4], F32, ph); wibt = Tok()
            for tt_ in range(NT):
                b = bk.next()
                for c in range(8):
                    k.mm(b.ap(), hT[:, c, tt_ * 128:(tt_ + 1) * 128], wv[:, c, :], c == 0, c == 7,
                         [hTt, wvt], [b.tok])
                V = vb.next()
                k.act(V.t[:], b.ap(), AF.Copy, [b.tok], [V.tok])
                k.dma("sp", VAB[tt_ * 128:(tt_ + 1) * 128, :], V.t[:], [V.tok], [dtok["VAB"]])
                b = bk.next()
                for c in range(8):
                    k.mm(b.ap(0, 128, 0, 4), hT[:, c, tt_ * 128:(tt_ + 1) * 128], wiw[:, c, :], c == 0, c == 7,
                         [hTt, wiwt], [b.tok])
                k.ts("dve", wib[:, tt_, :], b.ap(0, 128, 0, 4), 0.0625, None, ALU.mult, None, [b.tok], [wibt])
            k.dma("sp", WI.rearrange("(t p) c -> p t c", p=128), wib[:], [wibt], [dtok["WI"]])

            k.act(clam[:], spt[:, SP_LAM:SP_LAM + 4], AF.Exp, [sptok], [clamtok], scale=-1.0)
            k.act(clam[:], clam[:], AF.Ln, [clamtok], [clamtok], bias=1.0)
            k.ts("dve", clam[:], clam[:], -8.0, None, ALU.mult, None, [clamtok], [clamtok])
            cxb = sbuf("cxb", [128, 3 + S], F32, ph)
            cxt = [Tok() for _ in range(NB + 1)]
            k.memset("dve", cxb[:, 0:3], 0.0, [cxt[NB]])
            wbd = Ring([Slot(sbuf(f"wbd{i}", [128, 2, 128], BF16, ph)) for i in range(2)])
            f32r = {n: Ring([Slot(sbuf(f"c{n}{i}", [128, 512], F32, ph)) for i in range(2)])
                    for n in ("gy", "u", "r", "i", "a", "m", "bb", "hs")}
            ubr = Ring([Slot(sbuf(f"ub{i}", [128, 512], BF16, ph)) for i in range(2)])
            for cc in range(4):
                WX = load_w(C_CX + cc * 128, 128)
                WY = load_w(C_CY + cc * 128, 128)
                BD = wbd.next()
                k.dma("pool", BD.t[:], lrubd[l, :, cc].rearrange("m p n -> p m n"), [], [BD.tok])
                O = ob.next()
                prev_hs = None
                for tb in range(NB):
                    b = proj_mm(WX, 128, tb)
                    k.act(cxb[:, 3 + tb * 512:3 + (tb + 1) * 512], b.ap(), AF.Copy, [b.tok], [cxt[tb]])
                    b = proj_mm(WY, 128, tb)
                    GY = f32r["gy"].next()
                    k.act(GY.t[:], b.ap(), AF.Gelu, [b.tok], [GY.tok])
                    U = f32r["u"].next()
                    rd = [cxt[tb], cxt[tb - 1] if tb > 0 else cxt[NB], sptok]
                    cw = lambda j: spt[:, SP_CW + j * 4 + cc:SP_CW + j * 4 + cc + 1]
                    k.ts("dve", U.t[:], cxb[:, tb * 512:tb * 512 + 512], cw(0),
                         spt[:, SP_CB + cc:SP_CB + cc + 1], ALU.mult, ALU.add, rd, [U.tok])
                    for j in range(1, 4):
                        k.stt("dve", U.t[:], cxb[:, tb * 512 + j:tb * 512 + j + 512], cw(j), U.t[:],
                              ALU.mult, ALU.add, rd + [U.tok], [U.tok])
                    UB = ubr.next()
                    k.act(UB.t[:], U.t[:], AF.Copy, [U.tok], [UB.tok])
                    bR = bk.next()
                    k.mm(bR.ap(), BD.t[:, 0, :], UB.t[:], True, True, [BD.tok, UB.tok], [bR.tok])
                    bI = bk.next()
                    k.mm(bI.ap(), BD.t[:, 1, :], UB.t[:], True, True, [BD.tok, UB.tok], [bI.tok])
                    R = f32r["r"].next(); I_ = f32r["i"].next(); A = f32r["a"].next(); M = f32r["m"].next()
                    k.act(R.t[:], bR.ap(), AF.Sigmoid, [bR.tok, sptok], [R.tok], bias=spt[:, SP_BA + cc:SP_BA + cc + 1])
                    k.act(I_.t[:], bI.ap(), AF.Sigmoid, [bI.tok, sptok], [I_.tok], bias=spt[:, SP_BX + cc:SP_BX + cc + 1])
                    k.act(A.t[:], R.t[:], AF.Exp, [R.tok, clamtok], [A.tok], scale=clam[:, cc:cc + 1])
                    k.act(M.t[:], A.t[:], AF.Square, [A.tok], [M.tok])
                    k.act(M.t[:], M.t[:], AF.Sqrt, [M.tok], [M.tok], bias=1.0, scale=-1.0)
                    BB = f32r["bb"].next()
                    k.tt("pool", BB.t[:], I_.t[:], U.t[:], ALU.mult, [I_.tok, U.tok], [BB.tok])
                    k.tt("pool", BB.t[:], BB.t[:], M.t[:], ALU.mult, [BB.tok, M.tok], [BB.tok])
                    HS = f32r["hs"].next()
                    if prev_hs is None:
                        k.op("dve", lambda e, HS=HS, A=A, BB=BB: e.tensor_tensor_scan(
                            out=HS.t[:], data0=A.t[:], data1=BB.t[:], initial=0.0, op0=ALU.mult, op1=ALU.add),
                            [A.tok, BB.tok], [HS.tok])
                    else:
                        k.op("dve", lambda e, HS=HS, A=A, BB=BB, PH=prev_hs: e.tensor_tensor_scan(
                            out=HS.t[:], data0=A.t[:], data1=BB.t[:], initial=PH.t[:, 511:512],
                            op0=ALU.mult, op1=ALU.add), [A.tok, BB.tok, prev_hs.tok], [HS.tok])
                    prev_hs = HS
                    k.tt("pool", O.t[:, tb * 512:(tb + 1) * 512], HS.t[:], GY.t[:], ALU.mult,
                         [HS.tok, GY.tok], [O.tok])
                k.dma("sp", YM[512 + cc * 128:512 + (cc + 1) * 128, :], O.t[:], [O.tok], [YMt[2]])
            k.flush()
            if stop == 'A':
                raise _Stop(nc, k, es)

        def finalize_attn(t, acc, rdr, ytr, yor, row0, ymtok, pst_i):
            RD = rdr.next()
            a3 = acc.t[:, acc.c0:acc.c0 + 260].rearrange("p (h e) -> p h e", e=65)
            k.recip(RD.t[:], a3[:, :, 64:65], [acc.tok], [RD.tok])
            YT = ytr.next()
            for h in range(4):
                k.ts("dve", YT.t[:, h * 64:(h + 1) * 64], acc.ap(0, 128, h * 65, h * 65 + 64), RD.t[:, h, :], None,
                     ALU.mult, None, [acc.tok, RD.tok], [YT.tok])
            half = pst_i[0] % 2
            pst_i[0] += 1
            for c in range(2):
                k.tr(PSTs[half][:, c * 128:(c + 1) * 128], YT.t[:, c * 128:(c + 1) * 128], IDENT,
                     [YT.tok, cmt], [pstoks[half]])
            YO = yor.next()
            k.act(YO.t[:], PSTs[half][:, 0:256], AF.Copy, [pstoks[half]], [YO.tok])
            k.dma("sp", YM[row0:row0 + 256, t * 128:(t + 1) * 128].rearrange("(c p) q -> p c q", p=128),
                  YO.t[:].rearrange("p (c q) -> p c q", c=2), [YO.tok], [ymtok])

        with ExitStack() as ph:
            KAs = sbuf("KAs", [128, 2, S], BF16, ph); kat = Tok()
            QAs = sbuf("QAs", [128, 2, S], BF16, ph); qat = Tok()
            VA4 = sbuf("VA4", [128, NT, 4, 65], BF16, ph); vat = Tok()
            EB = sbuf("EB", [128, 4, 5, 128], F32, ph); ebt = Tok()
            AM = sbuf("AM", [128, 5, 128], F32, ph); amt = Tok()
            k.dma("sp", KAs[:], KA.rearrange("(c p) t -> p c t", p=128), [dtok["KA"]], [kat])
            k.dma("sp", QAs[:], QA.rearrange("(c p) t -> p c t", p=128), [dtok["QA"]], [qat])
            for h in range(4):
                k.dma("sp", VA4[:, :, h, 0:64], VAB.rearrange("(t p) c -> p t c", p=128)[:, :, h * 64:(h + 1) * 64],
                      [dtok["VAB"]], [vat])
            k.memset("pool", VA4[:, :, :, 64:65], 1.0, [vat])
            k.dma("sp", EB[:], abias[l], [], [ebt])
            k.dma("sp", AM[:], amask, [], [amt])
            k.act(EB[:], EB[:], AF.Exp, [ebt], [ebt])
            for h in range(4):
                k.tt("dve", EB[:, h], EB[:, h], AM[:], ALU.mult, [ebt, amt], [ebt])
            Er = Ring([Slot(sbuf(f"Ea{i}", [128, 640], F32, ph)) for i in range(2)])
            Emr = Ring([Slot(sbuf(f"Ema{i}", [128, 640], BF16, ph)) for i in range(3)])
            rdr = Ring([Slot(sbuf(f"rda{i}", [128, 4, 1], F32, ph)) for i in range(2)])
            ytr = Ring([Slot(sbuf(f"yta{i}", [128, 256], BF16, ph)) for i in range(2)])
            yor = Ring([Slot(sbuf(f"yoa{i}", [128, 256], BF16, ph)) for i in range(2)])
            stb = Ring([(PS2[0], banks[0], banks[1]), (PS2[1], banks[2], banks[3])])
            accb = Ring([banks[4], banks[5]])
            pst_i = [0]

            def a_stage(t, h):
                ch, pb = h // 2, (h % 2) * 64
                js = [j for j in range(5) if t - 4 + j >= 0]
                j0 = js[0]
                PT_, ba, bb_ = stb.next()
                for j in js:
                    k.mm(PT_[:, j * 128:(j + 1) * 128], KAs[pb:pb + 64, ch, (t - 4 + j) * 128:(t - 3 + j) * 128],
                         QAs[pb:pb + 64, ch, t * 128:(t + 1) * 128], True, True,
                         [kat, qat], [ba.tok if j < 4 else bb_.tok])
                E = Er.next(); Em = Emr.next()
                k.act(E.t[:, j0 * 128:640], PT_[:, j0 * 128:640], AF.Exp, [ba.tok, bb_.tok], [E.tok], scale=0.125)
                k.tt("dve", Em.t[:, j0 * 128:640], E.t[:, j0 * 128:640],
                     EB[:, h, j0:5, :].rearrange("p j q -> p (j q)"), ALU.mult, [E.tok, ebt], [Em.tok])
                return Em, js

            items = [(t, h) for t in range(NT) for h in range(4)]
            pend = a_stage(*items[0])
            acc = None
            for i, (t, h) in enumerate(items):
                Em, js = pend
                if i + 1 < len(items):
                    pend = a_stage(*items[i + 1])
                if h == 0:
                    acc = accb.next()
                for j in js:
                    k.mm(acc.ap(0, 128, h * 65, h * 65 + 65), Em.t[:, j * 128:(j + 1) * 128], VA4[:, t - 4 + j, h, :],
                         j == js[0], j == 4, [vat, Em.tok], [acc.tok])
                if h == 3:
                    finalize_attn(t, acc, rdr, ytr, yor, 0, YMt[0], pst_i)
            k.flush()
            if stop == 'B1':
                raise _Stop(nc, k, es)

        with ExitStack() as ph:
            KI2 = sbuf("KI2", [128, S], BF16, ph); kit = Tok()
            QIs = sbuf("QIs", [128, 2, S], BF16, ph); qit = Tok()
            KBs = sbuf("KBs", [128, 2, S], BF16, ph); kbt = Tok()
            QBs = sbuf("QBs", [128, 2, S], BF16, ph); qbt = Tok()
            VB4 = sbuf("VB4", [128, NT, 4, 65], BF16, ph); vbt = Tok()
            WIs = sbuf("WIs", [128, NT, 4], F32, ph); wit = Tok()
            P2 = sbuf("P2", [128, NBIS + 1], F32, ph); p2t = Tok()
            k.dma("sp", KI2[0:64, :], KI, [dtok["KI"]], [kit])
            k.dma("sp", KI2[64:128, :], KI, [dtok["KI"]], [kit])
            k.dma("sp", QIs[:], QI.rearrange("(c p) t -> p c t", p=128), [dtok["QI"]], [qit])
            k.dma("sp", KBs[:], KB.rearrange("(c p) t -> p c t", p=128), [dtok["KB"]], [kbt])
            k.dma("sp", QBs[:], QB.rearrange("(c p) t -> p c t", p=128), [dtok["QB"]], [qbt])
            for h in range(4):
                k.dma("sp", VB4[:, :, h, 0:64],
                      VAB.rearrange("(t p) c -> p t c", p=128)[:, :, 256 + h * 64:256 + (h + 1) * 64],
                      [dtok["VAB"]], [vbt])
            k.memset("pool", VB4[:, :, :, 64:65], 1.0, [vbt])
            k.dma("sp", WIs[:], WI.rearrange("(t p) c -> p t c", p=128), [dtok["WI"]], [wit])
            k.dma("sp", P2[:], pow2, [], [p2t])
            scr = Ring([Slot(sbuf(f"sc{i}", [128, S], F32, ph)) for i in range(2)])
            rlr = Ring([Slot(sbuf(f"rl{i}", [128, 512], F32, ph)) for i in range(3)])
            mkr = Ring([Slot(sbuf(f"mk{i}", [128, S], BF16, ph)) for i in range(2)])
            mTall = Ring([Slot(sbuf(f"mTa{i}", [128, S], BF16, ph)) for i in range(2)])
            Er = Ring([Slot(sbuf(f"Eb{i}", [128, 512], BF16, ph)) for i in range(3)])
            Emr = Ring([Slot(sbuf(f"Emb{i}", [128, 512], BF16, ph)) for i in range(3)])
            junk = sbuf("junk", [128, S], BF16, ph); jt = Tok()
            smr = Ring([Slot(sbuf(f"sm{i}", [128, 8 + NBIS + 1], F32, ph)) for i in range(2)])
            rdr = Ring([Slot(sbuf(f"rdb{i}", [128, 4, 1], F32, ph)) for i in range(2)])
            ytr = Ring([Slot(sbuf(f"ytb{i}", [128, 256], BF16, ph)) for i in range(2)])
            yor = Ring([Slot(sbuf(f"yob{i}", [128, 256], BF16, ph)) for i in range(2)])
            dbk = Ring([banks[0], banks[1]])
            sbk = Ring([banks[2], banks[3]])
            accb = Ring([banks[4], banks[5]])
            pst_i = [0]

            def prep(t):
                nk = 128 * (t + 1)
                SC = scr.next()
                nblk = (nk + 511) // 512
                for kb_ in range(nblk):
                    w = min(512, nk - kb_ * 512)
                    cs_ = slice(kb_ * 512, kb_ * 512 + w)
                    for h in range(4):
                        ch, pb = h // 2, (h % 2) * 64
                        b = dbk.next()
                        k.mm(b.ap(0, 128, 0, w), QIs[pb:pb + 64, ch, t * 128:(t + 1) * 128], KI2[pb:pb + 64, cs_],
                             True, True, [qit, kit], [b.tok])
                        wsc = WIs[:, t, h:h + 1]
                        if h == 0:
                            k.ts("dve", SC.t[:, cs_], b.ap(0, 128, 0, w), 0.0, wsc, ALU.max, ALU.mult,
                                 [b.tok, wit], [SC.tok])
                        else:
                            RL = rlr.next()
                            k.act(RL.t[:, 0:w], b.ap(0, 128, 0, w), AF.Relu, [b.tok], [RL.tok])
                            k.act(RL.t[:, 0:w], RL.t[:, 0:w], AF.Copy, [RL.tok, wit], [RL.tok], scale=wsc)
                            k.tt("pool", SC.t[:, cs_], SC.t[:, cs_], RL.t[:, 0:w], ALU.add,
                                 [RL.tok, SC.tok], [SC.tok])
                SM = smr.next()
                mx, mn, rng, mid, cnt, dd, thr = (SM.t[:, i:i + 1] for i in range(7))
                steps = SM.t[:, 8:8 + NBIS + 1]
                bis = t >= 2 and not OPT.get('b2_nobis')
                if bis:
                    k.op("dve", lambda e: e.tensor_reduce(out=mx, in_=SC.t[:, 0:nk], axis=AX.X, op=ALU.max),
                         [SC.tok], [SM.tok])
                k.op("dve", lambda e: e.tensor_reduce(out=mn, in_=SC.t[:, 0:nk], axis=AX.X, op=ALU.min),
                     [SC.tok], [SM.tok])
                k.memset("dve", SC.t[0:64, nk - 64:nk], -1.0e30, [SC.tok])
                if bis:
                    k.tt("dve", rng, mx, mn, ALU.subtract, [SM.tok], [SM.tok])
                    k.ts("dve", steps, P2[:], rng, None, ALU.mult, None, [p2t, SM.tok], [SM.tok])
                    k.tt("dve", mid, mn, steps[:, 0:1], ALU.add, [SM.tok], [SM.tok])
                    for it in range(NBIS):
                        k.ts("dve", junk[:, 0:nk], SC.t[:, 0:nk], mid, 0.0, ALU.is_ge, ALU.add,
                             [SC.tok, SM.tok], [jt, SM.tok], accum_out=cnt)
                        k.ts("dve", dd, cnt, 255.5, 0.5, ALU.is_ge, ALU.subtract, [SM.tok], [SM.tok])
                        k.stt("dve", mid, dd, steps[:, it:it + 1], mid, ALU.mult, ALU.add, [SM.tok], [SM.tok])
                    k.tt("dve", thr, mid, steps[:, NBIS:NBIS + 1], ALU.subtract, [SM.tok], [SM.tok])
                else:
                    k.copy("dve", thr, mn, [SM.tok], [SM.tok])
                MK = mkr.next()
                k.ts("dve", MK.t[:, 0:nk], SC.t[:, 0:nk], thr, None, ALU.is_ge, None, [SC.tok, SM.tok], [MK.tok])
                return MK

            def prep_b(t, MK):
                ngrp = (t + 1 + 3) // 4
                MT = mTall.next()
                for g in range(ngrp):
                    jts = list(range(g * 4, min(t + 1, g * 4 + 4)))
                    half = pst_i[0] % 2
                    pst_i[0] += 1
                    for jj, jt_ in enumerate(jts):
                        k.tr(PSTs[half][:, jj * 128:(jj + 1) * 128],
                             MK.t[:, jt_ * 128:(jt_ + 1) * 128], IDENT, [MK.tok, cmt], [pstoks[half]])
                    n = len(jts) * 128
                    k.act(MT.t[:, g * 512:g * 512 + n], PSTs[half][:, 0:n], AF.Copy,
                          [pstoks[half]], [MT.tok])
                return MT

            def b_stage(t, h, g, MT):
                ch, pb = h // 2, (h % 2) * 64
                jts = list(range(g * 4, min(t + 1, g * 4 + 4)))
                n = len(jts) * 128
                b = sbk.next()
                for jj, jt_ in enumerate(jts):
                    k.mm(b.ap(0, 128, jj * 128, (jj + 1) * 128), KBs[pb:pb + 64, ch, jt_ * 128:(jt_ + 1) * 128],
                         QBs[pb:pb + 64, ch, t * 128:(t + 1) * 128], True, True, [kbt, qbt], [b.tok])
                E = Er.next(); Em = Emr.next()
                k.act(E.t[:, 0:n], b.ap(0, 128, 0, n), AF.Exp, [b.tok], [E.tok], scale=0.125)
                k.tt("pool", Em.t[:, 0:n], E.t[:, 0:n], MT.t[:, g * 512:g * 512 + n], ALU.mult,
                     [E.tok, MT.tok], [Em.tok])
                return Em, jts

            ntile = OPT.get('b2_tmax', NT)
            MTs = {0: prep_b(0, prep(0))}
            for t in range(ntile):
                MKn = prep(t + 1) if t + 1 < ntile else None
                if OPT.get('b2_noattn'):
                    continue
                MT = MTs.pop(t)
                ngrp = (t + 1 + 3) // 4
                items = [(h, g) for h in range(4) for g in range(ngrp)]
                pend = b_stage(t, *items[0], MT)
                acc = accb.next()
                for i, (h, g) in enumerate(items):
                    Em, jts = pend
                    if i + 1 < len(items):
                        pend = b_stage(t, *items[i + 1], MT)
                    for jj, jt_ in enumerate(jts):
                        k.mm(acc.ap(0, 128, h * 65, h * 65 + 65), Em.t[:, jj * 128:(jj + 1) * 128], VB4[:, jt_, h, :],
                             jt_ == 0, jt_ == t, [vbt, Em.tok], [acc.tok])
                if MKn is not None:
                    MTs[t + 1] = prep_b(t + 1, MKn)
                finalize_attn(t, acc, rdr, ytr, yor, 256, YMt[1], pst_i)
            k.flush()
            if stop == 'B2':
                raise _Stop(nc, k, es)

        with ExitStack() as ph:
            wg = sbuf("wg", [128, 8, 3072], BF16, ph); wgt = Tok()
            wb = sbuf("wb", [128, 8, 1024], BF16, ph); wbt = Tok()
            wo = sbuf("wo", [128, 8, 1024], BF16, ph); wot = Tok()
            for c in range(8):
                k.dma("pool", wg[:, c, :], w_in[l, c * 128:(c + 1) * 128, C_GT:C_GT + 3072], [], [wgt])
            k.dma("pool", wb[:], wview(w_br[l], 0, 1024), [], [wbt])
            k.dma("pool", wo[:], wview(w_out[l], 0, 1024), [], [wot])
            X = Slot(sbuf("xc", [128, 8, 512], F32, ph))
            sq = sbuf("sqc", [128, 8, 512], BF16, ph); sqt = Tok()
            sd = sbuf("sdc", [128, 512], F32, ph); sdt = Tok()
            rstd = sbuf("rstdc", [128, 512], F32, ph); rst = Tok()
            hTb = sbuf("hTb", [128, 8, 512], BF16, ph); hbt = Tok()
            ym = sbuf("ymc", [128, 8, 512], BF16, ph); ymt = Tok()
            mg = sbuf("mg", [128, 8, 512], BF16, ph); mgt = [Tok() for _ in range(8)]
            sgr = Ring([Slot(sbuf(f"sg{i}", [128, 512], F32, ph)) for i in range(3)])
            tmr = Ring([Slot(sbuf(f"tm{i}", [128, 512], F32, ph)) for i in range(3)])
            acr = Ring([Slot(sbuf(f"ac{i}", [128, 512], F32, ph)) for i in range(2)])
            xo = Ring([Slot(sbuf(f"xo{i}", [128, 512], F32, ph)) for i in range(3)])
            bk = Ring(banks)
            KR = [(0, 2), (2, 4), (4, 8)]
            for tb in range(NB):
                k.dma("sp", X.t[:], xview(xsrc, tb), [xsrct], [X.tok])
                k.dma("sp", ym[:], xview(YM, tb), YMt, [ymt])
                make_hT(X.t, X.tok, SP_GMIX, sq, sqt, sd, sdt, rstd, rst, lambda c: hTb[:, c, :], hbt, bk.next())
                for dc in range(8):
                    AC = acr.next()
                    for br in range(3):
                        bg = bk.next()
                        for c in range(8):
                            k.mm(bg.ap(), wg[:, c, br * 1024 + dc * 128:br * 1024 + (dc + 1) * 128], hTb[:, c, :],
                                 c == 0, c == 7, [wgt, hbt], [bg.tok])
                        SG = sgr.next()
                        bcol = SP_BG + br * 8 + dc
                        k.act(SG.t[:], bg.ap(), AF.Sigmoid, [bg.tok, sptok], [SG.tok], bias=spt[:, bcol:bcol + 1])
                        bb_ = bk.next()
                        k0, k1 = KR[br]
                        for c in range(k0, k1):
                            k.mm(bb_.ap(), wb[:, c, dc * 128:(dc + 1) * 128], ym[:, c, :], c == k0, c == k1 - 1,
                                 [wbt, ymt], [bb_.tok])
                        if br == 0:
                            k.tt("dve", AC.t[:], SG.t[:], bb_.ap(), ALU.mult, [SG.tok, bb_.tok], [AC.tok])
                        else:
                            TM = tmr.next()
                            k.tt("dve", TM.t[:], SG.t[:], bb_.ap(), ALU.mult, [SG.tok, bb_.tok], [TM.tok])
                            if br == 1:
                                k.tt("pool", AC.t[:], AC.t[:], TM.t[:], ALU.add, [AC.tok, TM.tok], [AC.tok])
                            else:
                                k.tt("pool", mg[:, dc, :], AC.t[:], TM.t[:], ALU.add, [AC.tok, TM.tok], [mgt[dc]])
                for oc in range(8):
                    bo = bk.next()
                    for c in range(8):
                        k.mm(bo.ap(), wo[:, c, oc * 128:(oc + 1) * 128], mg[:, c, :], c == 0, c == 7,
                             [wot, mgt[c]], [bo.tok])
                    XO = xo.next()
                    k.tt("dve", XO.t[:], X.t[:, oc, :], bo.ap(), ALU.add, [X.tok, bo.tok], [XO.tok])
                    k.dma("sp", XT[oc * 128:(oc + 1) * 128, tb * 512:(tb + 1) * 512], XO.t[:], [XO.tok], [dtok["XT"]])
            k.flush()
            if stop == 'C':
                raise _Stop(nc, k, es)

        with ExitStack() as ph:
            w1 = sbuf("w1", [128, 8, 2 * DFF], BF16, ph); w1t = Tok()
            w2 = sbuf("w2", [128, 22, 1024], BF16, ph); w2t = Tok()
            for c in range(8):
                k.dma("pool", w1[:, c, :], w_f1[l, c * 128:(c + 1) * 128, :], [], [w1t])
            for c in range(22):
                k.dma("pool", w2[:, c, :], w_f2[l, c * 128:(c + 1) * 128, :], [], [w2t])
            X = Slot(sbuf("xd", [128, 8, 512], F32, ph))
            sq = sbuf("sqd", [128, 8, 512], BF16, ph); sqt = Tok()
            sd = sbuf("sdd", [128, 512], F32, ph); sdt = Tok()
            rstd = sbuf("rstdd", [128, 512], F32, ph); rst = Tok()
            hTb = sbuf("hTd", [128, 8, 512], BF16, ph); hbt = Tok()
            av = sbuf("av", [128, 22, 512], BF16, ph); avt = [Tok() for _ in range(22)]
            sgr = Ring([Slot(sbuf(f"sl{i}", [128, 512], F32, ph)) for i in range(3)])
            xo = Ring([Slot(sbuf(f"xod{i}", [128, 512], F32, ph)) for i in range(3)])
            bk = Ring(banks)
            xdst, xdstt = (yT, dtok["yT"]) if last else (XT, dtok["XT"])
            for tb in range(NB):
                k.dma("sp", X.t[:], xview(XT, tb), [dtok["XT"]], [X.tok])
                make_hT(X.t, X.tok, SP_GFFN, sq, sqt, sd, sdt, rstd, rst, lambda c: hTb[:, c, :], hbt, bk.next())
                for fc in range(22):
                    bg = bk.next()
                    for c in range(8):
                        k.mm(bg.ap(), w1[:, c, fc * 128:(fc + 1) * 128], hTb[:, c, :], c == 0, c == 7,
                             [w1t, hbt], [bg.tok])
                    bu = bk.next()
                    for c in range(8):
                        k.mm(bu.ap(), w1[:, c, DFF + fc * 128:DFF + (fc + 1) * 128], hTb[:, c, :], c == 0, c == 7,
                             [w1t, hbt], [bu.tok])
                    SG = sgr.next()
                    k.act(SG.t[:], bg.ap(), AF.Silu, [bg.tok], [SG.tok])
                    k.tt("dve", av[:, fc, :], SG.t[:], bu.ap(), ALU.mult, [SG.tok, bu.tok], [avt[fc]])
                for oc in range(8):
                    bo = bk.next()
                    for c in range(22):
                        k.mm(bo.ap(), w2[:, c, oc * 128:(oc + 1) * 128], av[:, c, :], c == 0, c == 21,
                             [w2t, avt[c]], [bo.tok])
                    XO = xo.next()
                    k.tt("dve", XO.t[:], X.t[:, oc, :], bo.ap(), ALU.add, [X.tok, bo.tok], [XO.tok])
                    k.dma("sp", xdst[oc * 128:(oc + 1) * 128, tb * 512:(tb + 1) * 512], XO.t[:], [XO.tok], [xdstt])
            if last:
                k.wait_all_dma()
            k.flush()
            if stop == 'D':
                raise _Stop(nc, k, es)
    es.close()
    return nc, k


def host_consts():
    pos = np.arange(S, dtype=np.float32)
    inv = (1.0 / (np.float32(10000.0) ** (np.arange(0, 64, 2, dtype=np.float32) / np.float32(64)))).astype(np.float32)
    ang = pos[:, None] * inv[None, :]
    ang = np.concatenate([ang, ang], axis=-1)
    cosT = np.cos(ang).astype(np.float32).T
    sinT = np.sin(ang).astype(np.float32).T
    cs = np.zeros((128, 2, S), np.float32)
    cs[0:64, 0], cs[64:128, 0] = cosT, cosT
    cs[0:64, 1], cs[64:128, 1] = sinT, sinT
    ones = np.ones((128, 128), np.float32)
    onesblk = np.zeros((128, 128), np.float32)
    onesblk[0:64, 0:64] = 1.0
    onesblk[64:128, 64:128] = 1.0
    rotm = np.zeros((128, 128), np.float32)
    for hb in (0, 64):
        for m in range(32):
            rotm[hb + m + 32, hb + m] = -1.0
            rotm[hb + m, hb + m + 32] = 1.0
    ident = np.eye(128, dtype=np.float32)
    cmat = np.stack([ones, onesblk, rotm, ident]).astype(np.float32)
    kk = np.arange(128)[:, None, None]
    jj = np.arange(5)[None, :, None]
    qq = np.arange(128)[None, None, :]
    cq = (qq >= 64).astype(np.int64)
    ck = 2 * jj - 8 + (kk >= 64)
    amask = ((ck >= cq - 8) & (ck <= cq)).astype(np.float32)
    relidx = np.clip(128 * (4 - jj) + qq - kk, -128, 128) + 128
    pow2 = np.tile((2.0 ** -(np.arange(NBIS + 1) + 1.0)).astype(np.float32)[None, :], (128, 1))
    return cs, cmat, amask, relidx, pow2


def host_pack(inp):
    cs, cmat, amask, relidx, pow2 = host_consts()
    f = lambda a: np.ascontiguousarray(np.asarray(a, dtype=np.float32))
    spar = np.zeros((L, 128, NSP), np.float32)
    p = np.arange(128)
    for l in range(L):
        spar[l, :, SP_GMIX:SP_GMIX + 8] = f(inp["g_mix"])[l].reshape(8, 128).T
        spar[l, :, SP_GFFN:SP_GFFN + 8] = f(inp["g_ffn"])[l].reshape(8, 128).T
        spar[l, :, SP_BG:SP_BG + 24] = f(inp["b_gate"])[l].reshape(24, 128).T
        spar[l, :, SP_GAQ] = f(inp["qk_gain_a"])[l, 0][p % 64]
        spar[l, :, SP_GAK] = f(inp["qk_gain_a"])[l, 1][p % 64]
        spar[l, :, SP_GBQ] = f(inp["qk_gain_b"])[l, 0][p % 64]
        spar[l, :, SP_GBK] = f(inp["qk_gain_b"])[l, 1][p % 64]
        spar[l, :, SP_GIK] = f(inp["g_idx_k"])[l][p % 64]
        cw = f(inp["conv_w"])[l]
        for j in range(4):
            spar[l, :, SP_CW + j * 4:SP_CW + j * 4 + 4] = cw[j].reshape(4, 128).T
        spar[l, :, SP_CB:SP_CB + 4] = f(inp["conv_b"])[l].reshape(4, 128).T
        spar[l, :, SP_BA:SP_BA + 4] = f(inp["lru_ba"])[l].reshape(4, 128).T
        spar[l, :, SP_BX:SP_BX + 4] = f(inp["lru_bx"])[l].reshape(4, 128).T
        spar[l, :, SP_LAM:SP_LAM + 4] = f(inp["lru_lambda"])[l].reshape(4, 128).T
    lrubd = np.zeros((L, 2, 4, 128, 128), np.float32)
    for m, nm in enumerate(("lru_wa", "lru_wx")):
        wsrc = f(inp[nm])
        for cc in range(4):
            lrubd[:, m, cc, 0:64, 0:64] = wsrc[:, 2 * cc]
            lrubd[:, m, cc, 64:128, 64:128] = wsrc[:, 2 * cc + 1]
    rb = f(inp["rel_bias"])
    ab = rb[:, :, relidx]
    abias = np.ascontiguousarray(ab.transpose(0, 2, 1, 3, 4))
    shared = {"w_in": f(inp["w_in"]), "w_branch": f(inp["w_branch"]), "w_out": f(inp["w_out"]),
              "w_ffn_in": f(inp["w_ffn_in"]), "w_ffn_out": f(inp["w_ffn_out"]),
              "spar": spar, "lrubd": lrubd, "abias": abias, "cs": cs, "cmat": cmat,
              "amask": amask, "pow2": pow2}
    return shared


_CACHE = {}


def kernel(**inputs):
    x = np.asarray(inputs["x"], dtype=np.float32)
    shared = host_pack(inputs)
    if "nc" not in _CACHE:
        _CACHE["nc"] = build()[0]
    nc = _CACHE["nc"]
    in_maps = []
    for b in range(8):
        m = dict(shared)
        m["xT"] = np.ascontiguousarray(x[b].T)
        in_maps.append(m)
    res = run_bass_kernel_spmd(nc, in_maps, core_ids=list(range(8)))
    out = np.stack([np.ascontiguousarray(r["yT"].T) for r in res.results], axis=0)
    return out.astype(np.float32)
```

```python
import math
from contextlib import ExitStack
import numpy as np
import concourse.bass as bass
import concourse.mybir as mybir
from concourse.bass_utils import run_bass_kernel_spmd

F32 = mybir.dt.float32
BF16 = mybir.dt.bfloat16
AF = mybir.ActivationFunctionType
ALU = mybir.AluOpType
AX = mybir.AxisListType

D = 1024; S = 4096; L = 4; DIN = 5956; DFF = 2816
NT = S // 128; NB = S // 512
EPS = 1e-6
NBIS = 14
C_AQ, C_AK, C_AV, C_BQ, C_BK, C_BV, C_IQ, C_IK, C_IW, C_CX, C_CY, C_GT = (
    0, 256, 512, 768, 1024, 1280, 1536, 1792, 1856, 1860, 2372, 2884)
SP_GMIX, SP_GFFN, SP_BG, SP_GAQ, SP_GAK, SP_GBQ, SP_GBK, SP_GIK, SP_CW, SP_CB, SP_BA, SP_BX, SP_LAM = (
    0, 8, 16, 40, 41, 42, 43, 44, 45, 61, 65, 69, 73)
NSP = 77


class Tok:
    __slots__ = ("w", "r")

    def __init__(self):
        self.w = None
        self.r = {}


class Ring:
    def __init__(self, items):
        self.items = items
        self.i = -1

    def next(self):
        self.i = (self.i + 1) % len(self.items)
        return self.items[self.i]


class Slot:
    def __init__(self, t):
        self.t = t
        self.tok = Tok()


class K:
    INC = {"pe": 1, "act": 1, "dve": 1, "pool": 1, "dsp": 16, "dpool": 16, "dact": 16}

    def __init__(self, nc, es):
        self.nc = nc
        self.sem = {f"{n}@{l}": es.enter_context(nc.semaphore(f"s_{n}_{l}")) for n in self.INC for l in range(L)}
        self.cnt = {n: 0 for n in self.sem}
        self.ep = 0
        self.streams = {e: [] for e in ("pe", "act", "dve", "pool", "sp")}
        self.waited = {e: {} for e in self.streams}
        self.ninstr = 0

    def op(self, stream, fn, reads=(), writes=(), counter=None):
        counter = f"{counter or stream}@{self.ep}"
        deps = {}
        for t in reads:
            if t.w is not None and deps.get(t.w[0], 0) < t.w[1]:
                deps[t.w[0]] = t.w[1]
        for t in writes:
            if t.w is not None and deps.get(t.w[0], 0) < t.w[1]:
                deps[t.w[0]] = t.w[1]
            for c, s in t.r.items():
                if deps.get(c, 0) < s:
                    deps[c] = s
        wd = self.waited[stream]
        waits = []
        for c, s in deps.items():
            if c[:3] == "pe@" and counter[:3] == "pe@":
                continue
            if wd.get(c, 0) >= s:
                continue
            wd[c] = s
            waits.append((c, s))
        self.cnt[counter] += 1
        seq = self.cnt[counter]
        for t in reads:
            if t.r.get(counter, 0) < seq:
                t.r[counter] = seq
        for t in writes:
            t.w = (counter, seq)
            t.r = {}
        self.streams[stream].append((waits, fn, counter))
        self.ninstr += 1

    def wait_all_dma(self):
        waits = [(c, self.cnt[c]) for c in self.cnt if c[0] == "d" and c[:3] != "dve" and self.cnt[c] > 0]
        self.streams["sp"].append((waits, None, None))

    def flush(self):
        nc = self.nc
        streams = self.streams
        self.streams = {e: [] for e in streams}
        sem, INC = self.sem, self.INC

        def mk(lst):
            def f(eng):
                for waits, fn, counter in lst:
                    for c, s in waits:
                        eng.wait_ge(sem[c], s * INC[c.split("@")[0]])
                    if fn is not None:
                        fn(eng).then_inc(sem[counter], INC[counter.split("@")[0]])
            return f

        with nc.Block() as block:
            block.tensor(mk(streams["pe"]))
            block.scalar(mk(streams["act"]))
            block.vector(mk(streams["dve"]))
            block.gpsimd(mk(streams["pool"]))
            block.sync(mk(streams["sp"]))

    def dma(self, q, out, in_, reads, writes):
        self.op(q, lambda e: e.dma_start(out=out, in_=in_), reads, writes, counter="d" + q)

    def mm(self, out, lhsT, rhs, start, stop, reads, writes):
        self.op("pe", lambda e: e.matmul(out, lhsT, rhs, start=start, stop=stop), reads, writes)

    def tr(self, out, in_, ident, reads, writes):
        self.op("pe", lambda e: e.transpose(out, in_, ident), reads, writes)

    def act(self, out, in_, func, reads, writes, bias=None, scale=None):
        kw = {}
        if bias is not None:
            kw["bias"] = bias
        if scale is not None:
            kw["scale"] = scale
        self.op("act", lambda e: e.activation(out=out, in_=in_, func=func, **kw), reads, writes)

    def tt(self, eng, out, in0, in1, op, reads, writes):
        self.op(eng, lambda e: e.tensor_tensor(out=out, in0=in0, in1=in1, op=op), reads, writes)

    def ts(self, eng, out, in0, s1, s2, op0, op1, reads, writes, accum_out=None):
        if op1 is None:
            self.op(eng, lambda e: e.tensor_scalar(out=out, in0=in0, scalar1=s1, scalar2=None, op0=op0),
                    reads, writes)
        elif accum_out is None:
            self.op(eng, lambda e: e.tensor_scalar(out=out, in0=in0, scalar1=s1, scalar2=s2, op0=op0, op1=op1),
                    reads, writes)
        else:
            self.op(eng, lambda e: e.tensor_scalar(out=out, in0=in0, scalar1=s1, scalar2=s2, op0=op0, op1=op1,
                                                   accum_out=accum_out), reads, writes)

    def stt(self, eng, out, in0, scalar, in1, op0, op1, reads, writes):
        self.op(eng, lambda e: e.scalar_tensor_tensor(out=out, in0=in0, scalar=scalar, in1=in1, op0=op0, op1=op1),
                reads, writes)

    def recip(self, out, in_, reads, writes):
        self.op("dve", lambda e: e.reciprocal(out=out, in_=in_), reads, writes)

    def memset(self, eng, ap, val, writes):
        self.op(eng, lambda e: e.memset(ap, val), (), writes)

    def copy(self, eng, out, in_, reads, writes):
        self.op(eng, lambda e: e.tensor_copy(out=out, in_=in_), reads, writes)


OPT = {}


class _Stop(Exception):
    pass


def build(nlayers=L, dbg=False, stop=None):
    try:
        return _build(nlayers, dbg, stop)
    except _Stop as e:
        nc, k, es = e.args
        k.wait_all_dma()
        k.flush()
        return nc, k


def _build(nlayers, dbg, stop):
    nc = bass.Bass("TRN2", target_bir_lowering=False)
    es = ExitStack()

    def din(name, shape, dt=F32):
        return nc.dram_tensor(name, list(shape), dt, kind="ExternalInput").ap()

    kind_dbg = "ExternalOutput" if dbg else "Internal"

    def dscr(name, shape, dt):
        return nc.dram_tensor(name, list(shape), dt, kind=kind_dbg).ap()

    xT_in = din("xT", [D, S])
    w_in = din("w_in", [L, D, DIN])
    w_br = din("w_branch", [L, D, D])
    w_out = din("w_out", [L, D, D])
    w_f1 = din("w_ffn_in", [L, D, 2 * DFF])
    w_f2 = din("w_ffn_out", [L, DFF, D])
    spar = din("spar", [L, 128, NSP])
    lrubd = din("lrubd", [L, 2, 4, 128, 128])
    abias = din("abias", [L, 128, 4, 5, 128])
    cs_d = din("cs", [128, 2, S])
    cmat = din("cmat", [4, 128, 128])
    amask = din("amask", [128, 5, 128])
    pow2 = din("pow2", [128, NBIS + 1])
    yT = nc.dram_tensor("yT", [D, S], F32, kind="ExternalOutput").ap()

    XT = dscr("XT", [D, S], F32)
    QA = dscr("QA", [256, S], BF16)
    KA = dscr("KA", [256, S], BF16)
    QB = dscr("QB", [256, S], BF16)
    KB = dscr("KB", [256, S], BF16)
    QI = dscr("QI", [256, S], BF16)
    KI = dscr("KI", [64, S], BF16)
    VAB = dscr("VAB", [S, 512], BF16)
    WI = dscr("WI", [S, 4], F32)
    YM = dscr("YM", [D, S], BF16)
    dtok = {n: Tok() for n in ("XT", "QA", "KA", "QB", "KB", "QI", "KI", "VAB", "WI", "YM", "xin", "yT")}
    YMt = [Tok() for _ in range(3)]

    k = K(nc, es)

    PS2 = [es.enter_context(nc.psum_tensor(f"ps2_{i}", [128, 1024], F32)) for i in range(3)]
    PSTs = [es.enter_context(nc.psum_tensor(f"pst{i}", [128, 1024], BF16)) for i in range(2)]

    class Bank:
        def __init__(self, t, c0):
            self.t, self.c0, self.tok = t, c0, Tok()

        def ap(self, p0=0, p1=128, a=0, b=512):
            return self.t[p0:p1, self.c0 + a:self.c0 + b]

    banks = []
    for t in PS2:
        banks.append(Bank(t, 0))
        banks.append(Bank(t, 512))
    pstoks = [Tok(), Tok()]

    uid = [0]

    def sbuf(name, shape, dt, stack=es):
        uid[0] += 1
        return stack.enter_context(nc.sbuf_tensor(f"{name}_{uid[0]}", list(shape), dt))

    cm = sbuf("cm", [128, 4, 128], BF16)
    cmt = Tok()
    epsc = sbuf("epsc", [128, 1], F32)
    epst = Tok()
    spt = sbuf("spt", [128, NSP], F32)
    sptok = Tok()
    clam = sbuf("clam", [128, 4], F32)
    clamtok = Tok()
    k.dma("pool", cm[:], cmat.rearrange("m p n -> p m n"), [], [cmt])
    k.memset("dve", epsc[:], EPS, [epst])
    ONES, ONESBLK, ROTM, IDENT = (cm[:, i, :] for i in range(4))

    def xview(ap2d, tb):
        return ap2d.rearrange("(c p) t -> p c t", p=128)[:, :, tb * 512:(tb + 1) * 512]

    def wview(w2d, c0, n):
        return w2d.rearrange("(c p) n -> p c n", p=128)[:, :, c0:c0 + n]

    def make_hT(X, Xtok, gcol, sq, sqtok, sd, sdtok, rstd, rstok, hdst, htok, bank):
        k.act(sq[:], X[:], AF.Square, [Xtok], [sqtok])
        for c in range(8):
            k.mm(bank.ap(), ONES, sq[:, c, :], c == 0, c == 7, [sqtok, cmt], [bank.tok])
        k.act(sd[:], bank.ap(), AF.Sqrt, [bank.tok, epst], [sdtok], bias=epsc[:, 0:1], scale=1.0 / D)
        k.recip(rstd[:], sd[:], [sdtok], [rstok])
        for c in range(8):
            k.stt("dve", hdst(c), X[:, c, :], spt[:, gcol + c:gcol + c + 1], rstd[:],
                  ALU.mult, ALU.mult, [Xtok, sptok, rstok], [htok])

    for l in range(nlayers):
        xsrc, xsrct = (xT_in, dtok["xin"]) if l == 0 else (XT, dtok["XT"])
        last = l == nlayers - 1
        k.ep = l
        k.dma("sp", spt[:], spar[l], [], [sptok])

        with ExitStack() as ph:
            hT = sbuf("hT", [128, 8, S], BF16, ph); hTt = Tok()
            with ExitStack() as ph1:
                xs = Ring([Slot(sbuf(f"xs{i}", [128, 8, 512], F32, ph1)) for i in range(2)])
                sq = sbuf("sq", [128, 8, 512], BF16, ph1); sqt = Tok()
                sd = sbuf("sd", [128, 512], F32, ph1); sdt = Tok()
                rstd = sbuf("rstd", [128, 512], F32, ph1); rst = Tok()
                for tb in range(NB):
                    X = xs.next()
                    k.dma("sp", X.t[:], xview(xsrc, tb), [xsrct], [X.tok])
                    make_hT(X.t, X.tok, SP_GMIX, sq, sqt, sd, sdt, rstd, rst,
                            lambda c, tb=tb: hT[:, c, tb * 512:(tb + 1) * 512], hTt, banks[tb % 6])
                k.flush()
                if stop == 'A1':
                    raise _Stop(nc, k, es)

            wr = Ring([Slot(sbuf(f"wr{i}", [128, 8, 128], BF16, ph)) for i in range(3)])
            ob = Ring([Slot(sbuf(f"ob{i}", [128, S], BF16, ph)) for i in range(2)])
            csr = Ring([Slot(sbuf(f"csr{i}", [128, 2, 512], F32, ph)) for i in range(3)])
            sqb = Ring([Slot(sbuf(f"sqb{i}", [128, 512], BF16, ph)) for i in range(3)])
            sd2 = Ring([Slot(sbuf(f"sd2{i}", [128, 512], F32, ph)) for i in range(3)])
            rs2 = Ring([Slot(sbuf(f"rs2{i}", [128, 512], F32, ph)) for i in range(3)])
            qnb = Ring([Slot(sbuf(f"qnb{i}", [128, 512], BF16, ph)) for i in range(3)])
            t1r = Ring([Slot(sbuf(f"t1r{i}", [128, 512], F32, ph)) for i in range(3)])
            t2r = Ring([Slot(sbuf(f"t2r{i}", [128, 512], F32, ph)) for i in range(3)])
            bk = Ring(banks)

            def load_w(c0, m):
                W = wr.next()
                k.dma("pool", W.t[:, :, 0:m], wview(w_in[l], c0, m), [], [W.tok])
                return W

            def proj_mm(W, m, tb):
                b = bk.next()
                for c in range(8):
                    k.mm(b.ap(0, m), W.t[:, c, 0:m], hT[:, c, tb * 512:(tb + 1) * 512], c == 0, c == 7,
                         [W.tok, hTt], [b.tok])
                return b

            def qk_chunk(c0, m, dst, dstt, row0, gcol, norm, rope):
                W = load_w(c0, m)
                O = ob.next()
                for tb in range(NB):
                    b = proj_mm(W, m, tb)
                    osl = O.t[0:m, tb * 512:(tb + 1) * 512]
                    if norm:
                        SQ = sqb.next(); SD = sd2.next(); RS = rs2.next()
                        k.act(SQ.t[0:m, :], b.ap(0, m), AF.Square, [b.tok], [SQ.tok])
                        b2 = bk.next()
                        k.mm(b2.ap(0, m), cm[0:m, 1, 0:m], SQ.t[0:m, :], True, True, [SQ.tok, cmt], [b2.tok])
                        k.act(SD.t[0:m, :], b2.ap(0, m), AF.Sqrt, [b2.tok, epst], [SD.tok],
                              bias=epsc[0:m, 0:1], scale=1.0 / 64)
                        k.recip(RS.t[0:m, :], SD.t[0:m, :], [SD.tok], [RS.tok])
                        if not rope:
                            k.stt("dve", osl, b.ap(0, m), spt[0:m, gcol:gcol + 1], RS.t[0:m, :],
                                  ALU.mult, ALU.mult, [b.tok, sptok, RS.tok], [O.tok])
                            continue
                        QN = qnb.next()
                        k.stt("dve", QN.t[0:m, :], b.ap(0, m), spt[0:m, gcol:gcol + 1], RS.t[0:m, :],
                              ALU.mult, ALU.mult, [b.tok, sptok, RS.tok], [QN.tok])
                    else:
                        QN = qnb.next()
                        k.act(QN.t[0:m, :], b.ap(0, m), AF.Copy, [b.tok], [QN.tok])
                    CS = csr.next()
                    k.dma("sp", CS.t[:], cs_d[:, :, tb * 512:(tb + 1) * 512], [], [CS.tok])
                    b3 = bk.next()
                    k.mm(b3.ap(0, m), cm[0:m, 2, 0:m], QN.t[0:m, :], True, True, [QN.tok, cmt], [b3.tok])
                    T1 = t1r.next(); T2 = t2r.next()
                    k.tt("dve", T1.t[0:m, :], QN.t[0:m, :], CS.t[0:m, 0, :], ALU.mult, [QN.tok, CS.tok], [T1.tok])
                    k.tt("dve", T2.t[0:m, :], b3.ap(0, m), CS.t[0:m, 1, :], ALU.mult, [b3.tok, CS.tok], [T2.tok])
                    k.tt("pool", osl, T1.t[0:m, :], T2.t[0:m, :], ALU.add, [T1.tok, T2.tok], [O.tok])
                k.dma("sp", dst[row0:row0 + m, :], O.t[0:m, :], [O.tok], [dstt])

            for ch in range(2):
                qk_chunk(C_AQ + ch * 128, 128, QA, dtok["QA"], ch * 128, SP_GAQ, True, False)
                qk_chunk(C_AK + ch * 128, 128, KA, dtok["KA"], ch * 128, SP_GAK, True, False)
                qk_chunk(C_BQ + ch * 128, 128, QB, dtok["QB"], ch * 128, SP_GBQ, True, True)
                qk_chunk(C_BK + ch * 128, 128, KB, dtok["KB"], ch * 128, SP_GBK, True, True)
                qk_chunk(C_IQ + ch * 128, 128, QI, dtok["QI"], ch * 128, 0, False, True)
            qk_chunk(C_IK, 64, KI, dtok["KI"], 0, SP_GIK, True, True)

            wv = sbuf("wv", [128, 8, 512], BF16, ph); wvt = Tok()
            wiw = sbuf("wiw", [128, 8, 4], BF16, ph); wiwt = Tok()
            k.dma("pool", wv[:, :, 0:256], wview(w_in[l], C_AV, 256), [], [wvt])
            k.dma("pool", wv[:, :, 256:512], wview(w_in[l], C_BV, 256), [], [wvt])
            k.dma("pool", wiw[:], wview(w_in[l], C_IW, 4), [], [wiwt])
            vb = Ring([Slot(sbuf(f"vb{i}", [128, 512], BF16, ph)) for i in range(2)])
            wib = sbuf("wib", [128, NT, 4], F32, ph); wibt = Tok()
            for tt_ in range(NT):
                b = bk.next()
                for c in range(8):
                    k.mm(b.ap(), hT[:, c, tt_ * 128:(tt_ + 1) * 128], wv[:, c, :], c == 0, c == 7,
                         [hTt, wvt], [b.tok])
                V = vb.next()
                k.act(V.t[:], b.ap(), AF.Copy, [b.tok], [V.tok])
                k.dma("sp", VAB[tt_ * 128:(tt_ + 1) * 128, :], V.t[:], [V.tok], [dtok["VAB"]])
                b = bk.next()
                for c in range(8):
                    k.mm(b.ap(0, 128, 0, 4), hT[:, c, tt_ * 128:(tt_ + 1) * 128], wiw[:, c, :], c == 0, c == 7,
                         [hTt, wiwt], [b.tok])
                k.ts("dve", wib[:, tt_, :], b.ap(0, 128, 0, 4), 0.0625, None, ALU.mult, None, [b.tok], [wibt])
            k.dma("sp", WI.rearrange("(t p) c -> p t c", p=128), wib[:], [wibt], [dtok["WI"]])

            k.act(clam[:], spt[:, SP_LAM:SP_LAM + 4], AF.Exp, [sptok], [clamtok], scale=-1.0)
            k.act(clam[:], clam[:], AF.Ln, [clamtok], [clamtok], bias=1.0)
            k.ts("dve", clam[:], clam[:], -8.0, None, ALU.mult, None, [clamtok], [clamtok])
            cxb = sbuf("cxb", [128, 3 + S], F32, ph)
            cxt = [Tok() for _ in range(NB + 1)]
            k.memset("dve", cxb[:, 0:3], 0.0, [cxt[NB]])
            wbd = Ring([Slot(sbuf(f"wbd{i}", [128, 2, 128], BF16, ph)) for i in range(2)])
            f32r = {n: Ring([Slot(sbuf(f"c{n}{i}", [128, 512], F32, ph)) for i in range(2)])
                    for n in ("gy", "u", "r", "i", "a", "m", "bb", "hs")}
            ubr = Ring([Slot(sbuf(f"ub{i}", [128, 512], BF16, ph)) for i in range(2)])
            for cc in range(4):
                WX = load_w(C_CX + cc * 128, 128)
                WY = load_w(C_CY + cc * 128, 128)
                BD = wbd.next()
                k.dma("pool", BD.t[:], lrubd[l, :, cc].rearrange("m p n -> p m n"), [], [BD.tok])
                O = ob.next()
                prev_hs = None
                for tb in range(NB):
                    b = proj_mm(WX, 128, tb)
                    k.act(cxb[:, 3 + tb * 512:3 + (tb + 1) * 512], b.ap(), AF.Copy, [b.tok], [cxt[tb]])
                    b = proj_mm(WY, 128, tb)
                    GY = f32r["gy"].next()
                    k.act(GY.t[:], b.ap(), AF.Gelu, [b.tok], [GY.tok])
                    U = f32r["u"].next()
                    rd = [cxt[tb], cxt[tb - 1] if tb > 0 else cxt[NB], sptok]
                    cw = lambda j: spt[:, SP_CW + j * 4 + cc:SP_CW + j * 4 + cc + 1]
                    k.ts("dve", U.t[:], cxb[:, tb * 512:tb * 512 + 512], cw(0),
                         spt[:, SP_CB + cc:SP_CB + cc + 1], ALU.mult, ALU.add, rd, [U.tok])
                    for j in range(1, 4):
                        k.stt("dve", U.t[:], cxb[:, tb * 512 + j:tb * 512 + j + 512], cw(j), U.t[:],
                              ALU.mult, ALU.add, rd + [U.tok], [U.tok])
                    UB = ubr.next()
                    k.act(UB.t[:], U.t[:], AF.Copy, [U.tok], [UB.tok])
                    bR = bk.next()
                    k.mm(bR.ap(), BD.t[:, 0, :], UB.t[:], True, True, [BD.tok, UB.tok], [bR.tok])
                    bI = bk.next()
                    k.mm(bI.ap(), BD.t[:, 1, :], UB.t[:], True, True, [BD.tok, UB.tok], [bI.tok])
                    R = f32r["r"].next(); I_ = f32r["i"].next(); A = f32r["a"].next(); M = f32r["m"].next()
                    k.act(R.t[:], bR.ap(), AF.Sigmoid, [bR.tok, sptok], [R.tok], bias=spt[:, SP_BA + cc:SP_BA + cc + 1])
                    k.act(I_.t[:], bI.ap(), AF.Sigmoid, [bI.tok, sptok], [I_.tok], bias=spt[:, SP_BX + cc:SP_BX + cc + 1])
                    k.act(A.t[:], R.t[:], AF.Exp, [R.tok, clamtok], [A.tok], scale=clam[:, cc:cc + 1])
                    k.act(M.t[:], A.t[:], AF.Square, [A.tok], [M.tok])
                    k.act(M.t[:], M.t[:], AF.Sqrt, [M.tok], [M.tok], bias=1.0, scale=-1.0)
                    BB = f32r["bb"].next()
                    k.tt("pool", BB.t[:], I_.t[:], U.t[:], ALU.mult, [I_.tok, U.tok], [BB.tok])
                    k.tt("pool", BB.t[:], BB.t[:], M.t[:], ALU.mult, [BB.tok, M.tok], [BB.tok])
                    HS = f32r["hs"].next()
                    if prev_hs is None:
                        k.op("dve", lambda e, HS=HS, A=A, BB=BB: e.tensor_tensor_scan(
                            out=HS.t[:], data0=A.t[:], data1=BB.t[:], initial=0.0, op0=ALU.mult, op1=ALU.add),
                            [A.tok, BB.tok], [HS.tok])
                    else:
                        k.op("dve", lambda e, HS=HS, A=A, BB=BB, PH=prev_hs: e.tensor_tensor_scan(
                            out=HS.t[:], data0=A.t[:], data1=BB.t[:], initial=PH.t[:, 511:512],
                            op0=ALU.mult, op1=ALU.add), [A.tok, BB.tok, prev_hs.tok], [HS.tok])
                    prev_hs = HS
                    k.tt("pool", O.t[:, tb * 512:(tb + 1) * 512], HS.t[:], GY.t[:], ALU.mult,
                         [HS.tok, GY.tok], [O.tok])
                k.dma("sp", YM[512 + cc * 128:512 + (cc + 1) * 128, :], O.t[:], [O.tok], [YMt[2]])
            k.flush()
            if stop == 'A':
                raise _Stop(nc, k, es)

        def finalize_attn(t, acc, rdr, ytr, yor, row0, ymtok, pst_i):
            RD = rdr.next()
            a3 = acc.t[:, acc.c0:acc.c0 + 260].rearrange("p (h e) -> p h e", e=65)
            k.recip(RD.t[:], a3[:, :, 64:65], [acc.tok], [RD.tok])
            YT = ytr.next()
            for h in range(4):
                k.ts("dve", YT.t[:, h * 64:(h + 1) * 64], acc.ap(0, 128, h * 65, h * 65 + 64), RD.t[:, h, :], None,
                     ALU.mult, None, [acc.tok, RD.tok], [YT.tok])
            half = pst_i[0] % 2
            pst_i[0] += 1
            for c in range(2):
                k.tr(PSTs[half][:, c * 128:(c + 1) * 128], YT.t[:, c * 128:(c + 1) * 128], IDENT,
                     [YT.tok, cmt], [pstoks[half]])
            YO = yor.next()
            k.act(YO.t[:], PSTs[half][:, 0:256], AF.Copy, [pstoks[half]], [YO.tok])
            k.dma("sp", YM[row0:row0 + 256, t * 128:(t + 1) * 128].rearrange("(c p) q -> p c q", p=128),
                  YO.t[:].rearrange("p (c q) -> p c q", c=2), [YO.tok], [ymtok])

        with ExitStack() as ph:
            KAs = sbuf("KAs", [128, 2, S], BF16, ph); kat = Tok()
            QAs = sbuf("QAs", [128, 2, S], BF16, ph); qat = Tok()
            VA4 = sbuf("VA4", [128, NT, 4, 65], BF16, ph); vat = Tok()
            EB = sbuf("EB", [128, 4, 5, 128], F32, ph); ebt = Tok()
            AM = sbuf("AM", [128, 5, 128], F32, ph); amt = Tok()
            k.dma("sp", KAs[:], KA.rearrange("(c p) t -> p c t", p=128), [dtok["KA"]], [kat])
            k.dma("sp", QAs[:], QA.rearrange("(c p) t -> p c t", p=128), [dtok["QA"]], [qat])
            for h in range(4):
                k.dma("sp", VA4[:, :, h, 0:64], VAB.rearrange("(t p) c -> p t c", p=128)[:, :, h * 64:(h + 1) * 64],
                      [dtok["VAB"]], [vat])
            k.memset("pool", VA4[:, :, :, 64:65], 1.0, [vat])
            k.dma("sp", EB[:], abias[l], [], [ebt])
            k.dma("sp", AM[:], amask, [], [amt])
            k.act(EB[:], EB[:], AF.Exp, [ebt], [ebt])
            for h in range(4):
                k.tt("dve", EB[:, h], EB[:, h], AM[:], ALU.mult, [ebt, amt], [ebt])
            Er = Ring([Slot(sbuf(f"Ea{i}", [128, 640], F32, ph)) for i in range(2)])
            Emr = Ring([Slot(sbuf(f"Ema{i}", [128, 640], BF16, ph)) for i in range(3)])
            rdr = Ring([Slot(sbuf(f"rda{i}", [128, 4, 1], F32, ph)) for i in range(2)])
            ytr = Ring([Slot(sbuf(f"yta{i}", [128, 256], BF16, ph)) for i in range(2)])
            yor = Ring([Slot(sbuf(f"yoa{i}", [128, 256], BF16, ph)) for i in range(2)])
            stb = Ring([(PS2[0], banks[0], banks[1]), (PS2[1], banks[2], banks[3])])
            accb = Ring([banks[4], banks[5]])
            pst_i = [0]

            def a_stage(t, h):
                ch, pb = h // 2, (h % 2) * 64
                js = [j for j in range(5) if t - 4 + j >= 0]
                j0 = js[0]
                PT_, ba, bb_ = stb.next()
                for j in js:
                    k.mm(PT_[:, j * 128:(j + 1) * 128], KAs[pb:pb + 64, ch, (t - 4 + j) * 128:(t - 3 + j) * 128],
                         QAs[pb:pb + 64, ch, t * 128:(t + 1) * 128], True, True,
                         [kat, qat], [ba.tok if j < 4 else bb_.tok])
                E = Er.next(); Em = Emr.next()
                k.act(E.t[:, j0 * 128:640], PT_[:, j0 * 128:640], AF.Exp, [ba.tok, bb_.tok], [E.tok], scale=0.125)
                k.tt("dve", Em.t[:, j0 * 128:640], E.t[:, j0 * 128:640],
                     EB[:, h, j0:5, :].rearrange("p j q -> p (j q)"), ALU.mult, [E.tok, ebt], [Em.tok])
                return Em, js

            items = [(t, h) for t in range(NT) for h in range(4)]
            pend = a_stage(*items[0])
            acc = None
            for i, (t, h) in enumerate(items):
                Em, js = pend
                if i + 1 < len(items):
                    pend = a_stage(*items[i + 1])
                if h == 0:
                    acc = accb.next()
                for j in js:
                    k.mm(acc.ap(0, 128, h * 65, h * 65 + 65), Em.t[:, j * 128:(j + 1) * 128], VA4[:, t - 4 + j, h, :],
                         j == js[0], j == 4, [vat, Em.tok], [acc.tok])
                if h == 3:
                    finalize_attn(t, acc, rdr, ytr, yor, 0, YMt[0], pst_i)
            k.flush()
            if stop == 'B1':
                raise _Stop(nc, k, es)

        with ExitStack() as ph:
            KI2 = sbuf("KI2", [128, S], BF16, ph); kit = Tok()
            QIs = sbuf("QIs", [128, 2, S], BF16, ph); qit = Tok()
            KBs = sbuf("KBs", [128, 2, S], BF16, ph); kbt = Tok()
            QBs = sbuf("QBs", [128, 2, S], BF16, ph); qbt = Tok()
            VB4 = sbuf("VB4", [128, NT, 4, 65], BF16, ph); vbt = Tok()
            WIs = sbuf("WIs", [128, NT, 4], F32, ph); wit = Tok()
            P2 = sbuf("P2", [128, NBIS + 1], F32, ph); p2t = Tok()
            k.dma("sp", KI2[0:64, :], KI, [dtok["KI"]], [kit])
            k.dma("sp", KI2[64:128, :], KI, [dtok["KI"]], [kit])
            k.dma("sp", QIs[:], QI.rearrange("(c p) t -> p c t", p=128), [dtok["QI"]], [qit])
            k.dma("sp", KBs[:], KB.rearrange("(c p) t -> p c t", p=128), [dtok["KB"]], [kbt])
            k.dma("sp", QBs[:], QB.rearrange("(c p) t -> p c t", p=128), [dtok["QB"]], [qbt])
            for h in range(4):
                k.dma("sp", VB4[:, :, h, 0:64],
                      VAB.rearrange("(t p) c -> p t c", p=128)[:, :, 256 + h * 64:256 + (h + 1) * 64],
                      [dtok["VAB"]], [vbt])
            k.memset("pool", VB4[:, :, :, 64:65], 1.0, [vbt])
            k.dma("sp", WIs[:], WI.rearrange("(t p) c -> p t c", p=128), [dtok["WI"]], [wit])
            k.dma("sp", P2[:], pow2, [], [p2t])
            scr = Ring([Slot(sbuf(f"sc{i}", [128, S], F32, ph)) for i in range(2)])
            rlr = Ring([Slot(sbuf(f"rl{i}", [128, 512], F32, ph)) for i in range(3)])
            mkr = Ring([Slot(sbuf(f"mk{i}", [128, S], BF16, ph)) for i in range(2)])
            mTall = Ring([Slot(sbuf(f"mTa{i}", [128, S], BF16, ph)) for i in range(2)])
            Er = Ring([Slot(sbuf(f"Eb{i}", [128, 512], BF16, ph)) for i in range(4)])
            Emr = Ring([Slot(sbuf(f"Emb{i}", [128, 512], BF16, ph)) for i in range(4)])
            junk = sbuf("junk", [128, S], BF16, ph); jt = Tok()
            smr = Ring([Slot(sbuf(f"sm{i}", [128, 8 + NBIS + 1], F32, ph)) for i in range(2)])
            rdr = Ring([Slot(sbuf(f"rdb{i}", [128, 4, 1], F32, ph)) for i in range(2)])
            ytr = Ring([Slot(sbuf(f"ytb{i}", [128, 256], BF16, ph)) for i in range(2)])
            yor = Ring([Slot(sbuf(f"yob{i}", [128, 256], BF16, ph)) for i in range(2)])
            dbk = Ring([banks[0], banks[1]])
            sbk = Ring([banks[2], banks[3], banks[4]])
            accb = Ring([banks[5]])
            pst_i = [0]

            def prep(t):
                nk = 128 * (t + 1)
                SC = scr.next()
                nblk = (nk + 511) // 512
                for kb_ in range(nblk):
                    w = min(512, nk - kb_ * 512)
                    cs_ = slice(kb_ * 512, kb_ * 512 + w)
                    for h in range(4):
                        ch, pb = h // 2, (h % 2) * 64
                        b = dbk.next()
                        k.mm(b.ap(0, 128, 0, w), QIs[pb:pb + 64, ch, t * 128:(t + 1) * 128], KI2[pb:pb + 64, cs_],
                             True, True, [qit, kit], [b.tok])
                        wsc = WIs[:, t, h:h + 1]
                        if h == 0:
                            k.ts("dve", SC.t[:, cs_], b.ap(0, 128, 0, w), 0.0, wsc, ALU.max, ALU.mult,
                                 [b.tok, wit], [SC.tok])
                        else:
                            RL = rlr.next()
                            k.act(RL.t[:, 0:w], b.ap(0, 128, 0, w), AF.Relu, [b.tok], [RL.tok])
                            k.act(RL.t[:, 0:w], RL.t[:, 0:w], AF.Copy, [RL.tok, wit], [RL.tok], scale=wsc)
                            k.tt("pool", SC.t[:, cs_], SC.t[:, cs_], RL.t[:, 0:w], ALU.add,
                                 [RL.tok, SC.tok], [SC.tok])
                SM = smr.next()
                mx, mn, rng, mid, cnt, dd, thr = (SM.t[:, i:i + 1] for i in range(7))
                steps = SM.t[:, 8:8 + NBIS + 1]
                bis = t >= 2 and not OPT.get('b2_nobis')
                if bis:
                    k.op("dve", lambda e: e.tensor_reduce(out=mx, in_=SC.t[:, 0:nk], axis=AX.X, op=ALU.max),
                         [SC.tok], [SM.tok])
                k.op("dve", lambda e: e.tensor_reduce(out=mn, in_=SC.t[:, 0:nk], axis=AX.X, op=ALU.min),
                     [SC.tok], [SM.tok])
                k.memset("dve", SC.t[0:64, nk - 64:nk], -1.0e30, [SC.tok])
                if bis:
                    k.tt("dve", rng, mx, mn, ALU.subtract, [SM.tok], [SM.tok])
                    k.ts("dve", steps, P2[:], rng, None, ALU.mult, None, [p2t, SM.tok], [SM.tok])
                    k.tt("dve", mid, mn, steps[:, 0:1], ALU.add, [SM.tok], [SM.tok])
                    for it in range(NBIS):
                        k.ts("dve", junk[:, 0:nk], SC.t[:, 0:nk], mid, 0.0, ALU.is_ge, ALU.add,
                             [SC.tok, SM.tok], [jt, SM.tok], accum_out=cnt)
                        k.ts("dve", dd, cnt, 255.5, 0.5, ALU.is_ge, ALU.subtract, [SM.tok], [SM.tok])
                        k.stt("dve", mid, dd, steps[:, it:it + 1], mid, ALU.mult, ALU.add, [SM.tok], [SM.tok])
                    k.tt("dve", thr, mid, steps[:, NBIS:NBIS + 1], ALU.subtract, [SM.tok], [SM.tok])
                else:
                    k.copy("dve", thr, mn, [SM.tok], [SM.tok])
                MK = mkr.next()
                k.ts("dve", MK.t[:, 0:nk], SC.t[:, 0:nk], thr, None, ALU.is_ge, None, [SC.tok, SM.tok], [MK.tok])
                return MK

            def prep_b(t, MK):
                ngrp = (t + 1 + 3) // 4
                MT = mTall.next()
                for g in range(ngrp):
                    jts = list(range(g * 4, min(t + 1, g * 4 + 4)))
                    half = pst_i[0] % 2
                    pst_i[0] += 1
                    for jj, jt_ in enumerate(jts):
                        k.tr(PSTs[half][:, jj * 128:(jj + 1) * 128],
                             MK.t[:, jt_ * 128:(jt_ + 1) * 128], IDENT, [MK.tok, cmt], [pstoks[half]])
                    n = len(jts) * 128
                    k.act(MT.t[:, g * 512:g * 512 + n], PSTs[half][:, 0:n], AF.Copy,
                          [pstoks[half]], [MT.tok])
                return MT

            def b_stage(t, h, g, MT):
                ch, pb = h // 2, (h % 2) * 64
                jts = list(range(g * 4, min(t + 1, g * 4 + 4)))
                n = len(jts) * 128
                b = sbk.next()
                for jj, jt_ in enumerate(jts):
                    k.mm(b.ap(0, 128, jj * 128, (jj + 1) * 128), KBs[pb:pb + 64, ch, jt_ * 128:(jt_ + 1) * 128],
                         QBs[pb:pb + 64, ch, t * 128:(t + 1) * 128], True, True, [kbt, qbt], [b.tok])
                E = Er.next(); Em = Emr.next()
                k.act(E.t[:, 0:n], b.ap(0, 128, 0, n), AF.Exp, [b.tok], [E.tok], scale=0.125)
                k.tt("pool", Em.t[:, 0:n], E.t[:, 0:n], MT.t[:, g * 512:g * 512 + n], ALU.mult,
                     [E.tok, MT.tok], [Em.tok])
                return Em, jts

            ntile = OPT.get('b2_tmax', NT)
            MTs = {0: prep_b(0, prep(0))}
            for t in range(ntile):
                MKn = prep(t + 1) if t + 1 < ntile else None
                if OPT.get('b2_noattn'):
                    continue
                MT = MTs.pop(t)
                ngrp = (t + 1 + 3) // 4
                items = [(h, g) for h in range(4) for g in range(ngrp)]
                pend = [b_stage(t, *it_, MT) for it_ in items[0:2]]
                acc = accb.next()
                for i, (h, g) in enumerate(items):
                    if i + 2 < len(items):
                        pend.append(b_stage(t, *items[i + 2], MT))
                    Em, jts = pend.pop(0)
                    for jj, jt_ in enumerate(jts):
                        k.mm(acc.ap(0, 128, h * 65, h * 65 + 65), Em.t[:, jj * 128:(jj + 1) * 128], VB4[:, jt_, h, :],
                             jt_ == 0, jt_ == t, [vbt, Em.tok], [acc.tok])
                if MKn is not None:
                    MTs[t + 1] = prep_b(t + 1, MKn)
                finalize_attn(t, acc, rdr, ytr, yor, 256, YMt[1], pst_i)
            k.flush()
            if stop == 'B2':
                raise _Stop(nc, k, es)

        with ExitStack() as ph:
            wg = sbuf("wg", [128, 8, 3072], BF16, ph); wgt = Tok()
            wb = sbuf("wb", [128, 8, 1024], BF16, ph); wbt = Tok()
            wo = sbuf("wo", [128, 8, 1024], BF16, ph); wot = Tok()
            for c in range(8):
                k.dma("pool", wg[:, c, :], w_in[l, c * 128:(c + 1) * 128, C_GT:C_GT + 3072], [], [wgt])
            k.dma("pool", wb[:], wview(w_br[l], 0, 1024), [], [wbt])
            k.dma("pool", wo[:], wview(w_out[l], 0, 1024), [], [wot])
            X = Slot(sbuf("xc", [128, 8, 512], F32, ph))
            sq = sbuf("sqc", [128, 8, 512], BF16, ph); sqt = Tok()
            sd = sbuf("sdc", [128, 512], F32, ph); sdt = Tok()
            rstd = sbuf("rstdc", [128, 512], F32, ph); rst = Tok()
            hTb = sbuf("hTb", [128, 8, 512], BF16, ph); hbt = Tok()
            ym = sbuf("ymc", [128, 8, 512], BF16, ph); ymt = Tok()
            mg = sbuf("mg", [128, 8, 512], BF16, ph); mgt = [Tok() for _ in range(8)]
            sgr = Ring([Slot(sbuf(f"sg{i}", [128, 512], F32, ph)) for i in range(3)])
            tmr = Ring([Slot(sbuf(f"tm{i}", [128, 512], F32, ph)) for i in range(3)])
            acr = Ring([Slot(sbuf(f"ac{i}", [128, 512], F32, ph)) for i in range(2)])
            xo = Ring([Slot(sbuf(f"xo{i}", [128, 512], F32, ph)) for i in range(3)])
            bk = Ring(banks)
            KR = [(0, 2), (2, 4), (4, 8)]
            for tb in range(NB):
                k.dma("sp", X.t[:], xview(xsrc, tb), [xsrct], [X.tok])
                k.dma("sp", ym[:], xview(YM, tb), YMt, [ymt])
                make_hT(X.t, X.tok, SP_GMIX, sq, sqt, sd, sdt, rstd, rst, lambda c: hTb[:, c, :], hbt, bk.next())
                for dc in range(8):
                    AC = acr.next()
                    for br in range(3):
                        bg = bk.next()
                        for c in range(8):
                            k.mm(bg.ap(), wg[:, c, br * 1024 + dc * 128:br * 1024 + (dc + 1) * 128], hTb[:, c, :],
                                 c == 0, c == 7, [wgt, hbt], [bg.tok])
                        SG = sgr.next()
                        bcol = SP_BG + br * 8 + dc
                        k.act(SG.t[:], bg.ap(), AF.Sigmoid, [bg.tok, sptok], [SG.tok], bias=spt[:, bcol:bcol + 1])
                        bb_ = bk.next()
                        k0, k1 = KR[br]
                        for c in range(k0, k1):
                            k.mm(bb_.ap(), wb[:, c, dc * 128:(dc + 1) * 128], ym[:, c, :], c == k0, c == k1 - 1,
                                 [wbt, ymt], [bb_.tok])
                        if br == 0:
                            k.tt("dve", AC.t[:], SG.t[:], bb_.ap(), ALU.mult, [SG.tok, bb_.tok], [AC.tok])
                        else:
                            TM = tmr.next()
                            k.tt("dve", TM.t[:], SG.t[:], bb_.ap(), ALU.mult, [SG.tok, bb_.tok], [TM.tok])
                            if br == 1:
                                k.tt("pool", AC.t[:], AC.t[:], TM.t[:], ALU.add, [AC.tok, TM.tok], [AC.tok])
                            else:
                                k.tt("pool", mg[:, dc, :], AC.t[:], TM.t[:], ALU.add, [AC.tok, TM.tok], [mgt[dc]])
                for oc in range(8):
                    bo = bk.next()
                    for c in range(8):
                        k.mm(bo.ap(), wo[:, c, oc * 128:(oc + 1) * 128], mg[:, c, :], c == 0, c == 7,
                             [wot, mgt[c]], [bo.tok])
                    XO = xo.next()
                    k.tt("dve", XO.t[:], X.t[:, oc, :], bo.ap(), ALU.add, [X.tok, bo.tok], [XO.tok])
                    k.dma("sp", XT[oc * 128:(oc + 1) * 128, tb * 512:(tb + 1) * 512], XO.t[:], [XO.tok], [dtok["XT"]])
            k.flush()
            if stop == 'C':
                raise _Stop(nc, k, es)

        with ExitStack() as ph:
            w1 = sbuf("w1", [128, 8, 2 * DFF], BF16, ph); w1t = Tok()
            w2 = sbuf("w2", [128, 22, 1024], BF16, ph); w2t = Tok()
            for c in range(8):
                k.dma("pool", w1[:, c, :], w_f1[l, c * 128:(c + 1) * 128, :], [], [w1t])
            for c in range(22):
                k.dma("pool", w2[:, c, :], w_f2[l, c * 128:(c + 1) * 128, :], [], [w2t])
            X = Slot(sbuf("xd", [128, 8, 512], F32, ph))
            sq = sbuf("sqd", [128, 8, 512], BF16, ph); sqt = Tok()
            sd = sbuf("sdd", [128, 512], F32, ph); sdt = Tok()
            rstd = sbuf("rstdd", [128, 512], F32, ph); rst = Tok()
            hTb = sbuf("hTd", [128, 8, 512], BF16, ph); hbt = Tok()
            av = sbuf("av", [128, 22, 512], BF16, ph); avt = [Tok() for _ in range(22)]
            sgr = Ring([Slot(sbuf(f"sl{i}", [128, 512], F32, ph)) for i in range(3)])
            xo = Ring([Slot(sbuf(f"xod{i}", [128, 512], F32, ph)) for i in range(3)])
            bk = Ring(banks)
            xdst, xdstt = (yT, dtok["yT"]) if last else (XT, dtok["XT"])
            for tb in range(NB):
                k.dma("sp", X.t[:], xview(XT, tb), [dtok["XT"]], [X.tok])
                make_hT(X.t, X.tok, SP_GFFN, sq, sqt, sd, sdt, rstd, rst, lambda c: hTb[:, c, :], hbt, bk.next())
                for fc in range(22):
                    bg = bk.next()
                    for c in range(8):
                        k.mm(bg.ap(), w1[:, c, fc * 128:(fc + 1) * 128], hTb[:, c, :], c == 0, c == 7,
                             [w1t, hbt], [bg.tok])
                    bu = bk.next()
                    for c in range(8):
                        k.mm(bu.ap(), w1[:, c, DFF + fc * 128:DFF + (fc + 1) * 128], hTb[:, c, :], c == 0, c == 7,
                             [w1t, hbt], [bu.tok])
                    SG = sgr.next()
                    k.act(SG.t[:], bg.ap(), AF.Silu, [bg.tok], [SG.tok])
                    k.tt("dve", av[:, fc, :], SG.t[:], bu.ap(), ALU.mult, [SG.tok, bu.tok], [avt[fc]])
                for oc in range(8):
                    bo = bk.next()
                    for c in range(22):
                        k.mm(bo.ap(), w2[:, c, oc * 128:(oc + 1) * 128], av[:, c, :], c == 0, c == 21,
                             [w2t, avt[c]], [bo.tok])
                    XO = xo.next()
                    k.tt("dve", XO.t[:], X.t[:, oc, :], bo.ap(), ALU.add, [X.tok, bo.tok], [XO.tok])
                    k.dma("sp", xdst[oc * 128:(oc + 1) * 128, tb * 512:(tb + 1) * 512], XO.t[:], [XO.tok], [xdstt])
            if last:
                k.wait_all_dma()
            k.flush()
            if stop == 'D':
                raise _Stop(nc, k, es)
    es.close()
    return nc, k


def host_consts():
    pos = np.arange(S, dtype=np.float32)
    inv = (1.0 / (np.float32(10000.0) ** (np.arange(0, 64, 2, dtype=np.float32) / np.float32(64)))).astype(np.float32)
    ang = pos[:, None] * inv[None, :]
    ang = np.concatenate([ang, ang], axis=-1)
    cosT = np.cos(ang).astype(np.float32).T
    sinT = np.sin(ang).astype(np.float32).T
    cs = np.zeros((128, 2, S), np.float32)
    cs[0:64, 0], cs[64:128, 0] = cosT, cosT
    cs[0:64, 1], cs[64:128, 1] = sinT, sinT
    ones = np.ones((128, 128), np.float32)
    onesblk = np.zeros((128, 128), np.float32)
    onesblk[0:64, 0:64] = 1.0
    onesblk[64:128, 64:128] = 1.0
    rotm = np.zeros((128, 128), np.float32)
    for hb in (0, 64):
        for m in range(32):
            rotm[hb + m + 32, hb + m] = -1.0
            rotm[hb + m, hb + m + 32] = 1.0
    ident = np.eye(128, dtype=np.float32)
    cmat = np.stack([ones, onesblk, rotm, ident]).astype(np.float32)
    kk = np.arange(128)[:, None, None]
    jj = np.arange(5)[None, :, None]
    qq = np.arange(128)[None, None, :]
    cq = (qq >= 64).astype(np.int64)
    ck = 2 * jj - 8 + (kk >= 64)
    amask = ((ck >= cq - 8) & (ck <= cq)).astype(np.float32)
    relidx = np.clip(128 * (4 - jj) + qq - kk, -128, 128) + 128
    pow2 = np.tile((2.0 ** -(np.arange(NBIS + 1) + 1.0)).astype(np.float32)[None, :], (128, 1))
    return cs, cmat, amask, relidx, pow2


def host_pack(inp):
    cs, cmat, amask, relidx, pow2 = host_consts()
    f = lambda a: np.ascontiguousarray(np.asarray(a, dtype=np.float32))
    spar = np.zeros((L, 128, NSP), np.float32)
    p = np.arange(128)
    for l in range(L):
        spar[l, :, SP_GMIX:SP_GMIX + 8] = f(inp["g_mix"])[l].reshape(8, 128).T
        spar[l, :, SP_GFFN:SP_GFFN + 8] = f(inp["g_ffn"])[l].reshape(8, 128).T
        spar[l, :, SP_BG:SP_BG + 24] = f(inp["b_gate"])[l].reshape(24, 128).T
        spar[l, :, SP_GAQ] = f(inp["qk_gain_a"])[l, 0][p % 64]
        spar[l, :, SP_GAK] = f(inp["qk_gain_a"])[l, 1][p % 64]
        spar[l, :, SP_GBQ] = f(inp["qk_gain_b"])[l, 0][p % 64]
        spar[l, :, SP_GBK] = f(inp["qk_gain_b"])[l, 1][p % 64]
        spar[l, :, SP_GIK] = f(inp["g_idx_k"])[l][p % 64]
        cw = f(inp["conv_w"])[l]
        for j in range(4):
            spar[l, :, SP_CW + j * 4:SP_CW + j * 4 + 4] = cw[j].reshape(4, 128).T
        spar[l, :, SP_CB:SP_CB + 4] = f(inp["conv_b"])[l].reshape(4, 128).T
        spar[l, :, SP_BA:SP_BA + 4] = f(inp["lru_ba"])[l].reshape(4, 128).T
        spar[l, :, SP_BX:SP_BX + 4] = f(inp["lru_bx"])[l].reshape(4, 128).T
        spar[l, :, SP_LAM:SP_LAM + 4] = f(inp["lru_lambda"])[l].reshape(4, 128).T
    lrubd = np.zeros((L, 2, 4, 128, 128), np.float32)
    for m, nm in enumerate(("lru_wa", "lru_wx")):
        wsrc = f(inp[nm])
        for cc in range(4):
            lrubd[:, m, cc, 0:64, 0:64] = wsrc[:, 2 * cc]
            lrubd[:, m, cc, 64:128, 64:128] = wsrc[:, 2 * cc + 1]
    rb = f(inp["rel_bias"])
    ab = rb[:, :, relidx]
    abias = np.ascontiguousarray(ab.transpose(0, 2, 1, 3, 4))
    shared = {"w_in": f(inp["w_in"]), "w_branch": f(inp["w_branch"]), "w_out": f(inp["w_out"]),
              "w_ffn_in": f(inp["w_ffn_in"]), "w_ffn_out": f(inp["w_ffn_out"]),
              "spar": spar, "lrubd": lrubd, "abias": abias, "cs": cs, "cmat": cmat,
              "amask": amask, "pow2": pow2}
    return shared


_CACHE = {}


def kernel(**inputs):
    x = np.asarray(inputs["x"], dtype=np.float32)
    shared = host_pack(inputs)
    if "nc" not in _CACHE:
        _CACHE["nc"] = build()[0]
    nc = _CACHE["nc"]
    in_maps = []
    for b in range(8):
        m = dict(shared)
        m["xT"] = np.ascontiguousarray(x[b].T)
        in_maps.append(m)
    res = run_bass_kernel_spmd(nc, in_maps, core_ids=list(range(8)))
    out = np.stack([np.ascontiguousarray(r["yT"].T) for r in res.results], axis=0)
    return out.astype(np.float32)
```

```python
import math
from contextlib import ExitStack
import numpy as np
import concourse.bass as bass
import concourse.mybir as mybir
from concourse.bass_utils import run_bass_kernel_spmd

F32 = mybir.dt.float32
BF16 = mybir.dt.bfloat16
AF = mybir.ActivationFunctionType
ALU = mybir.AluOpType
AX = mybir.AxisListType

D = 1024; S = 4096; L = 4; DIN = 5956; DFF = 2816
NT = S // 128; NB = S // 512
EPS = 1e-6
NBIS = 14
C_AQ, C_AK, C_AV, C_BQ, C_BK, C_BV, C_IQ, C_IK, C_IW, C_CX, C_CY, C_GT = (
    0, 256, 512, 768, 1024, 1280, 1536, 1792, 1856, 1860, 2372, 2884)
SP_GMIX, SP_GFFN, SP_BG, SP_GAQ, SP_GAK, SP_GBQ, SP_GBK, SP_GIK, SP_CW, SP_CB, SP_BA, SP_BX, SP_LAM = (
    0, 8, 16, 40, 41, 42, 43, 44, 45, 61, 65, 69, 73)
NSP = 77


class Tok:
    __slots__ = ("w", "r")

    def __init__(self):
        self.w = None
        self.r = {}


class Ring:
    def __init__(self, items):
        self.items = items
        self.i = -1

    def next(self):
        self.i = (self.i + 1) % len(self.items)
        return self.items[self.i]


class Slot:
    def __init__(self, t):
        self.t = t
        self.tok = Tok()


class K:
    INC = {"pe": 1, "act": 1, "dve": 1, "pool": 1, "dsp": 16, "dpool": 16, "dact": 16}

    def __init__(self, nc, es):
        self.nc = nc
        self.sem = {f"{n}@{l}": es.enter_context(nc.semaphore(f"s_{n}_{l}")) for n in self.INC for l in range(L)}
        self.cnt = {n: 0 for n in self.sem}
        self.ep = 0
        self.streams = {e: [] for e in ("pe", "act", "dve", "pool", "sp")}
        self.waited = {e: {} for e in self.streams}
        self.ninstr = 0

    def op(self, stream, fn, reads=(), writes=(), counter=None):
        counter = f"{counter or stream}@{self.ep}"
        deps = {}
        for t in reads:
            if t.w is not None and deps.get(t.w[0], 0) < t.w[1]:
                deps[t.w[0]] = t.w[1]
        for t in writes:
            if t.w is not None and deps.get(t.w[0], 0) < t.w[1]:
                deps[t.w[0]] = t.w[1]
            for c, s in t.r.items():
                if deps.get(c, 0) < s:
                    deps[c] = s
        wd = self.waited[stream]
        waits = []
        for c, s in deps.items():
            if c[:3] == "pe@" and counter[:3] == "pe@":
                continue
            if wd.get(c, 0) >= s:
                continue
            wd[c] = s
            waits.append((c, s))
        self.cnt[counter] += 1
        seq = self.cnt[counter]
        for t in reads:
            if t.r.get(counter, 0) < seq:
                t.r[counter] = seq
        for t in writes:
            t.w = (counter, seq)
            t.r = {}
        self.streams[stream].append((waits, fn, counter))
        self.ninstr += 1

    def wait_all_dma(self):
        waits = [(c, self.cnt[c]) for c in self.cnt if c[0] == "d" and c[:3] != "dve" and self.cnt[c] > 0]
        self.streams["sp"].append((waits, None, None))

    def flush(self):
        nc = self.nc
        streams = self.streams
        self.streams = {e: [] for e in streams}
        sem, INC = self.sem, self.INC

        def mk(lst):
            def f(eng):
                for waits, fn, counter in lst:
                    for c, s in waits:
                        eng.wait_ge(sem[c], s * INC[c.split("@")[0]])
                    if fn is not None:
                        fn(eng).then_inc(sem[counter], INC[counter.split("@")[0]])
            return f

        with nc.Block() as block:
            block.tensor(mk(streams["pe"]))
            block.scalar(mk(streams["act"]))
            block.vector(mk(streams["dve"]))
            block.gpsimd(mk(streams["pool"]))
            block.sync(mk(streams["sp"]))

    def dma(self, q, out, in_, reads, writes):
        self.op(q, lambda e: e.dma_start(out=out, in_=in_), reads, writes, counter="d" + q)

    def mm(self, out, lhsT, rhs, start, stop, reads, writes):
        self.op("pe", lambda e: e.matmul(out, lhsT, rhs, start=start, stop=stop), reads, writes)

    def tr(self, out, in_, ident, reads, writes):
        self.op("pe", lambda e: e.transpose(out, in_, ident), reads, writes)

    def act(self, out, in_, func, reads, writes, bias=None, scale=None):
        kw = {}
        if bias is not None:
            kw["bias"] = bias
        if scale is not None:
            kw["scale"] = scale
        self.op("act", lambda e: e.activation(out=out, in_=in_, func=func, **kw), reads, writes)

    def tt(self, eng, out, in0, in1, op, reads, writes):
        self.op(eng, lambda e: e.tensor_tensor(out=out, in0=in0, in1=in1, op=op), reads, writes)

    def ts(self, eng, out, in0, s1, s2, op0, op1, reads, writes, accum_out=None):
        if op1 is None:
            self.op(eng, lambda e: e.tensor_scalar(out=out, in0=in0, scalar1=s1, scalar2=None, op0=op0),
                    reads, writes)
        elif accum_out is None:
            self.op(eng, lambda e: e.tensor_scalar(out=out, in0=in0, scalar1=s1, scalar2=s2, op0=op0, op1=op1),
                    reads, writes)
        else:
            self.op(eng, lambda e: e.tensor_scalar(out=out, in0=in0, scalar1=s1, scalar2=s2, op0=op0, op1=op1,
                                                   accum_out=accum_out), reads, writes)

    def stt(self, eng, out, in0, scalar, in1, op0, op1, reads, writes):
        self.op(eng, lambda e: e.scalar_tensor_tensor(out=out, in0=in0, scalar=scalar, in1=in1, op0=op0, op1=op1),
                reads, writes)

    def recip(self, out, in_, reads, writes):
        self.op("dve", lambda e: e.reciprocal(out=out, in_=in_), reads, writes)

    def memset(self, eng, ap, val, writes):
        self.op(eng, lambda e: e.memset(ap, val), (), writes)

    def copy(self, eng, out, in_, reads, writes):
        self.op(eng, lambda e: e.tensor_copy(out=out, in_=in_), reads, writes)


OPT = {}


class _Stop(Exception):
    pass


def build(nlayers=L, dbg=False, stop=None):
    try:
        return _build(nlayers, dbg, stop)
    except _Stop as e:
        nc, k, es = e.args
        k.wait_all_dma()
        k.flush()
        return nc, k


def _build(nlayers, dbg, stop):
    nc = bass.Bass("TRN2", target_bir_lowering=False)
    es = ExitStack()

    def din(name, shape, dt=F32):
        return nc.dram_tensor(name, list(shape), dt, kind="ExternalInput").ap()

    kind_dbg = "ExternalOutput" if dbg else "Internal"

    def dscr(name, shape, dt):
        return nc.dram_tensor(name, list(shape), dt, kind=kind_dbg).ap()

    xT_in = din("xT", [D, S])
    w_in = din("w_in", [L, D, DIN])
    w_br = din("w_branch", [L, D, D])
    w_out = din("w_out", [L, D, D])
    w_f1 = din("w_ffn_in", [L, D, 2 * DFF])
    w_f2 = din("w_ffn_out", [L, DFF, D])
    spar = din("spar", [L, 128, NSP])
    lrubd = din("lrubd", [L, 2, 4, 128, 128])
    abias = din("abias", [L, 128, 4, 5, 128])
    cs_d = din("cs", [128, 2, S])
    cmat = din("cmat", [4, 128, 128])
    amask = din("amask", [128, 5, 128])
    pow2 = din("pow2", [128, NBIS + 1])
    yT = nc.dram_tensor("yT", [D, S], F32, kind="ExternalOutput").ap()

    XT = dscr("XT", [D, S], F32)
    QA = dscr("QA", [256, S], BF16)
    KA = dscr("KA", [256, S], BF16)
    QB = dscr("QB", [256, S], BF16)
    KB = dscr("KB", [256, S], BF16)
    QI = dscr("QI", [256, S], BF16)
    KI = dscr("KI", [64, S], BF16)
    VAB = dscr("VAB", [S, 512], BF16)
    WI = dscr("WI", [S, 4], F32)
    YM = dscr("YM", [D, S], BF16)
    dtok = {n: Tok() for n in ("XT", "QA", "KA", "QB", "KB", "QI", "KI", "VAB", "WI", "YM", "xin", "yT")}
    YMt = [Tok() for _ in range(3)]

    k = K(nc, es)

    PS2 = [es.enter_context(nc.psum_tensor(f"ps2_{i}", [128, 1024], F32)) for i in range(3)]
    PSTs = [es.enter_context(nc.psum_tensor(f"pst{i}", [128, 1024], BF16)) for i in range(2)]

    class Bank:
        def __init__(self, t, c0):
            self.t, self.c0, self.tok = t, c0, Tok()

        def ap(self, p0=0, p1=128, a=0, b=512):
            return self.t[p0:p1, self.c0 + a:self.c0 + b]

    banks = []
    for t in PS2:
        banks.append(Bank(t, 0))
        banks.append(Bank(t, 512))
    pstoks = [Tok(), Tok()]

    uid = [0]

    def sbuf(name, shape, dt, stack=es):
        uid[0] += 1
        return stack.enter_context(nc.sbuf_tensor(f"{name}_{uid[0]}", list(shape), dt))

    cm = sbuf("cm", [128, 4, 128], BF16)
    cmt = Tok()
    epsc = sbuf("epsc", [128, 1], F32)
    epst = Tok()
    spt = sbuf("spt", [128, NSP], F32)
    sptok = Tok()
    clam = sbuf("clam", [128, 4], F32)
    clamtok = Tok()
    k.dma("pool", cm[:], cmat.rearrange("m p n -> p m n"), [], [cmt])
    k.memset("dve", epsc[:], EPS, [epst])
    ONES, ONESBLK, ROTM, IDENT = (cm[:, i, :] for i in range(4))

    def xview(ap2d, tb):
        return ap2d.rearrange("(c p) t -> p c t", p=128)[:, :, tb * 512:(tb + 1) * 512]

    def wview(w2d, c0, n):
        return w2d.rearrange("(c p) n -> p c n", p=128)[:, :, c0:c0 + n]

    def make_hT(X, Xtok, gcol, sq, sqtok, sd, sdtok, rstd, rstok, hdst, htok, bank):
        k.act(sq[:], X[:], AF.Square, [Xtok], [sqtok])
        for c in range(8):
            k.mm(bank.ap(), ONES, sq[:, c, :], c == 0, c == 7, [sqtok, cmt], [bank.tok])
        k.act(sd[:], bank.ap(), AF.Sqrt, [bank.tok, epst], [sdtok], bias=epsc[:, 0:1], scale=1.0 / D)
        k.recip(rstd[:], sd[:], [sdtok], [rstok])
        for c in range(8):
            k.stt("dve", hdst(c), X[:, c, :], spt[:, gcol + c:gcol + c + 1], rstd[:],
                  ALU.mult, ALU.mult, [Xtok, sptok, rstok], [htok])

    for l in range(nlayers):
        xsrc, xsrct = (xT_in, dtok["xin"]) if l == 0 else (XT, dtok["XT"])
        last = l == nlayers - 1
        k.ep = l
        k.dma("sp", spt[:], spar[l], [], [sptok])

        with ExitStack() as ph:
            hT = sbuf("hT", [128, 8, S], BF16, ph); hTt = Tok()
            with ExitStack() as ph1:
                xs = Ring([Slot(sbuf(f"xs{i}", [128, 8, 512], F32, ph1)) for i in range(2)])
                sq = sbuf("sq", [128, 8, 512], BF16, ph1); sqt = Tok()
                sd = sbuf("sd", [128, 512], F32, ph1); sdt = Tok()
                rstd = sbuf("rstd", [128, 512], F32, ph1); rst = Tok()
                for tb in range(NB):
                    X = xs.next()
                    k.dma("sp", X.t[:], xview(xsrc, tb), [xsrct], [X.tok])
                    make_hT(X.t, X.tok, SP_GMIX, sq, sqt, sd, sdt, rstd, rst,
                            lambda c, tb=tb: hT[:, c, tb * 512:(tb + 1) * 512], hTt, banks[tb % 6])
                k.flush()
                if stop == 'A1':
                    raise _Stop(nc, k, es)

            wr = Ring([Slot(sbuf(f"wr{i}", [128, 8, 128], BF16, ph)) for i in range(3)])
            ob = Ring([Slot(sbuf(f"ob{i}", [128, S], BF16, ph)) for i in range(2)])
            csr = Ring([Slot(sbuf(f"csr{i}", [128, 2, 512], F32, ph)) for i in range(3)])
            sqb = Ring([Slot(sbuf(f"sqb{i}", [128, 512], BF16, ph)) for i in range(3)])
            sd2 = Ring([Slot(sbuf(f"sd2{i}", [128, 512], F32, ph)) for i in range(3)])
            rs2 = Ring([Slot(sbuf(f"rs2{i}", [128, 512], F32, ph)) for i in range(3)])
            qnb = Ring([Slot(sbuf(f"qnb{i}", [128, 512], BF16, ph)) for i in range(3)])
            t1r = Ring([Slot(sbuf(f"t1r{i}", [128, 512], F32, ph)) for i in range(3)])
            t2r = Ring([Slot(sbuf(f"t2r{i}", [128, 512], F32, ph)) for i in range(3)])
            bk = Ring(banks)

            def load_w(c0, m):
                W = wr.next()
                k.dma("pool", W.t[:, :, 0:m], wview(w_in[l], c0, m), [], [W.tok])
                return W

            def proj_mm(W, m, tb):
                b = bk.next()
                for c in range(8):
                    k.mm(b.ap(0, m), W.t[:, c, 0:m], hT[:, c, tb * 512:(tb + 1) * 512], c == 0, c == 7,
                         [W.tok, hTt], [b.tok])
                return b

            chunks = []
            for ch in range(2):
                chunks.append((C_AQ + ch * 128, 128, QA, dtok["QA"], ch * 128, SP_GAQ, True, False))
                chunks.append((C_AK + ch * 128, 128, KA, dtok["KA"], ch * 128, SP_GAK, True, False))
                chunks.append((C_BQ + ch * 128, 128, QB, dtok["QB"], ch * 128, SP_GBQ, True, True))
                chunks.append((C_BK + ch * 128, 128, KB, dtok["KB"], ch * 128, SP_GBK, True, True))
                chunks.append((C_IQ + ch * 128, 128, QI, dtok["QI"], ch * 128, 0, False, True))
            chunks.append((C_IK, 64, KI, dtok["KI"], 0, SP_GIK, True, True))
            items = [(ci, tb) for ci in range(len(chunks)) for tb in range(NB)]
            ist, cst = {}, {}

            def rope_start(st, m, tb):
                QN = st["QN"]
                CS = csr.next()
                k.dma("sp", CS.t[:], cs_d[:, :, tb * 512:(tb + 1) * 512], [], [CS.tok])
                b3 = bk.next()
                k.mm(b3.ap(0, m), cm[0:m, 2, 0:m], QN.t[0:m, :], True, True, [QN.tok, cmt], [b3.tok])
                st["CS"], st["b3"] = CS, b3

            def S1(i):
                ci, tb = items[i]
                c0, m = chunks[ci][0:2]
                if tb == 0:
                    cst[ci] = (load_w(c0, m), ob.next())
                ist[i] = {"b": proj_mm(cst[ci][0], m, tb)}

            def S2(i):
                ci, tb = items[i]
                c0, m, dst, dstt, row0, gcol, norm, rope = chunks[ci]
                st = ist[i]
                b = st["b"]
                if norm:
                    SQ = sqb.next()
                    k.act(SQ.t[0:m, :], b.ap(0, m), AF.Square, [b.tok], [SQ.tok])
                    b2 = bk.next()
                    k.mm(b2.ap(0, m), cm[0:m, 1, 0:m], SQ.t[0:m, :], True, True, [SQ.tok, cmt], [b2.tok])
                    st["b2"] = b2
                else:
                    QN = qnb.next()
                    k.act(QN.t[0:m, :], b.ap(0, m), AF.Copy, [b.tok], [QN.tok])
                    st["QN"] = QN
                    rope_start(st, m, tb)

            def S3(i):
                ci, tb = items[i]
                c0, m, dst, dstt, row0, gcol, norm, rope = chunks[ci]
                st = ist[i]
                if not norm:
                    return
                b, b2 = st["b"], st["b2"]
                O = cst[ci][1]
                SD = sd2.next(); RS = rs2.next()
                k.act(SD.t[0:m, :], b2.ap(0, m), AF.Sqrt, [b2.tok, epst], [SD.tok],
                      bias=epsc[0:m, 0:1], scale=1.0 / 64)
                k.recip(RS.t[0:m, :], SD.t[0:m, :], [SD.tok], [RS.tok])
                if not rope:
                    k.stt("dve", O.t[0:m, tb * 512:(tb + 1) * 512], b.ap(0, m), spt[0:m, gcol:gcol + 1], RS.t[0:m, :],
                          ALU.mult, ALU.mult, [b.tok, sptok, RS.tok], [O.tok])
                    return
                QN = qnb.next()
                k.stt("dve", QN.t[0:m, :], b.ap(0, m), spt[0:m, gcol:gcol + 1], RS.t[0:m, :],
                      ALU.mult, ALU.mult, [b.tok, sptok, RS.tok], [QN.tok])
                st["QN"] = QN
                rope_start(st, m, tb)

            def S4(i):
                ci, tb = items[i]
                c0, m, dst, dstt, row0, gcol, norm, rope = chunks[ci]
                st = ist.pop(i)
                O = cst[ci][1]
                if rope:
                    QN, CS, b3 = st["QN"], st["CS"], st["b3"]
                    T1 = t1r.next(); T2 = t2r.next()
                    k.tt("dve", T1.t[0:m, :], QN.t[0:m, :], CS.t[0:m, 0, :], ALU.mult, [QN.tok, CS.tok], [T1.tok])
                    k.tt("dve", T2.t[0:m, :], b3.ap(0, m), CS.t[0:m, 1, :], ALU.mult, [b3.tok, CS.tok], [T2.tok])
                    k.tt("pool", O.t[0:m, tb * 512:(tb + 1) * 512], T1.t[0:m, :], T2.t[0:m, :], ALU.add,
                         [T1.tok, T2.tok], [O.tok])
                if tb == NB - 1:
                    k.dma("sp", dst[row0:row0 + m, :], O.t[0:m, :], [O.tok], [dstt])

            for s_ in range(len(items) + 3):
                for stage, off in ((S4, 3), (S3, 2), (S2, 1), (S1, 0)):
                    i = s_ - off
                    if 0 <= i < len(items):
                        stage(i)

            wv = sbuf("wv", [128, 8, 512], BF16, ph); wvt = Tok()
            wiw = sbuf("wiw", [128, 8, 4], BF16, ph); wiwt = Tok()
            k.dma("pool", wv[:, :, 0:256], wview(w_in[l], C_AV, 256), [], [wvt])
            k.dma("pool", wv[:, :, 256:512], wview(w_in[l], C_BV, 256), [], [wvt])
            k.dma("pool", wiw[:], wview(w_in[l], C_IW, 4), [], [wiwt])
            vb = Ring([Slot(sbuf(f"vb{i}", [128, 512], BF16, ph)) for i in range(2)])
            wib = sbuf("wib", [128, NT, 4], F32, ph); wibt = Tok()
            for tt_ in range(NT):
                b = bk.next()
                for c in range(8):
                    k.mm(b.ap(), hT[:, c, tt_ * 128:(tt_ + 1) * 128], wv[:, c, :], c == 0, c == 7,
                         [hTt, wvt], [b.tok])
                V = vb.next()
                k.act(V.t[:], b.ap(), AF.Copy, [b.tok], [V.tok])
                k.dma("sp", VAB[tt_ * 128:(tt_ + 1) * 128, :], V.t[:], [V.tok], [dtok["VAB"]])
                b = bk.next()
                for c in range(8):
                    k.mm(b.ap(0, 128, 0, 4), hT[:, c, tt_ * 128:(tt_ + 1) * 128], wiw[:, c, :], c == 0, c == 7,
                         [hTt, wiwt], [b.tok])
                k.ts("dve", wib[:, tt_, :], b.ap(0, 128, 0, 4), 0.0625, None, ALU.mult, None, [b.tok], [wibt])
            k.dma("sp", WI.rearrange("(t p) c -> p t c", p=128), wib[:], [wibt], [dtok["WI"]])

            k.act(clam[:], spt[:, SP_LAM:SP_LAM + 4], AF.Exp, [sptok], [clamtok], scale=-1.0)
            k.act(clam[:], clam[:], AF.Ln, [clamtok], [clamtok], bias=1.0)
            k.ts("dve", clam[:], clam[:], -8.0, None, ALU.mult, None, [clamtok], [clamtok])
            cxb = sbuf("cxb", [128, 3 + S], F32, ph)
            cxt = [Tok() for _ in range(NB + 1)]
            k.memset("dve", cxb[:, 0:3], 0.0, [cxt[NB]])
            wbd = Ring([Slot(sbuf(f"wbd{i}", [128, 2, 128], BF16, ph)) for i in range(2)])
            f32r = {n: Ring([Slot(sbuf(f"c{n}{i}", [128, 512], F32, ph)) for i in range(2)])
                    for n in ("gy", "u", "r", "i", "a", "m", "bb", "hs")}
            ubr = Ring([Slot(sbuf(f"ub{i}", [128, 512], BF16, ph)) for i in range(2)])
            for cc in range(4):
                WX = load_w(C_CX + cc * 128, 128)
                WY = load_w(C_CY + cc * 128, 128)
                BD = wbd.next()
                k.dma("pool", BD.t[:], lrubd[l, :, cc].rearrange("m p n -> p m n"), [], [BD.tok])
                O = ob.next()
                prev_hs = None
                for tb in range(NB):
                    b = proj_mm(WX, 128, tb)
                    k.act(cxb[:, 3 + tb * 512:3 + (tb + 1) * 512], b.ap(), AF.Copy, [b.tok], [cxt[tb]])
                    b = proj_mm(WY, 128, tb)
                    GY = f32r["gy"].next()
                    k.act(GY.t[:], b.ap(), AF.Gelu, [b.tok], [GY.tok])
                    U = f32r["u"].next()
                    rd = [cxt[tb], cxt[tb - 1] if tb > 0 else cxt[NB], sptok]
                    cw = lambda j: spt[:, SP_CW + j * 4 + cc:SP_CW + j * 4 + cc + 1]
                    k.ts("dve", U.t[:], cxb[:, tb * 512:tb * 512 + 512], cw(0),
                         spt[:, SP_CB + cc:SP_CB + cc + 1], ALU.mult, ALU.add, rd, [U.tok])
                    for j in range(1, 4):
                        k.stt("dve", U.t[:], cxb[:, tb * 512 + j:tb * 512 + j + 512], cw(j), U.t[:],
                              ALU.mult, ALU.add, rd + [U.tok], [U.tok])
                    UB = ubr.next()
                    k.act(UB.t[:], U.t[:], AF.Copy, [U.tok], [UB.tok])
                    bR = bk.next()
                    k.mm(bR.ap(), BD.t[:, 0, :], UB.t[:], True, True, [BD.tok, UB.tok], [bR.tok])
                    bI = bk.next()
                    k.mm(bI.ap(), BD.t[:, 1, :], UB.t[:], True, True, [BD.tok, UB.tok], [bI.tok])
                    R = f32r["r"].next(); I_ = f32r["i"].next(); A = f32r["a"].next(); M = f32r["m"].next()
                    k.act(R.t[:], bR.ap(), AF.Sigmoid, [bR.tok, sptok], [R.tok], bias=spt[:, SP_BA + cc:SP_BA + cc + 1])
                    k.act(I_.t[:], bI.ap(), AF.Sigmoid, [bI.tok, sptok], [I_.tok], bias=spt[:, SP_BX + cc:SP_BX + cc + 1])
                    k.act(A.t[:], R.t[:], AF.Exp, [R.tok, clamtok], [A.tok], scale=clam[:, cc:cc + 1])
                    k.act(M.t[:], A.t[:], AF.Square, [A.tok], [M.tok])
                    k.act(M.t[:], M.t[:], AF.Sqrt, [M.tok], [M.tok], bias=1.0, scale=-1.0)
                    BB = f32r["bb"].next()
                    k.tt("pool", BB.t[:], I_.t[:], U.t[:], ALU.mult, [I_.tok, U.tok], [BB.tok])
                    k.tt("pool", BB.t[:], BB.t[:], M.t[:], ALU.mult, [BB.tok, M.tok], [BB.tok])
                    HS = f32r["hs"].next()
                    if prev_hs is None:
                        k.op("dve", lambda e, HS=HS, A=A, BB=BB: e.tensor_tensor_scan(
                            out=HS.t[:], data0=A.t[:], data1=BB.t[:], initial=0.0, op0=ALU.mult, op1=ALU.add),
                            [A.tok, BB.tok], [HS.tok])
                    else:
                        k.op("dve", lambda e, HS=HS, A=A, BB=BB, PH=prev_hs: e.tensor_tensor_scan(
                            out=HS.t[:], data0=A.t[:], data1=BB.t[:], initial=PH.t[:, 511:512],
                            op0=ALU.mult, op1=ALU.add), [A.tok, BB.tok, prev_hs.tok], [HS.tok])
                    prev_hs = HS
                    k.tt("pool", O.t[:, tb * 512:(tb + 1) * 512], HS.t[:], GY.t[:], ALU.mult,
                         [HS.tok, GY.tok], [O.tok])
                k.dma("sp", YM[512 + cc * 128:512 + (cc + 1) * 128, :], O.t[:], [O.tok], [YMt[2]])
            k.flush()
            if stop == 'A':
                raise _Stop(nc, k, es)

        def finalize_attn(t, acc, rdr, ytr, yor, row0, ymtok, pst_i):
            RD = rdr.next()
            a3 = acc.t[:, acc.c0:acc.c0 + 260].rearrange("p (h e) -> p h e", e=65)
            k.recip(RD.t[:], a3[:, :, 64:65], [acc.tok], [RD.tok])
            YT = ytr.next()
            for h in range(4):
                k.ts("dve", YT.t[:, h * 64:(h + 1) * 64], acc.ap(0, 128, h * 65, h * 65 + 64), RD.t[:, h, :], None,
                     ALU.mult, None, [acc.tok, RD.tok], [YT.tok])
            half = pst_i[0] % 2
            pst_i[0] += 1
            for c in range(2):
                k.tr(PSTs[half][:, c * 128:(c + 1) * 128], YT.t[:, c * 128:(c + 1) * 128], IDENT,
                     [YT.tok, cmt], [pstoks[half]])
            YO = yor.next()
            k.act(YO.t[:], PSTs[half][:, 0:256], AF.Copy, [pstoks[half]], [YO.tok])
            k.dma("sp", YM[row0:row0 + 256, t * 128:(t + 1) * 128].rearrange("(c p) q -> p c q", p=128),
                  YO.t[:].rearrange("p (c q) -> p c q", c=2), [YO.tok], [ymtok])

        with ExitStack() as ph:
            KAs = sbuf("KAs", [128, 2, S], BF16, ph); kat = Tok()
            QAs = sbuf("QAs", [128, 2, S], BF16, ph); qat = Tok()
            VA4 = sbuf("VA4", [128, NT, 4, 65], BF16, ph); vat = Tok()
            EB = sbuf("EB", [128, 4, 5, 128], F32, ph); ebt = Tok()
            AM = sbuf("AM", [128, 5, 128], F32, ph); amt = Tok()
            k.dma("sp", KAs[:], KA.rearrange("(c p) t -> p c t", p=128), [dtok["KA"]], [kat])
            k.dma("sp", QAs[:], QA.rearrange("(c p) t -> p c t", p=128), [dtok["QA"]], [qat])
            for h in range(4):
                k.dma("sp", VA4[:, :, h, 0:64], VAB.rearrange("(t p) c -> p t c", p=128)[:, :, h * 64:(h + 1) * 64],
                      [dtok["VAB"]], [vat])
            k.memset("pool", VA4[:, :, :, 64:65], 1.0, [vat])
            k.dma("sp", EB[:], abias[l], [], [ebt])
            k.dma("sp", AM[:], amask, [], [amt])
            k.act(EB[:], EB[:], AF.Exp, [ebt], [ebt])
            for h in range(4):
                k.tt("dve", EB[:, h], EB[:, h], AM[:], ALU.mult, [ebt, amt], [ebt])
            Er = Ring([Slot(sbuf(f"Ea{i}", [128, 640], F32, ph)) for i in range(2)])
            Emr = Ring([Slot(sbuf(f"Ema{i}", [128, 640], BF16, ph)) for i in range(3)])
            rdr = Ring([Slot(sbuf(f"rda{i}", [128, 4, 1], F32, ph)) for i in range(2)])
            ytr = Ring([Slot(sbuf(f"yta{i}", [128, 256], BF16, ph)) for i in range(2)])
            yor = Ring([Slot(sbuf(f"yoa{i}", [128, 256], BF16, ph)) for i in range(2)])
            stb = Ring([(PS2[0], banks[0], banks[1]), (PS2[1], banks[2], banks[3])])
            accb = Ring([banks[4], banks[5]])
            pst_i = [0]

            def a_stage(t, h):
                ch, pb = h // 2, (h % 2) * 64
                js = [j for j in range(5) if t - 4 + j >= 0]
                j0 = js[0]
                PT_, ba, bb_ = stb.next()
                for j in js:
                    k.mm(PT_[:, j * 128:(j + 1) * 128], KAs[pb:pb + 64, ch, (t - 4 + j) * 128:(t - 3 + j) * 128],
                         QAs[pb:pb + 64, ch, t * 128:(t + 1) * 128], True, True,
                         [kat, qat], [ba.tok if j < 4 else bb_.tok])
                E = Er.next(); Em = Emr.next()
                k.act(E.t[:, j0 * 128:640], PT_[:, j0 * 128:640], AF.Exp, [ba.tok, bb_.tok], [E.tok], scale=0.125)
                k.tt("dve", Em.t[:, j0 * 128:640], E.t[:, j0 * 128:640],
                     EB[:, h, j0:5, :].rearrange("p j q -> p (j q)"), ALU.mult, [E.tok, ebt], [Em.tok])
                return Em, js

            items = [(t, h) for t in range(NT) for h in range(4)]
            pend = a_stage(*items[0])
            acc = None
            for i, (t, h) in enumerate(items):
                Em, js = pend
                if i + 1 < len(items):
                    pend = a_stage(*items[i + 1])
                if h == 0:
                    acc = accb.next()
                for j in js:
                    k.mm(acc.ap(0, 128, h * 65, h * 65 + 65), Em.t[:, j * 128:(j + 1) * 128], VA4[:, t - 4 + j, h, :],
                         j == js[0], j == 4, [vat, Em.tok], [acc.tok])
                if h == 3:
                    finalize_attn(t, acc, rdr, ytr, yor, 0, YMt[0], pst_i)
            k.flush()
            if stop == 'B1':
                raise _Stop(nc, k, es)

        with ExitStack() as ph:
            KI2 = sbuf("KI2", [128, S], BF16, ph); kit = Tok()
            QIs = sbuf("QIs", [128, 2, S], BF16, ph); qit = Tok()
            KBs = sbuf("KBs", [128, 2, S], BF16, ph); kbt = Tok()
            QBs = sbuf("QBs", [128, 2, S], BF16, ph); qbt = Tok()
            VB4 = sbuf("VB4", [128, NT, 4, 65], BF16, ph); vbt = Tok()
            WIs = sbuf("WIs", [128, NT, 4], F32, ph); wit = Tok()
            P2 = sbuf("P2", [128, NBIS + 1], F32, ph); p2t = Tok()
            k.dma("sp", KI2[0:64, :], KI, [dtok["KI"]], [kit])
            k.dma("sp", KI2[64:128, :], KI, [dtok["KI"]], [kit])
            k.dma("sp", QIs[:], QI.rearrange("(c p) t -> p c t", p=128), [dtok["QI"]], [qit])
            k.dma("sp", KBs[:], KB.rearrange("(c p) t -> p c t", p=128), [dtok["KB"]], [kbt])
            k.dma("sp", QBs[:], QB.rearrange("(c p) t -> p c t", p=128), [dtok["QB"]], [qbt])
            for h in range(4):
                k.dma("sp", VB4[:, :, h, 0:64],
                      VAB.rearrange("(t p) c -> p t c", p=128)[:, :, 256 + h * 64:256 + (h + 1) * 64],
                      [dtok["VAB"]], [vbt])
            k.memset("pool", VB4[:, :, :, 64:65], 1.0, [vbt])
            k.dma("sp", WIs[:], WI.rearrange("(t p) c -> p t c", p=128), [dtok["WI"]], [wit])
            k.dma("sp", P2[:], pow2, [], [p2t])
            scr = Ring([Slot(sbuf(f"sc{i}", [128, S], F32, ph)) for i in range(2)])
            rlr = Ring([Slot(sbuf(f"rl{i}", [128, 512], F32, ph)) for i in range(3)])
            mkr = Ring([Slot(sbuf(f"mk{i}", [128, S], BF16, ph)) for i in range(2)])
            mTall = Ring([Slot(sbuf(f"mTa{i}", [128, S], BF16, ph)) for i in range(2)])
            Er = Ring([Slot(sbuf(f"Eb{i}", [128, 512], BF16, ph)) for i in range(4)])
            Emr = Ring([Slot(sbuf(f"Emb{i}", [128, 512], BF16, ph)) for i in range(4)])
            junk = sbuf("junk", [128, S], BF16, ph); jt = Tok()
            smr = Ring([Slot(sbuf(f"sm{i}", [128, 8 + NBIS + 1], F32, ph)) for i in range(2)])
            rdr = Ring([Slot(sbuf(f"rdb{i}", [128, 4, 1], F32, ph)) for i in range(2)])
            ytr = Ring([Slot(sbuf(f"ytb{i}", [128, 256], BF16, ph)) for i in range(2)])
            yor = Ring([Slot(sbuf(f"yob{i}", [128, 256], BF16, ph)) for i in range(2)])
            dbk = Ring([banks[0], banks[1]])
            sbk = Ring([banks[2], banks[3], banks[4]])
            accb = Ring([banks[5]])
            pst_i = [0]

            def prep(t):
                nk = 128 * (t + 1)
                SC = scr.next()
                nblk = (nk + 511) // 512
                for kb_ in range(nblk):
                    w = min(512, nk - kb_ * 512)
                    cs_ = slice(kb_ * 512, kb_ * 512 + w)
                    for h in range(4):
                        ch, pb = h // 2, (h % 2) * 64
                        b = dbk.next()
                        k.mm(b.ap(0, 128, 0, w), QIs[pb:pb + 64, ch, t * 128:(t + 1) * 128], KI2[pb:pb + 64, cs_],
                             True, True, [qit, kit], [b.tok])
                        wsc = WIs[:, t, h:h + 1]
                        if h == 0:
                            k.ts("dve", SC.t[:, cs_], b.ap(0, 128, 0, w), 0.0, wsc, ALU.max, ALU.mult,
                                 [b.tok, wit], [SC.tok])
                        else:
                            RL = rlr.next()
                            k.act(RL.t[:, 0:w], b.ap(0, 128, 0, w), AF.Relu, [b.tok], [RL.tok])
                            k.act(RL.t[:, 0:w], RL.t[:, 0:w], AF.Copy, [RL.tok, wit], [RL.tok], scale=wsc)
                            k.tt("pool", SC.t[:, cs_], SC.t[:, cs_], RL.t[:, 0:w], ALU.add,
                                 [RL.tok, SC.tok], [SC.tok])
                SM = smr.next()
                mx, mn, rng, mid, cnt, dd, thr = (SM.t[:, i:i + 1] for i in range(7))
                steps = SM.t[:, 8:8 + NBIS + 1]
                bis = t >= 2 and not OPT.get('b2_nobis')
                if bis:
                    k.op("dve", lambda e: e.tensor_reduce(out=mx, in_=SC.t[:, 0:nk], axis=AX.X, op=ALU.max),
                         [SC.tok], [SM.tok])
                k.op("dve", lambda e: e.tensor_reduce(out=mn, in_=SC.t[:, 0:nk], axis=AX.X, op=ALU.min),
                     [SC.tok], [SM.tok])
                k.memset("dve", SC.t[0:64, nk - 64:nk], -1.0e30, [SC.tok])
                if bis:
                    k.tt("dve", rng, mx, mn, ALU.subtract, [SM.tok], [SM.tok])
                    k.ts("dve", steps, P2[:], rng, None, ALU.mult, None, [p2t, SM.tok], [SM.tok])
                    k.tt("dve", mid, mn, steps[:, 0:1], ALU.add, [SM.tok], [SM.tok])
                    for it in range(NBIS):
                        k.ts("dve", junk[:, 0:nk], SC.t[:, 0:nk], mid, 0.0, ALU.is_ge, ALU.add,
                             [SC.tok, SM.tok], [jt, SM.tok], accum_out=cnt)
                        k.ts("dve", dd, cnt, 255.5, 0.5, ALU.is_ge, ALU.subtract, [SM.tok], [SM.tok])
                        k.stt("dve", mid, dd, steps[:, it:it + 1], mid, ALU.mult, ALU.add, [SM.tok], [SM.tok])
                    k.tt("dve", thr, mid, steps[:, NBIS:NBIS + 1], ALU.subtract, [SM.tok], [SM.tok])
                else:
                    k.copy("dve", thr, mn, [SM.tok], [SM.tok])
                MK = mkr.next()
                k.ts("dve", MK.t[:, 0:nk], SC.t[:, 0:nk], thr, None, ALU.is_ge, None, [SC.tok, SM.tok], [MK.tok])
                return MK

            def prep_b(t, MK):
                ngrp = (t + 1 + 3) // 4
                MT = mTall.next()
                for g in range(ngrp):
                    jts = list(range(g * 4, min(t + 1, g * 4 + 4)))
                    half = pst_i[0] % 2
                    pst_i[0] += 1
                    for jj, jt_ in enumerate(jts):
                        k.tr(PSTs[half][:, jj * 128:(jj + 1) * 128],
                             MK.t[:, jt_ * 128:(jt_ + 1) * 128], IDENT, [MK.tok, cmt], [pstoks[half]])
                    n = len(jts) * 128
                    k.act(MT.t[:, g * 512:g * 512 + n], PSTs[half][:, 0:n], AF.Copy,
                          [pstoks[half]], [MT.tok])
                return MT

            def b_stage(t, h, g, MT):
                ch, pb = h // 2, (h % 2) * 64
                jts = list(range(g * 4, min(t + 1, g * 4 + 4)))
                n = len(jts) * 128
                b = sbk.next()
                for jj, jt_ in enumerate(jts):
                    k.mm(b.ap(0, 128, jj * 128, (jj + 1) * 128), KBs[pb:pb + 64, ch, jt_ * 128:(jt_ + 1) * 128],
                         QBs[pb:pb + 64, ch, t * 128:(t + 1) * 128], True, True, [kbt, qbt], [b.tok])
                E = Er.next(); Em = Emr.next()
                k.act(E.t[:, 0:n], b.ap(0, 128, 0, n), AF.Exp, [b.tok], [E.tok], scale=0.125)
                k.tt("pool", Em.t[:, 0:n], E.t[:, 0:n], MT.t[:, g * 512:g * 512 + n], ALU.mult,
                     [E.tok, MT.tok], [Em.tok])
                return Em, jts

            ntile = OPT.get('b2_tmax', NT)
            MTs = {0: prep_b(0, prep(0))}
            for t in range(ntile):
                MKn = prep(t + 1) if t + 1 < ntile else None
                if OPT.get('b2_noattn'):
                    continue
                MT = MTs.pop(t)
                ngrp = (t + 1 + 3) // 4
                items = [(h, g) for h in range(4) for g in range(ngrp)]
                pend = [b_stage(t, *it_, MT) for it_ in items[0:2]]
                acc = accb.next()
                for i, (h, g) in enumerate(items):
                    if i + 2 < len(items):
                        pend.append(b_stage(t, *items[i + 2], MT))
                    Em, jts = pend.pop(0)
                    for jj, jt_ in enumerate(jts):
                        k.mm(acc.ap(0, 128, h * 65, h * 65 + 65), Em.t[:, jj * 128:(jj + 1) * 128], VB4[:, jt_, h, :],
                             jt_ == 0, jt_ == t, [vbt, Em.tok], [acc.tok])
                if MKn is not None:
                    MTs[t + 1] = prep_b(t + 1, MKn)
                finalize_attn(t, acc, rdr, ytr, yor, 256, YMt[1], pst_i)
            k.flush()
            if stop == 'B2':
                raise _Stop(nc, k, es)

        with ExitStack() as ph:
            wg = sbuf("wg", [128, 8, 3072], BF16, ph); wgt = Tok()
            wb = sbuf("wb", [128, 8, 1024], BF16, ph); wbt = Tok()
            wo = sbuf("wo", [128, 8, 1024], BF16, ph); wot = Tok()
            for c in range(8):
                k.dma("pool", wg[:, c, :], w_in[l, c * 128:(c + 1) * 128, C_GT:C_GT + 3072], [], [wgt])
            k.dma("pool", wb[:], wview(w_br[l], 0, 1024), [], [wbt])
            k.dma("pool", wo[:], wview(w_out[l], 0, 1024), [], [wot])
            X = Slot(sbuf("xc", [128, 8, 512], F32, ph))
            sq = sbuf("sqc", [128, 8, 512], BF16, ph); sqt = Tok()
            sd = sbuf("sdc", [128, 512], F32, ph); sdt = Tok()
            rstd = sbuf("rstdc", [128, 512], F32, ph); rst = Tok()
            hTb = sbuf("hTb", [128, 8, 512], BF16, ph); hbt = Tok()
            ym = sbuf("ymc", [128, 8, 512], BF16, ph); ymt = Tok()
            mg = sbuf("mg", [128, 8, 512], BF16, ph); mgt = [Tok() for _ in range(8)]
            sgr = Ring([Slot(sbuf(f"sg{i}", [128, 512], F32, ph)) for i in range(3)])
            tmr = Ring([Slot(sbuf(f"tm{i}", [128, 512], F32, ph)) for i in range(3)])
            acr = Ring([Slot(sbuf(f"ac{i}", [128, 512], F32, ph)) for i in range(2)])
            xo = Ring([Slot(sbuf(f"xo{i}", [128, 512], F32, ph)) for i in range(3)])
            bk = Ring(banks)
            KR = [(0, 2), (2, 4), (4, 8)]
            for tb in range(NB):
                k.dma("sp", X.t[:], xview(xsrc, tb), [xsrct], [X.tok])
                k.dma("sp", ym[:], xview(YM, tb), YMt, [ymt])
                make_hT(X.t, X.tok, SP_GMIX, sq, sqt, sd, sdt, rstd, rst, lambda c: hTb[:, c, :], hbt, bk.next())
                for dc in range(8):
                    AC = acr.next()
                    for br in range(3):
                        bg = bk.next()
                        for c in range(8):
                            k.mm(bg.ap(), wg[:, c, br * 1024 + dc * 128:br * 1024 + (dc + 1) * 128], hTb[:, c, :],
                                 c == 0, c == 7, [wgt, hbt], [bg.tok])
                        SG = sgr.next()
                        bcol = SP_BG + br * 8 + dc
                        k.act(SG.t[:], bg.ap(), AF.Sigmoid, [bg.tok, sptok], [SG.tok], bias=spt[:, bcol:bcol + 1])
                        bb_ = bk.next()
                        k0, k1 = KR[br]
                        for c in range(k0, k1):
                            k.mm(bb_.ap(), wb[:, c, dc * 128:(dc + 1) * 128], ym[:, c, :], c == k0, c == k1 - 1,
                                 [wbt, ymt], [bb_.tok])
                        if br == 0:
                            k.tt("dve", AC.t[:], SG.t[:], bb_.ap(), ALU.mult, [SG.tok, bb_.tok], [AC.tok])
                        else:
                            TM = tmr.next()
                            k.tt("dve", TM.t[:], SG.t[:], bb_.ap(), ALU.mult, [SG.tok, bb_.tok], [TM.tok])
                            if br == 1:
                                k.tt("pool", AC.t[:], AC.t[:], TM.t[:], ALU.add, [AC.tok, TM.tok], [AC.tok])
                            else:
                                k.tt("pool", mg[:, dc, :], AC.t[:], TM.t[:], ALU.add, [AC.tok, TM.tok], [mgt[dc]])
                for oc in range(8):
                    bo = bk.next()
                    for c in range(8):
                        k.mm(bo.ap(), wo[:, c, oc * 128:(oc + 1) * 128], mg[:, c, :], c == 0, c == 7,
                             [wot, mgt[c]], [bo.tok])
                    XO = xo.next()
                    k.tt("dve", XO.t[:], X.t[:, oc, :], bo.ap(), ALU.add, [X.tok, bo.tok], [XO.tok])
                    k.dma("sp", XT[oc * 128:(oc + 1) * 128, tb * 512:(tb + 1) * 512], XO.t[:], [XO.tok], [dtok["XT"]])
            k.flush()
            if stop == 'C':
                raise _Stop(nc, k, es)

        with ExitStack() as ph:
            w1 = sbuf("w1", [128, 8, 2 * DFF], BF16, ph); w1t = Tok()
            w2 = sbuf("w2", [128, 22, 1024], BF16, ph); w2t = Tok()
            for c in range(8):
                k.dma("pool", w1[:, c, :], w_f1[l, c * 128:(c + 1) * 128, :], [], [w1t])
            for c in range(22):
                k.dma("pool", w2[:, c, :], w_f2[l, c * 128:(c + 1) * 128, :], [], [w2t])
            X = Slot(sbuf("xd", [128, 8, 512], F32, ph))
            sq = sbuf("sqd", [128, 8, 512], BF16, ph); sqt = Tok()
            sd = sbuf("sdd", [128, 512], F32, ph); sdt = Tok()
            rstd = sbuf("rstdd", [128, 512], F32, ph); rst = Tok()
            hTb = sbuf("hTd", [128, 8, 512], BF16, ph); hbt = Tok()
            av = sbuf("av", [128, 22, 512], BF16, ph); avt = [Tok() for _ in range(22)]
            sgr = Ring([Slot(sbuf(f"sl{i}", [128, 512], F32, ph)) for i in range(3)])
            xo = Ring([Slot(sbuf(f"xod{i}", [128, 512], F32, ph)) for i in range(3)])
            bk = Ring(banks)
            xdst, xdstt = (yT, dtok["yT"]) if last else (XT, dtok["XT"])
            for tb in range(NB):
                k.dma("sp", X.t[:], xview(XT, tb), [dtok["XT"]], [X.tok])
                make_hT(X.t, X.tok, SP_GFFN, sq, sqt, sd, sdt, rstd, rst, lambda c: hTb[:, c, :], hbt, bk.next())
                for fc in range(22):
                    bg = bk.next()
                    for c in range(8):
                        k.mm(bg.ap(), w1[:, c, fc * 128:(fc + 1) * 128], hTb[:, c, :], c == 0, c == 7,
                             [w1t, hbt], [bg.tok])
                    bu = bk.next()
                    for c in range(8):
                        k.mm(bu.ap(), w1[:, c, DFF + fc * 128:DFF + (fc + 1) * 128], hTb[:, c, :], c == 0, c == 7,
                             [w1t, hbt], [bu.tok])
                    SG = sgr.next()
                    k.act(SG.t[:], bg.ap(), AF.Silu, [bg.tok], [SG.tok])
                    k.tt("dve", av[:, fc, :], SG.t[:], bu.ap(), ALU.mult, [SG.tok, bu.tok], [avt[fc]])
                for oc in range(8):
                    bo = bk.next()
                    for c in range(22):
                        k.mm(bo.ap(), w2[:, c, oc * 128:(oc + 1) * 128], av[:, c, :], c == 0, c == 21,
                             [w2t, avt[c]], [bo.tok])
                    XO = xo.next()
                    k.tt("dve", XO.t[:], X.t[:, oc, :], bo.ap(), ALU.add, [X.tok, bo.tok], [XO.tok])
                    k.dma("sp", xdst[oc * 128:(oc + 1) * 128, tb * 512:(tb + 1) * 512], XO.t[:], [XO.tok], [xdstt])
            if last:
                k.wait_all_dma()
            k.flush()
            if stop == 'D':
                raise _Stop(nc, k, es)
    es.close()
    return nc, k


def host_consts():
    pos = np.arange(S, dtype=np.float32)
    inv = (1.0 / (np.float32(10000.0) ** (np.arange(0, 64, 2, dtype=np.float32) / np.float32(64)))).astype(np.float32)
    ang = pos[:, None] * inv[None, :]
    ang = np.concatenate([ang, ang], axis=-1)
    cosT = np.cos(ang).astype(np.float32).T
    sinT = np.sin(ang).astype(np.float32).T
    cs = np.zeros((128, 2, S), np.float32)
    cs[0:64, 0], cs[64:128, 0] = cosT, cosT
    cs[0:64, 1], cs[64:128, 1] = sinT, sinT
    ones = np.ones((128, 128), np.float32)
    onesblk = np.zeros((128, 128), np.float32)
    onesblk[0:64, 0:64] = 1.0
    onesblk[64:128, 64:128] = 1.0
    rotm = np.zeros((128, 128), np.float32)
    for hb in (0, 64):
        for m in range(32):
            rotm[hb + m + 32, hb + m] = -1.0
            rotm[hb + m, hb + m + 32] = 1.0
    ident = np.eye(128, dtype=np.float32)
    cmat = np.stack([ones, onesblk, rotm, ident]).astype(np.float32)
    kk = np.arange(128)[:, None, None]
    jj = np.arange(5)[None, :, None]
    qq = np.arange(128)[None, None, :]
    cq = (qq >= 64).astype(np.int64)
    ck = 2 * jj - 8 + (kk >= 64)
    amask = ((ck >= cq - 8) & (ck <= cq)).astype(np.float32)
    relidx = np.clip(128 * (4 - jj) + qq - kk, -128, 128) + 128
    pow2 = np.tile((2.0 ** -(np.arange(NBIS + 1) + 1.0)).astype(np.float32)[None, :], (128, 1))
    return cs, cmat, amask, relidx, pow2


def host_pack(inp):
    cs, cmat, amask, relidx, pow2 = host_consts()
    f = lambda a: np.ascontiguousarray(np.asarray(a, dtype=np.float32))
    spar = np.zeros((L, 128, NSP), np.float32)
    p = np.arange(128)
    for l in range(L):
        spar[l, :, SP_GMIX:SP_GMIX + 8] = f(inp["g_mix"])[l].reshape(8, 128).T
        spar[l, :, SP_GFFN:SP_GFFN + 8] = f(inp["g_ffn"])[l].reshape(8, 128).T
        spar[l, :, SP_BG:SP_BG + 24] = f(inp["b_gate"])[l].reshape(24, 128).T
        spar[l, :, SP_GAQ] = f(inp["qk_gain_a"])[l, 0][p % 64]
        spar[l, :, SP_GAK] = f(inp["qk_gain_a"])[l, 1][p % 64]
        spar[l, :, SP_GBQ] = f(inp["qk_gain_b"])[l, 0][p % 64]
        spar[l, :, SP_GBK] = f(inp["qk_gain_b"])[l, 1][p % 64]
        spar[l, :, SP_GIK] = f(inp["g_idx_k"])[l][p % 64]
        cw = f(inp["conv_w"])[l]
        for j in range(4):
            spar[l, :, SP_CW + j * 4:SP_CW + j * 4 + 4] = cw[j].reshape(4, 128).T
        spar[l, :, SP_CB:SP_CB + 4] = f(inp["conv_b"])[l].reshape(4, 128).T
        spar[l, :, SP_BA:SP_BA + 4] = f(inp["lru_ba"])[l].reshape(4, 128).T
        spar[l, :, SP_BX:SP_BX + 4] = f(inp["lru_bx"])[l].reshape(4, 128).T
        spar[l, :, SP_LAM:SP_LAM + 4] = f(inp["lru_lambda"])[l].reshape(4, 128).T
    lrubd = np.zeros((L, 2, 4, 128, 128), np.float32)
    for m, nm in enumerate(("lru_wa", "lru_wx")):
        wsrc = f(inp[nm])
        for cc in range(4):
            lrubd[:, m, cc, 0:64, 0:64] = wsrc[:, 2 * cc]
            lrubd[:, m, cc, 64:128, 64:128] = wsrc[:, 2 * cc + 1]
    rb = f(inp["rel_bias"])
    ab = rb[:, :, relidx]
    abias = np.ascontiguousarray(ab.transpose(0, 2, 1, 3, 4))
    shared = {"w_in": f(inp["w_in"]), "w_branch": f(inp["w_branch"]), "w_out": f(inp["w_out"]),
              "w_ffn_in": f(inp["w_ffn_in"]), "w_ffn_out": f(inp["w_ffn_out"]),
              "spar": spar, "lrubd": lrubd, "abias": abias, "cs": cs, "cmat": cmat,
              "amask": amask, "pow2": pow2}
    return shared


_CACHE = {}


def kernel(**inputs):
    x = np.asarray(inputs["x"], dtype=np.float32)
    shared = host_pack(inputs)
    if "nc" not in _CACHE:
        _CACHE["nc"] = build()[0]
    nc = _CACHE["nc"]
    in_maps = []
    for b in range(8):
        m = dict(shared)
        m["xT"] = np.ascontiguousarray(x[b].T)
        in_maps.append(m)
    res = run_bass_kernel_spmd(nc, in_maps, core_ids=list(range(8)))
    out = np.stack([np.ascontiguousarray(r["yT"].T) for r in res.results], axis=0)
    return out.astype(np.float32)
```

```python
import math
from contextlib import ExitStack
import numpy as np
import concourse.bass as bass
import concourse.mybir as mybir
from concourse.bass_utils import run_bass_kernel_spmd

F32 = mybir.dt.float32
BF16 = mybir.dt.bfloat16
AF = mybir.ActivationFunctionType
ALU = mybir.AluOpType
AX = mybir.AxisListType

D = 1024; S = 4096; L = 4; DIN = 5956; DFF = 2816
NT = S // 128; NB = S // 512
EPS = 1e-6
NBIS = 14
C_AQ, C_AK, C_AV, C_BQ, C_BK, C_BV, C_IQ, C_IK, C_IW, C_CX, C_CY, C_GT = (
    0, 256, 512, 768, 1024, 1280, 1536, 1792, 1856, 1860, 2372, 2884)
SP_GMIX, SP_GFFN, SP_BG, SP_GAQ, SP_GAK, SP_GBQ, SP_GBK, SP_GIK, SP_CW, SP_CB, SP_BA, SP_BX, SP_LAM = (
    0, 8, 16, 40, 41, 42, 43, 44, 45, 61, 65, 69, 73)
NSP = 77


class Tok:
    __slots__ = ("w", "r")

    def __init__(self):
        self.w = None
        self.r = {}


class Ring:
    def __init__(self, items):
        self.items = items
        self.i = -1

    def next(self):
        self.i = (self.i + 1) % len(self.items)
        return self.items[self.i]


class Slot:
    def __init__(self, t):
        self.t = t
        self.tok = Tok()


class K:
    INC = {"pe": 1, "act": 1, "dve": 1, "pool": 1}
    NDMA = {"sp": 12, "pool": 6}

    def __init__(self, nc, es):
        self.nc = nc
        self.sem = {f"{n}@{l}": es.enter_context(nc.semaphore(f"s_{n}_{l}")) for n in self.INC for l in range(L)}
        for q, n in self.NDMA.items():
            for r in range(n):
                self.sem[f"dma_{q}{r}"] = es.enter_context(nc.semaphore(f"s_dma_{q}{r}"))
        self.cnt = {n: 0 for n in self.sem}
        self.ep = 0
        self.dma_rr = {q: 0 for q in self.NDMA}
        self.streams = {e: [] for e in ("pe", "act", "dve", "pool", "sp")}
        self.waited = {e: {} for e in self.streams}
        self.ninstr = 0

    def op(self, stream, fn, reads=(), writes=(), counter=None):
        if counter is None:
            counter = f"{stream}@{self.ep}"
        deps = {}
        if counter[:4] == "dma_" and self.cnt[counter] > 0:
            deps[counter] = self.cnt[counter]
        for t in reads:
            if t.w is not None and deps.get(t.w[0], 0) < t.w[1]:
                deps[t.w[0]] = t.w[1]
        for t in writes:
            if t.w is not None and deps.get(t.w[0], 0) < t.w[1]:
                deps[t.w[0]] = t.w[1]
            for c, s in t.r.items():
                if deps.get(c, 0) < s:
                    deps[c] = s
        wd = self.waited[stream]
        waits = []
        for c, s in deps.items():
            if c[:3] == "pe@" and counter[:3] == "pe@":
                continue
            if wd.get(c, 0) >= s:
                continue
            wd[c] = s
            waits.append((c, s))
        self.cnt[counter] += 1
        seq = self.cnt[counter]
        for t in reads:
            if t.r.get(counter, 0) < seq:
                t.r[counter] = seq
        for t in writes:
            t.w = (counter, seq)
            t.r = {}
        self.streams[stream].append((waits, fn, counter))
        self.ninstr += 1

    def wait_all_dma(self):
        waits = [(c, self.cnt[c]) for c in self.cnt if c[:4] == "dma_" and self.cnt[c] > 0]
        self.streams["sp"].append((waits, None, None))

    def flush(self):
        nc = self.nc
        streams = self.streams
        self.streams = {e: [] for e in streams}
        sem, INC = self.sem, self.INC

        def mk(lst):
            def f(eng):
                for waits, fn, counter in lst:
                    for c, s in waits:
                        eng.wait_ge(sem[c], s * (16 if c[:4] == "dma_" else 1))
                    if fn is not None:
                        fn(eng).then_inc(sem[counter], 16 if counter[:4] == "dma_" else 1)
            return f

        with nc.Block() as block:
            block.tensor(mk(streams["pe"]))
            block.scalar(mk(streams["act"]))
            block.vector(mk(streams["dve"]))
            block.gpsimd(mk(streams["pool"]))
            block.sync(mk(streams["sp"]))

    def dma(self, q, out, in_, reads, writes):
        r = self.dma_rr[q] % self.NDMA[q]
        self.dma_rr[q] += 1
        self.op(q, lambda e: e.dma_start(out=out, in_=in_), reads, writes, counter=f"dma_{q}{r}")

    def mm(self, out, lhsT, rhs, start, stop, reads, writes):
        self.op("pe", lambda e: e.matmul(out, lhsT, rhs, start=start, stop=stop), reads, writes)

    def tr(self, out, in_, ident, reads, writes):
        self.op("pe", lambda e: e.transpose(out, in_, ident), reads, writes)

    def act(self, out, in_, func, reads, writes, bias=None, scale=None):
        kw = {}
        if bias is not None:
            kw["bias"] = bias
        if scale is not None:
            kw["scale"] = scale
        self.op("act", lambda e: e.activation(out=out, in_=in_, func=func, **kw), reads, writes)

    def tt(self, eng, out, in0, in1, op, reads, writes):
        self.op(eng, lambda e: e.tensor_tensor(out=out, in0=in0, in1=in1, op=op), reads, writes)

    def ts(self, eng, out, in0, s1, s2, op0, op1, reads, writes, accum_out=None):
        if op1 is None:
            self.op(eng, lambda e: e.tensor_scalar(out=out, in0=in0, scalar1=s1, scalar2=None, op0=op0),
                    reads, writes)
        elif accum_out is None:
            self.op(eng, lambda e: e.tensor_scalar(out=out, in0=in0, scalar1=s1, scalar2=s2, op0=op0, op1=op1),
                    reads, writes)
        else:
            self.op(eng, lambda e: e.tensor_scalar(out=out, in0=in0, scalar1=s1, scalar2=s2, op0=op0, op1=op1,
                                                   accum_out=accum_out), reads, writes)

    def stt(self, eng, out, in0, scalar, in1, op0, op1, reads, writes):
        self.op(eng, lambda e: e.scalar_tensor_tensor(out=out, in0=in0, scalar=scalar, in1=in1, op0=op0, op1=op1),
                reads, writes)

    def recip(self, out, in_, reads, writes):
        self.op("dve", lambda e: e.reciprocal(out=out, in_=in_), reads, writes)

    def memset(self, eng, ap, val, writes):
        self.op(eng, lambda e: e.memset(ap, val), (), writes)

    def copy(self, eng, out, in_, reads, writes):
        self.op(eng, lambda e: e.tensor_copy(out=out, in_=in_), reads, writes)


OPT = {}


class _Stop(Exception):
    pass


def build(nlayers=L, dbg=False, stop=None):
    try:
        return _build(nlayers, dbg, stop)
    except _Stop as e:
        nc, k, es = e.args
        k.wait_all_dma()
        k.flush()
        return nc, k


def _build(nlayers, dbg, stop):
    nc = bass.Bass("TRN2", target_bir_lowering=False)
    es = ExitStack()

    def din(name, shape, dt=F32):
        return nc.dram_tensor(name, list(shape), dt, kind="ExternalInput").ap()

    kind_dbg = "ExternalOutput" if dbg else "Internal"

    def dscr(name, shape, dt):
        return nc.dram_tensor(name, list(shape), dt, kind=kind_dbg).ap()

    xT_in = din("xT", [D, S])
    w_in = din("w_in", [L, D, DIN])
    w_br = din("w_branch", [L, D, D])
    w_out = din("w_out", [L, D, D])
    w_f1 = din("w_ffn_in", [L, D, 2 * DFF])
    w_f2 = din("w_ffn_out", [L, DFF, D])
    spar = din("spar", [L, 128, NSP])
    lrubd = din("lrubd", [L, 2, 4, 128, 128])
    abias = din("abias", [L, 128, 4, 5, 128])
    cs_d = din("cs", [128, 2, S])
    cmat = din("cmat", [4, 128, 128])
    amask = din("amask", [128, 5, 128])
    pow2 = din("pow2", [128, NBIS + 1])
    yT = nc.dram_tensor("yT", [D, S], F32, kind="ExternalOutput").ap()

    XT = dscr("XT", [D, S], F32)
    QA = dscr("QA", [256, S], BF16)
    KA = dscr("KA", [256, S], BF16)
    QB = dscr("QB", [256, S], BF16)
    KB = dscr("KB", [256, S], BF16)
    QI = dscr("QI", [256, S], BF16)
    KI = dscr("KI", [64, S], BF16)
    VAB = dscr("VAB", [S, 512], BF16)
    WI = dscr("WI", [S, 4], F32)
    YM = dscr("YM", [D, S], BF16)
    dtok = {n: Tok() for n in ("XT", "QA", "KA", "QB", "KB", "QI", "KI", "VAB", "WI", "YM", "xin", "yT")}
    YMt = [Tok() for _ in range(3)]

    k = K(nc, es)

    PS2 = [es.enter_context(nc.psum_tensor(f"ps2_{i}", [128, 1024], F32)) for i in range(3)]
    PSTs = [es.enter_context(nc.psum_tensor(f"pst{i}", [128, 1024], BF16)) for i in range(2)]

    class Bank:
        def __init__(self, t, c0):
            self.t, self.c0, self.tok = t, c0, Tok()

        def ap(self, p0=0, p1=128, a=0, b=512):
            return self.t[p0:p1, self.c0 + a:self.c0 + b]

    banks = []
    for t in PS2:
        banks.append(Bank(t, 0))
        banks.append(Bank(t, 512))
    pstoks = [Tok(), Tok()]

    uid = [0]

    def sbuf(name, shape, dt, stack=es):
        uid[0] += 1
        return stack.enter_context(nc.sbuf_tensor(f"{name}_{uid[0]}", list(shape), dt))

    cm = sbuf("cm", [128, 4, 128], BF16)
    cmt = Tok()
    epsc = sbuf("epsc", [128, 1], F32)
    epst = Tok()
    spt = sbuf("spt", [128, NSP], F32)
    sptok = Tok()
    clam = sbuf("clam", [128, 4], F32)
    clamtok = Tok()
    k.dma("pool", cm[:], cmat.rearrange("m p n -> p m n"), [], [cmt])
    k.memset("dve", epsc[:], EPS, [epst])
    ONES, ONESBLK, ROTM, IDENT = (cm[:, i, :] for i in range(4))

    def xview(ap2d, tb):
        return ap2d.rearrange("(c p) t -> p c t", p=128)[:, :, tb * 512:(tb + 1) * 512]

    def wview(w2d, c0, n):
        return w2d.rearrange("(c p) n -> p c n", p=128)[:, :, c0:c0 + n]

    def make_hT(X, Xtok, gcol, sq, sqtok, sd, sdtok, rstd, rstok, hdst, htok, bank):
        k.act(sq[:], X[:], AF.Square, [Xtok], [sqtok])
        for c in range(8):
            k.mm(bank.ap(), ONES, sq[:, c, :], c == 0, c == 7, [sqtok, cmt], [bank.tok])
        k.act(sd[:], bank.ap(), AF.Sqrt, [bank.tok, epst], [sdtok], bias=epsc[:, 0:1], scale=1.0 / D)
        k.recip(rstd[:], sd[:], [sdtok], [rstok])
        for c in range(8):
            k.stt("dve", hdst(c), X[:, c, :], spt[:, gcol + c:gcol + c + 1], rstd[:],
                  ALU.mult, ALU.mult, [Xtok, sptok, rstok], [htok])

    for l in range(nlayers):
        xsrc, xsrct = (xT_in, dtok["xin"]) if l == 0 else (XT, dtok["XT"])
        last = l == nlayers - 1
        k.ep = l
        k.dma("sp", spt[:], spar[l], [], [sptok])

        with ExitStack() as ph:
            hT = sbuf("hT", [128, 8, S], BF16, ph); hTt = Tok()
            with ExitStack() as ph1:
                xs = Ring([Slot(sbuf(f"xs{i}", [128, 8, 512], F32, ph1)) for i in range(2)])
                sq = sbuf("sq", [128, 8, 512], BF16, ph1); sqt = Tok()
                sd = sbuf("sd", [128, 512], F32, ph1); sdt = Tok()
                rstd = sbuf("rstd", [128, 512], F32, ph1); rst = Tok()
                for tb in range(NB):
                    X = xs.next()
                    k.dma("sp", X.t[:], xview(xsrc, tb), [xsrct], [X.tok])
                    make_hT(X.t, X.tok, SP_GMIX, sq, sqt, sd, sdt, rstd, rst,
                            lambda c, tb=tb: hT[:, c, tb * 512:(tb + 1) * 512], hTt, banks[tb % 6])
                k.flush()
                if stop == 'A1':
                    raise _Stop(nc, k, es)

            wr = Ring([Slot(sbuf(f"wr{i}", [128, 8, 128], BF16, ph)) for i in range(3)])
            ob = Ring([Slot(sbuf(f"ob{i}", [128, S], BF16, ph)) for i in range(2)])
            csr = Ring([Slot(sbuf(f"csr{i}", [128, 2, 512], F32, ph)) for i in range(3)])
            sqb = Ring([Slot(sbuf(f"sqb{i}", [128, 512], BF16, ph)) for i in range(3)])
            sd2 = Ring([Slot(sbuf(f"sd2{i}", [128, 512], F32, ph)) for i in range(3)])
            rs2 = Ring([Slot(sbuf(f"rs2{i}", [128, 512], F32, ph)) for i in range(3)])
            qnb = Ring([Slot(sbuf(f"qnb{i}", [128, 512], BF16, ph)) for i in range(3)])
            t1r = Ring([Slot(sbuf(f"t1r{i}", [128, 512], F32, ph)) for i in range(3)])
            t2r = Ring([Slot(sbuf(f"t2r{i}", [128, 512], F32, ph)) for i in range(3)])
            bk = Ring(banks)

            def load_w(c0, m):
                W = wr.next()
                k.dma("pool", W.t[:, :, 0:m], wview(w_in[l], c0, m), [], [W.tok])
                return W

            def proj_mm(W, m, tb):
                b = bk.next()
                for c in range(8):
                    k.mm(b.ap(0, m), W.t[:, c, 0:m], hT[:, c, tb * 512:(tb + 1) * 512], c == 0, c == 7,
                         [W.tok, hTt], [b.tok])
                return b

            chunks = []
            for ch in range(2):
                chunks.append((C_AQ + ch * 128, 128, QA, dtok["QA"], ch * 128, SP_GAQ, True, False))
                chunks.append((C_AK + ch * 128, 128, KA, dtok["KA"], ch * 128, SP_GAK, True, False))
                chunks.append((C_BQ + ch * 128, 128, QB, dtok["QB"], ch * 128, SP_GBQ, True, True))
                chunks.append((C_BK + ch * 128, 128, KB, dtok["KB"], ch * 128, SP_GBK, True, True))
                chunks.append((C_IQ + ch * 128, 128, QI, dtok["QI"], ch * 128, 0, False, True))
            chunks.append((C_IK, 64, KI, dtok["KI"], 0, SP_GIK, True, True))
            items = [(ci, tb) for ci in range(len(chunks)) for tb in range(NB)]
            ist, cst = {}, {}

            def rope_start(st, m, tb):
                QN = st["QN"]
                CS = csr.next()
                k.dma("sp", CS.t[:], cs_d[:, :, tb * 512:(tb + 1) * 512], [], [CS.tok])
                b3 = bk.next()
                k.mm(b3.ap(0, m), cm[0:m, 2, 0:m], QN.t[0:m, :], True, True, [QN.tok, cmt], [b3.tok])
                st["CS"], st["b3"] = CS, b3

            def S1(i):
                ci, tb = items[i]
                c0, m = chunks[ci][0:2]
                if tb == 0:
                    cst[ci] = (load_w(c0, m), ob.next())
                ist[i] = {"b": proj_mm(cst[ci][0], m, tb)}

            def S2(i):
                ci, tb = items[i]
                c0, m, dst, dstt, row0, gcol, norm, rope = chunks[ci]
                st = ist[i]
                b = st["b"]
                if norm:
                    SQ = sqb.next()
                    k.act(SQ.t[0:m, :], b.ap(0, m), AF.Square, [b.tok], [SQ.tok])
                    b2 = bk.next()
                    k.mm(b2.ap(0, m), cm[0:m, 1, 0:m], SQ.t[0:m, :], True, True, [SQ.tok, cmt], [b2.tok])
                    st["b2"] = b2
                else:
                    QN = qnb.next()
                    k.act(QN.t[0:m, :], b.ap(0, m), AF.Copy, [b.tok], [QN.tok])
                    st["QN"] = QN
                    rope_start(st, m, tb)

            def S3(i):
                ci, tb = items[i]
                c0, m, dst, dstt, row0, gcol, norm, rope = chunks[ci]
                st = ist[i]
                if not norm:
                    return
                b, b2 = st["b"], st["b2"]
                O = cst[ci][1]
                SD = sd2.next(); RS = rs2.next()
                k.act(SD.t[0:m, :], b2.ap(0, m), AF.Sqrt, [b2.tok, epst], [SD.tok],
                      bias=epsc[0:m, 0:1], scale=1.0 / 64)
                k.recip(RS.t[0:m, :], SD.t[0:m, :], [SD.tok], [RS.tok])
                if not rope:
                    k.stt("dve", O.t[0:m, tb * 512:(tb + 1) * 512], b.ap(0, m), spt[0:m, gcol:gcol + 1], RS.t[0:m, :],
                          ALU.mult, ALU.mult, [b.tok, sptok, RS.tok], [O.tok])
                    return
                QN = qnb.next()
                k.stt("dve", QN.t[0:m, :], b.ap(0, m), spt[0:m, gcol:gcol + 1], RS.t[0:m, :],
                      ALU.mult, ALU.mult, [b.tok, sptok, RS.tok], [QN.tok])
                st["QN"] = QN
                rope_start(st, m, tb)

            def S4(i):
                ci, tb = items[i]
                c0, m, dst, dstt, row0, gcol, norm, rope = chunks[ci]
                st = ist.pop(i)
                O = cst[ci][1]
                if rope:
                    QN, CS, b3 = st["QN"], st["CS"], st["b3"]
                    T1 = t1r.next(); T2 = t2r.next()
                    k.tt("dve", T1.t[0:m, :], QN.t[0:m, :], CS.t[0:m, 0, :], ALU.mult, [QN.tok, CS.tok], [T1.tok])
                    k.tt("dve", T2.t[0:m, :], b3.ap(0, m), CS.t[0:m, 1, :], ALU.mult, [b3.tok, CS.tok], [T2.tok])
                    k.tt("pool", O.t[0:m, tb * 512:(tb + 1) * 512], T1.t[0:m, :], T2.t[0:m, :], ALU.add,
                         [T1.tok, T2.tok], [O.tok])
                if tb == NB - 1:
                    k.dma("sp", dst[row0:row0 + m, :], O.t[0:m, :], [O.tok], [dstt])

            for s_ in range(len(items) + 3):
                for stage, off in ((S4, 3), (S3, 2), (S2, 1), (S1, 0)):
                    i = s_ - off
                    if 0 <= i < len(items):
                        stage(i)

            wv = sbuf("wv", [128, 8, 512], BF16, ph); wvt = Tok()
            wiw = sbuf("wiw", [128, 8, 4], BF16, ph); wiwt = Tok()
            k.dma("pool", wv[:, :, 0:256], wview(w_in[l], C_AV, 256), [], [wvt])
            k.dma("pool", wv[:, :, 256:512], wview(w_in[l], C_BV, 256), [], [wvt])
            k.dma("pool", wiw[:], wview(w_in[l], C_IW, 4), [], [wiwt])
            vb = Ring([Slot(sbuf(f"vb{i}", [128, 512], BF16, ph)) for i in range(2)])
            wib = sbuf("wib", [128, NT, 4], F32, ph); wibt = Tok()
            for tt_ in range(NT):
                b = bk.next()
                for c in range(8):
                    k.mm(b.ap(), hT[:, c, tt_ * 128:(tt_ + 1) * 128], wv[:, c, :], c == 0, c == 7,
                         [hTt, wvt], [b.tok])
                V = vb.next()
                k.act(V.t[:], b.ap(), AF.Copy, [b.tok], [V.tok])
                k.dma("sp", VAB[tt_ * 128:(tt_ + 1) * 128, :], V.t[:], [V.tok], [dtok["VAB"]])
                b = bk.next()
                for c in range(8):
                    k.mm(b.ap(0, 128, 0, 4), hT[:, c, tt_ * 128:(tt_ + 1) * 128], wiw[:, c, :], c == 0, c == 7,
                         [hTt, wiwt], [b.tok])
                k.ts("dve", wib[:, tt_, :], b.ap(0, 128, 0, 4), 0.0625, None, ALU.mult, None, [b.tok], [wibt])
            k.dma("sp", WI.rearrange("(t p) c -> p t c", p=128), wib[:], [wibt], [dtok["WI"]])

            k.act(clam[:], spt[:, SP_LAM:SP_LAM + 4], AF.Exp, [sptok], [clamtok], scale=-1.0)
            k.act(clam[:], clam[:], AF.Ln, [clamtok], [clamtok], bias=1.0)
            k.ts("dve", clam[:], clam[:], -8.0, None, ALU.mult, None, [clamtok], [clamtok])
            cxb = sbuf("cxb", [128, 3 + S], F32, ph)
            cxt = [Tok() for _ in range(NB + 1)]
            k.memset("dve", cxb[:, 0:3], 0.0, [cxt[NB]])
            wbd = Ring([Slot(sbuf(f"wbd{i}", [128, 2, 128], BF16, ph)) for i in range(2)])
            f32r = {n: Ring([Slot(sbuf(f"c{n}{i}", [128, 512], F32, ph)) for i in range(2)])
                    for n in ("gy", "u", "r", "i", "a", "m", "bb", "hs")}
            ubr = Ring([Slot(sbuf(f"ub{i}", [128, 512], BF16, ph)) for i in range(2)])
            for cc in range(4):
                WX = load_w(C_CX + cc * 128, 128)
                WY = load_w(C_CY + cc * 128, 128)
                BD = wbd.next()
                k.dma("pool", BD.t[:], lrubd[l, :, cc].rearrange("m p n -> p m n"), [], [BD.tok])
                O = ob.next()
                prev_hs = None
                for tb in range(NB):
                    b = proj_mm(WX, 128, tb)
                    k.act(cxb[:, 3 + tb * 512:3 + (tb + 1) * 512], b.ap(), AF.Copy, [b.tok], [cxt[tb]])
                    b = proj_mm(WY, 128, tb)
                    GY = f32r["gy"].next()
                    k.act(GY.t[:], b.ap(), AF.Gelu, [b.tok], [GY.tok])
                    U = f32r["u"].next()
                    rd = [cxt[tb], cxt[tb - 1] if tb > 0 else cxt[NB], sptok]
                    cw = lambda j: spt[:, SP_CW + j * 4 + cc:SP_CW + j * 4 + cc + 1]
                    k.ts("dve", U.t[:], cxb[:, tb * 512:tb * 512 + 512], cw(0),
                         spt[:, SP_CB + cc:SP_CB + cc + 1], ALU.mult, ALU.add, rd, [U.tok])
                    for j in range(1, 4):
                        k.stt("dve", U.t[:], cxb[:, tb * 512 + j:tb * 512 + j + 512], cw(j), U.t[:],
                              ALU.mult, ALU.add, rd + [U.tok], [U.tok])
                    UB = ubr.next()
                    k.act(UB.t[:], U.t[:], AF.Copy, [U.tok], [UB.tok])
                    bR = bk.next()
                    k.mm(bR.ap(), BD.t[:, 0, :], UB.t[:], True, True, [BD.tok, UB.tok], [bR.tok])
                    bI = bk.next()
                    k.mm(bI.ap(), BD.t[:, 1, :], UB.t[:], True, True, [BD.tok, UB.tok], [bI.tok])
                    R = f32r["r"].next(); I_ = f32r["i"].next(); A = f32r["a"].next(); M = f32r["m"].next()
                    k.act(R.t[:], bR.ap(), AF.Sigmoid, [bR.tok, sptok], [R.tok], bias=spt[:, SP_BA + cc:SP_BA + cc + 1])
                    k.act(I_.t[:], bI.ap(), AF.Sigmoid, [bI.tok, sptok], [I_.tok], bias=spt[:, SP_BX + cc:SP_BX + cc + 1])
                    k.act(A.t[:], R.t[:], AF.Exp, [R.tok, clamtok], [A.tok], scale=clam[:, cc:cc + 1])
                    k.act(M.t[:], A.t[:], AF.Square, [A.tok], [M.tok])
                    k.act(M.t[:], M.t[:], AF.Sqrt, [M.tok], [M.tok], bias=1.0, scale=-1.0)
                    BB = f32r["bb"].next()
                    k.tt("pool", BB.t[:], I_.t[:], U.t[:], ALU.mult, [I_.tok, U.tok], [BB.tok])
                    k.tt("pool", BB.t[:], BB.t[:], M.t[:], ALU.mult, [BB.tok, M.tok], [BB.tok])
                    HS = f32r["hs"].next()
                    if prev_hs is None:
                        k.op("dve", lambda e, HS=HS, A=A, BB=BB: e.tensor_tensor_scan(
                            out=HS.t[:], data0=A.t[:], data1=BB.t[:], initial=0.0, op0=ALU.mult, op1=ALU.add),
                            [A.tok, BB.tok], [HS.tok])
                    else:
                        k.op("dve", lambda e, HS=HS, A=A, BB=BB, PH=prev_hs: e.tensor_tensor_scan(
                            out=HS.t[:], data0=A.t[:], data1=BB.t[:], initial=PH.t[:, 511:512],
                            op0=ALU.mult, op1=ALU.add), [A.tok, BB.tok, prev_hs.tok], [HS.tok])
                    prev_hs = HS
                    k.tt("pool", O.t[:, tb * 512:(tb + 1) * 512], HS.t[:], GY.t[:], ALU.mult,
                         [HS.tok, GY.tok], [O.tok])
                k.dma("sp", YM[512 + cc * 128:512 + (cc + 1) * 128, :], O.t[:], [O.tok], [YMt[2]])
            k.flush()
            if stop == 'A':
                raise _Stop(nc, k, es)

        def finalize_attn(t, acc, rdr, ytr, yor, row0, ymtok, pst_i):
            RD = rdr.next()
            a3 = acc.t[:, acc.c0:acc.c0 + 260].rearrange("p (h e) -> p h e", e=65)
            k.recip(RD.t[:], a3[:, :, 64:65], [acc.tok], [RD.tok])
            YT = ytr.next()
            for h in range(4):
                k.ts("dve", YT.t[:, h * 64:(h + 1) * 64], acc.ap(0, 128, h * 65, h * 65 + 64), RD.t[:, h, :], None,
                     ALU.mult, None, [acc.tok, RD.tok], [YT.tok])
            half = pst_i[0] % 2
            pst_i[0] += 1
            for c in range(2):
                k.tr(PSTs[half][:, c * 128:(c + 1) * 128], YT.t[:, c * 128:(c + 1) * 128], IDENT,
                     [YT.tok, cmt], [pstoks[half]])
            YO = yor.next()
            k.act(YO.t[:], PSTs[half][:, 0:256], AF.Copy, [pstoks[half]], [YO.tok])
            k.dma("sp", YM[row0:row0 + 256, t * 128:(t + 1) * 128].rearrange("(c p) q -> p c q", p=128),
                  YO.t[:].rearrange("p (c q) -> p c q", c=2), [YO.tok], [ymtok])

        with ExitStack() as ph:
            KAs = sbuf("KAs", [128, 2, S], BF16, ph); kat = Tok()
            QAs = sbuf("QAs", [128, 2, S], BF16, ph); qat = Tok()
            VA4 = sbuf("VA4", [128, NT, 4, 65], BF16, ph); vat = Tok()
            EB = sbuf("EB", [128, 4, 5, 128], F32, ph); ebt = Tok()
            AM = sbuf("AM", [128, 5, 128], F32, ph); amt = Tok()
            k.dma("sp", KAs[:], KA.rearrange("(c p) t -> p c t", p=128), [dtok["KA"]], [kat])
            k.dma("sp", QAs[:], QA.rearrange("(c p) t -> p c t", p=128), [dtok["QA"]], [qat])
            for h in range(4):
                k.dma("sp", VA4[:, :, h, 0:64], VAB.rearrange("(t p) c -> p t c", p=128)[:, :, h * 64:(h + 1) * 64],
                      [dtok["VAB"]], [vat])
            k.memset("pool", VA4[:, :, :, 64:65], 1.0, [vat])
            k.dma("sp", EB[:], abias[l], [], [ebt])
            k.dma("sp", AM[:], amask, [], [amt])
            k.act(EB[:], EB[:], AF.Exp, [ebt], [ebt])
            for h in range(4):
                k.tt("dve", EB[:, h], EB[:, h], AM[:], ALU.mult, [ebt, amt], [ebt])
            Er = Ring([Slot(sbuf(f"Ea{i}", [128, 640], F32, ph)) for i in range(2)])
            Emr = Ring([Slot(sbuf(f"Ema{i}", [128, 640], BF16, ph)) for i in range(3)])
            rdr = Ring([Slot(sbuf(f"rda{i}", [128, 4, 1], F32, ph)) for i in range(2)])
            ytr = Ring([Slot(sbuf(f"yta{i}", [128, 256], BF16, ph)) for i in range(2)])
            yor = Ring([Slot(sbuf(f"yoa{i}", [128, 256], BF16, ph)) for i in range(2)])
            stb = Ring([(PS2[0], banks[0], banks[1]), (PS2[1], banks[2], banks[3])])
            accb = Ring([banks[4], banks[5]])
            pst_i = [0]

            def a_stage(t, h):
                ch, pb = h // 2, (h % 2) * 64
                js = [j for j in range(5) if t - 4 + j >= 0]
                j0 = js[0]
                PT_, ba, bb_ = stb.next()
                for j in js:
                    k.mm(PT_[:, j * 128:(j + 1) * 128], KAs[pb:pb + 64, ch, (t - 4 + j) * 128:(t - 3 + j) * 128],
                         QAs[pb:pb + 64, ch, t * 128:(t + 1) * 128], True, True,
                         [kat, qat], [ba.tok if j < 4 else bb_.tok])
                E = Er.next(); Em = Emr.next()
                k.act(E.t[:, j0 * 128:640], PT_[:, j0 * 128:640], AF.Exp, [ba.tok, bb_.tok], [E.tok], scale=0.125)
                k.tt("dve", Em.t[:, j0 * 128:640], E.t[:, j0 * 128:640],
                     EB[:, h, j0:5, :].rearrange("p j q -> p (j q)"), ALU.mult, [E.tok, ebt], [Em.tok])
                return Em, js

            items = [(t, h) for t in range(NT) for h in range(4)]
            pend = a_stage(*items[0])
            acc = None
            for i, (t, h) in enumerate(items):
                Em, js = pend
                if i + 1 < len(items):
                    pend = a_stage(*items[i + 1])
                if h == 0:
                    acc = accb.next()
                for j in js:
                    k.mm(acc.ap(0, 128, h * 65, h * 65 + 65), Em.t[:, j * 128:(j + 1) * 128], VA4[:, t - 4 + j, h, :],
                         j == js[0], j == 4, [vat, Em.tok], [acc.tok])
                if h == 3:
                    finalize_attn(t, acc, rdr, ytr, yor, 0, YMt[0], pst_i)
            k.flush()
            if stop == 'B1':
                raise _Stop(nc, k, es)

        with ExitStack() as ph:
            KI2 = sbuf("KI2", [128, S], BF16, ph); kit = Tok()
            QIs = sbuf("QIs", [128, 2, S], BF16, ph); qit = Tok()
            KBs = sbuf("KBs", [128, 2, S], BF16, ph); kbt = Tok()
            QBs = sbuf("QBs", [128, 2, S], BF16, ph); qbt = Tok()
            VB4 = sbuf("VB4", [128, NT, 4, 65], BF16, ph); vbt = Tok()
            WIs = sbuf("WIs", [128, NT, 4], F32, ph); wit = Tok()
            P2 = sbuf("P2", [128, NBIS + 1], F32, ph); p2t = Tok()
            k.dma("sp", KI2[0:64, :], KI, [dtok["KI"]], [kit])
            k.dma("sp", KI2[64:128, :], KI, [dtok["KI"]], [kit])
            k.dma("sp", QIs[:], QI.rearrange("(c p) t -> p c t", p=128), [dtok["QI"]], [qit])
            k.dma("sp", KBs[:], KB.rearrange("(c p) t -> p c t", p=128), [dtok["KB"]], [kbt])
            k.dma("sp", QBs[:], QB.rearrange("(c p) t -> p c t", p=128), [dtok["QB"]], [qbt])
            for h in range(4):
                k.dma("sp", VB4[:, :, h, 0:64],
                      VAB.rearrange("(t p) c -> p t c", p=128)[:, :, 256 + h * 64:256 + (h + 1) * 64],
                      [dtok["VAB"]], [vbt])
            k.memset("pool", VB4[:, :, :, 64:65], 1.0, [vbt])
            k.dma("sp", WIs[:], WI.rearrange("(t p) c -> p t c", p=128), [dtok["WI"]], [wit])
            k.dma("sp", P2[:], pow2, [], [p2t])
            scr = Ring([Slot(sbuf(f"sc{i}", [128, S], F32, ph)) for i in range(2)])
            rlr = Ring([Slot(sbuf(f"rl{i}", [128, 512], F32, ph)) for i in range(3)])
            mkr = Ring([Slot(sbuf(f"mk{i}", [128, S], BF16, ph)) for i in range(2)])
            mTall = Ring([Slot(sbuf(f"mTa{i}", [128, S], BF16, ph)) for i in range(2)])
            Er = Ring([Slot(sbuf(f"Eb{i}", [128, 512], BF16, ph)) for i in range(4)])
            Emr = Ring([Slot(sbuf(f"Emb{i}", [128, 512], BF16, ph)) for i in range(4)])
            junk = sbuf("junk", [128, S], BF16, ph); jt = Tok()
            smr = Ring([Slot(sbuf(f"sm{i}", [128, 8 + NBIS + 1], F32, ph)) for i in range(2)])
            rdr = Ring([Slot(sbuf(f"rdb{i}", [128, 4, 1], F32, ph)) for i in range(2)])
            ytr = Ring([Slot(sbuf(f"ytb{i}", [128, 256], BF16, ph)) for i in range(2)])
            yor = Ring([Slot(sbuf(f"yob{i}", [128, 256], BF16, ph)) for i in range(2)])
            dbk = Ring([banks[0], banks[1]])
            sbk = Ring([banks[2], banks[3], banks[4]])
            accb = Ring([banks[5]])
            pst_i = [0]

            def prep(t):
                nk = 128 * (t + 1)
                SC = scr.next()
                nblk = (nk + 511) // 512
                for kb_ in range(nblk):
                    w = min(512, nk - kb_ * 512)
                    cs_ = slice(kb_ * 512, kb_ * 512 + w)
                    for h in range(4):
                        ch, pb = h // 2, (h % 2) * 64
                        b = dbk.next()
                        k.mm(b.ap(0, 128, 0, w), QIs[pb:pb + 64, ch, t * 128:(t + 1) * 128], KI2[pb:pb + 64, cs_],
                             True, True, [qit, kit], [b.tok])
                        wsc = WIs[:, t, h:h + 1]
                        if h == 0:
                            k.ts("dve", SC.t[:, cs_], b.ap(0, 128, 0, w), 0.0, wsc, ALU.max, ALU.mult,
                                 [b.tok, wit], [SC.tok])
                        else:
                            RL = rlr.next()
                            k.act(RL.t[:, 0:w], b.ap(0, 128, 0, w), AF.Relu, [b.tok], [RL.tok])
                            k.act(RL.t[:, 0:w], RL.t[:, 0:w], AF.Copy, [RL.tok, wit], [RL.tok], scale=wsc)
                            k.tt("pool", SC.t[:, cs_], SC.t[:, cs_], RL.t[:, 0:w], ALU.add,
                                 [RL.tok, SC.tok], [SC.tok])
                SM = smr.next()
                mx, mn, rng, mid, cnt, dd, thr = (SM.t[:, i:i + 1] for i in range(7))
                steps = SM.t[:, 8:8 + NBIS + 1]
                bis = t >= 2 and not OPT.get('b2_nobis')
                if bis:
                    k.op("dve", lambda e: e.tensor_reduce(out=mx, in_=SC.t[:, 0:nk], axis=AX.X, op=ALU.max),
                         [SC.tok], [SM.tok])
                k.op("dve", lambda e: e.tensor_reduce(out=mn, in_=SC.t[:, 0:nk], axis=AX.X, op=ALU.min),
                     [SC.tok], [SM.tok])
                k.memset("dve", SC.t[0:64, nk - 64:nk], -1.0e30, [SC.tok])
                if bis:
                    k.tt("dve", rng, mx, mn, ALU.subtract, [SM.tok], [SM.tok])
                    k.ts("dve", steps, P2[:], rng, None, ALU.mult, None, [p2t, SM.tok], [SM.tok])
                    k.tt("dve", mid, mn, steps[:, 0:1], ALU.add, [SM.tok], [SM.tok])
                    for it in range(NBIS):
                        k.ts("dve", junk[:, 0:nk], SC.t[:, 0:nk], mid, 0.0, ALU.is_ge, ALU.add,
                             [SC.tok, SM.tok], [jt, SM.tok], accum_out=cnt)
                        k.ts("dve", dd, cnt, 255.5, 0.5, ALU.is_ge, ALU.subtract, [SM.tok], [SM.tok])
                        k.stt("dve", mid, dd, steps[:, it:it + 1], mid, ALU.mult, ALU.add, [SM.tok], [SM.tok])
                    k.tt("dve", thr, mid, steps[:, NBIS:NBIS + 1], ALU.subtract, [SM.tok], [SM.tok])
                else:
                    k.copy("dve", thr, mn, [SM.tok], [SM.tok])
                MK = mkr.next()
                k.ts("dve", MK.t[:, 0:nk], SC.t[:, 0:nk], thr, None, ALU.is_ge, None, [SC.tok, SM.tok], [MK.tok])
                return MK

            def prep_b(t, MK):
                ngrp = (t + 1 + 3) // 4
                MT = mTall.next()
                for g in range(ngrp):
                    jts = list(range(g * 4, min(t + 1, g * 4 + 4)))
                    half = pst_i[0] % 2
                    pst_i[0] += 1
                    for jj, jt_ in enumerate(jts):
                        k.tr(PSTs[half][:, jj * 128:(jj + 1) * 128],
                             MK.t[:, jt_ * 128:(jt_ + 1) * 128], IDENT, [MK.tok, cmt], [pstoks[half]])
                    n = len(jts) * 128
                    k.act(MT.t[:, g * 512:g * 512 + n], PSTs[half][:, 0:n], AF.Copy,
                          [pstoks[half]], [MT.tok])
                return MT

            def b_stage(t, h, g, MT):
                ch, pb = h // 2, (h % 2) * 64
                jts = list(range(g * 4, min(t + 1, g * 4 + 4)))
                n = len(jts) * 128
                b = sbk.next()
                for jj, jt_ in enumerate(jts):
                    k.mm(b.ap(0, 128, jj * 128, (jj + 1) * 128), KBs[pb:pb + 64, ch, jt_ * 128:(jt_ + 1) * 128],
                         QBs[pb:pb + 64, ch, t * 128:(t + 1) * 128], True, True, [kbt, qbt], [b.tok])
                E = Er.next(); Em = Emr.next()
                k.act(E.t[:, 0:n], b.ap(0, 128, 0, n), AF.Exp, [b.tok], [E.tok], scale=0.125)
                k.tt("pool", Em.t[:, 0:n], E.t[:, 0:n], MT.t[:, g * 512:g * 512 + n], ALU.mult,
                     [E.tok, MT.tok], [Em.tok])
                return Em, jts

            ntile = OPT.get('b2_tmax', NT)
            MTs = {0: prep_b(0, prep(0))}
            for t in range(ntile):
                MKn = prep(t + 1) if t + 1 < ntile else None
                if OPT.get('b2_noattn'):
                    continue
                MT = MTs.pop(t)
                ngrp = (t + 1 + 3) // 4
                items = [(h, g) for h in range(4) for g in range(ngrp)]
                pend = [b_stage(t, *it_, MT) for it_ in items[0:2]]
                acc = accb.next()
                for i, (h, g) in enumerate(items):
                    if i + 2 < len(items):
                        pend.append(b_stage(t, *items[i + 2], MT))
                    Em, jts = pend.pop(0)
                    for jj, jt_ in enumerate(jts):
                        k.mm(acc.ap(0, 128, h * 65, h * 65 + 65), Em.t[:, jj * 128:(jj + 1) * 128], VB4[:, jt_, h, :],
                             jt_ == 0, jt_ == t, [vbt, Em.tok], [acc.tok])
                if MKn is not None:
                    MTs[t + 1] = prep_b(t + 1, MKn)
                finalize_attn(t, acc, rdr, ytr, yor, 256, YMt[1], pst_i)
            k.flush()
            if stop == 'B2':
                raise _Stop(nc, k, es)

        with ExitStack() as ph:
            wg = sbuf("wg", [128, 8, 3072], BF16, ph); wgt = Tok()
            wb = sbuf("wb", [128, 8, 1024], BF16, ph); wbt = Tok()
            wo = sbuf("wo", [128, 8, 1024], BF16, ph); wot = Tok()
            for c in range(8):
                k.dma("pool", wg[:, c, :], w_in[l, c * 128:(c + 1) * 128, C_GT:C_GT + 3072], [], [wgt])
            k.dma("pool", wb[:], wview(w_br[l], 0, 1024), [], [wbt])
            k.dma("pool", wo[:], wview(w_out[l], 0, 1024), [], [wot])
            X = Slot(sbuf("xc", [128, 8, 512], F32, ph))
            sq = sbuf("sqc", [128, 8, 512], BF16, ph); sqt = Tok()
            sd = sbuf("sdc", [128, 512], F32, ph); sdt = Tok()
            rstd = sbuf("rstdc", [128, 512], F32, ph); rst = Tok()
            hTb = sbuf("hTb", [128, 8, 512], BF16, ph); hbt = Tok()
            ym = sbuf("ymc", [128, 8, 512], BF16, ph); ymt = Tok()
            mg = sbuf("mg", [128, 8, 512], BF16, ph); mgt = [Tok() for _ in range(8)]
            sgr = Ring([Slot(sbuf(f"sg{i}", [128, 512], F32, ph)) for i in range(3)])
            tmr = Ring([Slot(sbuf(f"tm{i}", [128, 512], F32, ph)) for i in range(3)])
            acr = Ring([Slot(sbuf(f"ac{i}", [128, 512], F32, ph)) for i in range(2)])
            xo = Ring([Slot(sbuf(f"xo{i}", [128, 512], F32, ph)) for i in range(3)])
            bk = Ring(banks)
            KR = [(0, 2), (2, 4), (4, 8)]
            for tb in range(NB):
                k.dma("sp", X.t[:], xview(xsrc, tb), [xsrct], [X.tok])
                k.dma("sp", ym[:], xview(YM, tb), YMt, [ymt])
                make_hT(X.t, X.tok, SP_GMIX, sq, sqt, sd, sdt, rstd, rst, lambda c: hTb[:, c, :], hbt, bk.next())
                for dc in range(8):
                    AC = acr.next()
                    for br in range(3):
                        bg = bk.next()
                        for c in range(8):
                            k.mm(bg.ap(), wg[:, c, br * 1024 + dc * 128:br * 1024 + (dc + 1) * 128], hTb[:, c, :],
                                 c == 0, c == 7, [wgt, hbt], [bg.tok])
                        SG = sgr.next()
                        bcol = SP_BG + br * 8 + dc
                        k.act(SG.t[:], bg.ap(), AF.Sigmoid, [bg.tok, sptok], [SG.tok], bias=spt[:, bcol:bcol + 1])
                        bb_ = bk.next()
                        k0, k1 = KR[br]
                        for c in range(k0, k1):
                            k.mm(bb_.ap(), wb[:, c, dc * 128:(dc + 1) * 128], ym[:, c, :], c == k0, c == k1 - 1,
                                 [wbt, ymt], [bb_.tok])
                        if br == 0:
                            k.tt("dve", AC.t[:], SG.t[:], bb_.ap(), ALU.mult, [SG.tok, bb_.tok], [AC.tok])
                        else:
                            TM = tmr.next()
                            k.tt("dve", TM.t[:], SG.t[:], bb_.ap(), ALU.mult, [SG.tok, bb_.tok], [TM.tok])
                            if br == 1:
                                k.tt("pool", AC.t[:], AC.t[:], TM.t[:], ALU.add, [AC.tok, TM.tok], [AC.tok])
                            else:
                                k.tt("pool", mg[:, dc, :], AC.t[:], TM.t[:], ALU.add, [AC.tok, TM.tok], [mgt[dc]])
                for oc in range(8):
                    bo = bk.next()
                    for c in range(8):
                        k.mm(bo.ap(), wo[:, c, oc * 128:(oc + 1) * 128], mg[:, c, :], c == 0, c == 7,
                             [wot, mgt[c]], [bo.tok])
                    XO = xo.next()
                    k.tt("dve", XO.t[:], X.t[:, oc, :], bo.ap(), ALU.add, [X.tok, bo.tok], [XO.tok])
                    k.dma("sp", XT[oc * 128:(oc + 1) * 128, tb * 512:(tb + 1) * 512], XO.t[:], [XO.tok], [dtok["XT"]])
            k.flush()
            if stop == 'C':
                raise _Stop(nc, k, es)

        with ExitStack() as ph:
            w1 = sbuf("w1", [128, 8, 2 * DFF], BF16, ph); w1t = Tok()
            w2 = sbuf("w2", [128, 22, 1024], BF16, ph); w2t = Tok()
            for c in range(8):
                k.dma("pool", w1[:, c, :], w_f1[l, c * 128:(c + 1) * 128, :], [], [w1t])
            for c in range(22):
                k.dma("pool", w2[:, c, :], w_f2[l, c * 128:(c + 1) * 128, :], [], [w2t])
            X = Slot(sbuf("xd", [128, 8, 512], F32, ph))
            sq = sbuf("sqd", [128, 8, 512], BF16, ph); sqt = Tok()
            sd = sbuf("sdd", [128, 512], F32, ph); sdt = Tok()
            rstd = sbuf("rstdd", [128, 512], F32, ph); rst = Tok()
            hTb = sbuf("hTd", [128, 8, 512], BF16, ph); hbt = Tok()
            av = sbuf("av", [128, 22, 512], BF16, ph); avt = [Tok() for _ in range(22)]
            sgr = Ring([Slot(sbuf(f"sl{i}", [128, 512], F32, ph)) for i in range(3)])
            xo = Ring([Slot(sbuf(f"xod{i}", [128, 512], F32, ph)) for i in range(3)])
            bk = Ring(banks)
            xdst, xdstt = (yT, dtok["yT"]) if last else (XT, dtok["XT"])
            for tb in range(NB):
                k.dma("sp", X.t[:], xview(XT, tb), [dtok["XT"]], [X.tok])
                make_hT(X.t, X.tok, SP_GFFN, sq, sqt, sd, sdt, rstd, rst, lambda c: hTb[:, c, :], hbt, bk.next())
                for fc in range(22):
                    bg = bk.next()
                    for c in range(8):
                        k.mm(bg.ap(), w1[:, c, fc * 128:(fc + 1) * 128], hTb[:, c, :], c == 0, c == 7,
                             [w1t, hbt], [bg.tok])
                    bu = bk.next()
                    for c in range(8):
                        k.mm(bu.ap(), w1[:, c, DFF + fc * 128:DFF + (fc + 1) * 128], hTb[:, c, :], c == 0, c == 7,
                             [w1t, hbt], [bu.tok])
                    SG = sgr.next()
                    k.act(SG.t[:], bg.ap(), AF.Silu, [bg.tok], [SG.tok])
                    k.tt("dve", av[:, fc, :], SG.t[:], bu.ap(), ALU.mult, [SG.tok, bu.tok], [avt[fc]])
                for oc in range(8):
                    bo = bk.next()
                    for c in range(22):
                        k.mm(bo.ap(), w2[:, c, oc * 128:(oc + 1) * 128], av[:, c, :], c == 0, c == 21,
                             [w2t, avt[c]], [bo.tok])
                    XO = xo.next()
                    k.tt("dve", XO.t[:], X.t[:, oc, :], bo.ap(), ALU.add, [X.tok, bo.tok], [XO.tok])
                    k.dma("sp", xdst[oc * 128:(oc + 1) * 128, tb * 512:(tb + 1) * 512], XO.t[:], [XO.tok], [xdstt])
            if last:
                k.wait_all_dma()
            k.flush()
            if stop == 'D':
                raise _Stop(nc, k, es)
    es.close()
    return nc, k


def host_consts():
    pos = np.arange(S, dtype=np.float32)
    inv = (1.0 / (np.float32(10000.0) ** (np.arange(0, 64, 2, dtype=np.float32) / np.float32(64)))).astype(np.float32)
    ang = pos[:, None] * inv[None, :]
    ang = np.concatenate([ang, ang], axis=-1)
    cosT = np.cos(ang).astype(np.float32).T
    sinT = np.sin(ang).astype(np.float32).T
    cs = np.zeros((128, 2, S), np.float32)
    cs[0:64, 0], cs[64:128, 0] = cosT, cosT
    cs[0:64, 1], cs[64:128, 1] = sinT, sinT
    ones = np.ones((128, 128), np.float32)
    onesblk = np.zeros((128, 128), np.float32)
    onesblk[0:64, 0:64] = 1.0
    onesblk[64:128, 64:128] = 1.0
    rotm = np.zeros((128, 128), np.float32)
    for hb in (0, 64):
        for m in range(32):
            rotm[hb + m + 32, hb + m] = -1.0
            rotm[hb + m, hb + m + 32] = 1.0
    ident = np.eye(128, dtype=np.float32)
    cmat = np.stack([ones, onesblk, rotm, ident]).astype(np.float32)
    kk = np.arange(128)[:, None, None]
    jj = np.arange(5)[None, :, None]
    qq = np.arange(128)[None, None, :]
    cq = (qq >= 64).astype(np.int64)
    ck = 2 * jj - 8 + (kk >= 64)
    amask = ((ck >= cq - 8) & (ck <= cq)).astype(np.float32)
    relidx = np.clip(128 * (4 - jj) + qq - kk, -128, 128) + 128
    pow2 = np.tile((2.0 ** -(np.arange(NBIS + 1) + 1.0)).astype(np.float32)[None, :], (128, 1))
    return cs, cmat, amask, relidx, pow2


def host_pack(inp):
    cs, cmat, amask, relidx, pow2 = host_consts()
    f = lambda a: np.ascontiguousarray(np.asarray(a, dtype=np.float32))
    spar = np.zeros((L, 128, NSP), np.float32)
    p = np.arange(128)
    for l in range(L):
        spar[l, :, SP_GMIX:SP_GMIX + 8] = f(inp["g_mix"])[l].reshape(8, 128).T
        spar[l, :, SP_GFFN:SP_GFFN + 8] = f(inp["g_ffn"])[l].reshape(8, 128).T
        spar[l, :, SP_BG:SP_BG + 24] = f(inp["b_gate"])[l].reshape(24, 128).T
        spar[l, :, SP_GAQ] = f(inp["qk_gain_a"])[l, 0][p % 64]
        spar[l, :, SP_GAK] = f(inp["qk_gain_a"])[l, 1][p % 64]
        spar[l, :, SP_GBQ] = f(inp["qk_gain_b"])[l, 0][p % 64]
        spar[l, :, SP_GBK] = f(inp["qk_gain_b"])[l, 1][p % 64]
        spar[l, :, SP_GIK] = f(inp["g_idx_k"])[l][p % 64]
        cw = f(inp["conv_w"])[l]
        for j in range(4):
            spar[l, :, SP_CW + j * 4:SP_CW + j * 4 + 4] = cw[j].reshape(4, 128).T
        spar[l, :, SP_CB:SP_CB + 4] = f(inp["conv_b"])[l].reshape(4, 128).T
        spar[l, :, SP_BA:SP_BA + 4] = f(inp["lru_ba"])[l].reshape(4, 128).T
        spar[l, :, SP_BX:SP_BX + 4] = f(inp["lru_bx"])[l].reshape(4, 128).T
        spar[l, :, SP_LAM:SP_LAM + 4] = f(inp["lru_lambda"])[l].reshape(4, 128).T
    lrubd = np.zeros((L, 2, 4, 128, 128), np.float32)
    for m, nm in enumerate(("lru_wa", "lru_wx")):
        wsrc = f(inp[nm])
        for cc in range(4):
            lrubd[:, m, cc, 0:64, 0:64] = wsrc[:, 2 * cc]
            lrubd[:, m, cc, 64:128, 64:128] = wsrc[:, 2 * cc + 1]
    rb = f(inp["rel_bias"])
    ab = rb[:, :, relidx]
    abias = np.ascontiguousarray(ab.transpose(0, 2, 1, 3, 4))
    shared = {"w_in": f(inp["w_in"]), "w_branch": f(inp["w_branch"]), "w_out": f(inp["w_out"]),
              "w_ffn_in": f(inp["w_ffn_in"]), "w_ffn_out": f(inp["w_ffn_out"]),
              "spar": spar, "lrubd": lrubd, "abias": abias, "cs": cs, "cmat": cmat,
              "amask": amask, "pow2": pow2}
    return shared


_CACHE = {}


def kernel(**inputs):
    x = np.asarray(inputs["x"], dtype=np.float32)
    shared = host_pack(inputs)
    if "nc" not in _CACHE:
        _CACHE["nc"] = build()[0]
    nc = _CACHE["nc"]
    in_maps = []
    for b in range(8):
        m = dict(shared)
        m["xT"] = np.ascontiguousarray(x[b].T)
        in_maps.append(m)
    res = run_bass_kernel_spmd(nc, in_maps, core_ids=list(range(8)))
    out = np.stack([np.ascontiguousarray(r["yT"].T) for r in res.results], axis=0)
    return out.astype(np.float32)
```

```python
import math
from contextlib import ExitStack
import numpy as np
import concourse.bass as bass
import concourse.mybir as mybir
from concourse.bass_utils import run_bass_kernel_spmd

F32 = mybir.dt.float32
BF16 = mybir.dt.bfloat16
AF = mybir.ActivationFunctionType
ALU = mybir.AluOpType
AX = mybir.AxisListType

D = 1024; S = 4096; L = 4; DIN = 5956; DFF = 2816
NT = S // 128; NB = S // 512
EPS = 1e-6
NBIS = 14
C_AQ, C_AK, C_AV, C_BQ, C_BK, C_BV, C_IQ, C_IK, C_IW, C_CX, C_CY, C_GT = (
    0, 256, 512, 768, 1024, 1280, 1536, 1792, 1856, 1860, 2372, 2884)
SP_GMIX, SP_GFFN, SP_BG, SP_GAQ, SP_GAK, SP_GBQ, SP_GBK, SP_GIK, SP_CW, SP_CB, SP_BA, SP_BX, SP_LAM = (
    0, 8, 16, 40, 41, 42, 43, 44, 45, 61, 65, 69, 73)
NSP = 77


class Tok:
    __slots__ = ("w", "r")

    def __init__(self):
        self.w = None
        self.r = {}


class Ring:
    def __init__(self, items):
        self.items = items
        self.i = -1

    def next(self):
        self.i = (self.i + 1) % len(self.items)
        return self.items[self.i]


class Slot:
    def __init__(self, t):
        self.t = t
        self.tok = Tok()


class K:
    INC = {"pe": 1, "act": 1, "dve": 1, "pool": 1}
    NDMA = {"sp": 12, "pool": 6}

    def __init__(self, nc, es):
        self.nc = nc
        self.sem = {f"{n}@{l}": es.enter_context(nc.semaphore(f"s_{n}_{l}")) for n in self.INC for l in range(L)}
        for q, n in self.NDMA.items():
            for r in range(n):
                self.sem[f"dma_{q}{r}"] = es.enter_context(nc.semaphore(f"s_dma_{q}{r}"))
        self.cnt = {n: 0 for n in self.sem}
        self.ep = 0
        self.dma_rr = {q: 0 for q in self.NDMA}
        self.streams = {e: [] for e in ("pe", "act", "dve", "pool", "sp")}
        self.waited = {e: {} for e in self.streams}
        self.ninstr = 0

    def op(self, stream, fn, reads=(), writes=(), counter=None):
        if counter is None:
            counter = f"{stream}@{self.ep}"
        deps = {}
        if counter[:4] == "dma_" and self.cnt[counter] > 0:
            deps[counter] = self.cnt[counter]
        for t in reads:
            if t.w is not None and deps.get(t.w[0], 0) < t.w[1]:
                deps[t.w[0]] = t.w[1]
        for t in writes:
            if t.w is not None and deps.get(t.w[0], 0) < t.w[1]:
                deps[t.w[0]] = t.w[1]
            for c, s in t.r.items():
                if deps.get(c, 0) < s:
                    deps[c] = s
        wd = self.waited[stream]
        waits = []
        for c, s in deps.items():
            if c[:3] == "pe@" and counter[:3] == "pe@":
                continue
            if wd.get(c, 0) >= s:
                continue
            wd[c] = s
            waits.append((c, s))
        self.cnt[counter] += 1
        seq = self.cnt[counter]
        for t in reads:
            if t.r.get(counter, 0) < seq:
                t.r[counter] = seq
        for t in writes:
            t.w = (counter, seq)
            t.r = {}
        self.streams[stream].append((waits, fn, counter))
        self.ninstr += 1

    def wait_all_dma(self):
        waits = [(c, self.cnt[c]) for c in self.cnt if c[:4] == "dma_" and self.cnt[c] > 0]
        self.streams["sp"].append((waits, None, None))

    def flush(self):
        self.wait_all_dma()
        nc = self.nc
        streams = self.streams
        self.streams = {e: [] for e in streams}
        sem, INC = self.sem, self.INC

        def mk(lst):
            def f(eng):
                for waits, fn, counter in lst:
                    for c, s in waits:
                        eng.wait_ge(sem[c], s * (16 if c[:4] == "dma_" else 1))
                    if fn is not None:
                        fn(eng).then_inc(sem[counter], 16 if counter[:4] == "dma_" else 1)
            return f

        with nc.Block() as block:
            block.tensor(mk(streams["pe"]))
            block.scalar(mk(streams["act"]))
            block.vector(mk(streams["dve"]))
            block.gpsimd(mk(streams["pool"]))
            block.sync(mk(streams["sp"]))

    def dma(self, q, out, in_, reads, writes):
        r = self.dma_rr[q] % self.NDMA[q]
        self.dma_rr[q] += 1
        self.op(q, lambda e: e.dma_start(out=out, in_=in_), reads, writes, counter=f"dma_{q}{r}")

    def mm(self, out, lhsT, rhs, start, stop, reads, writes):
        self.op("pe", lambda e: e.matmul(out, lhsT, rhs, start=start, stop=stop), reads, writes)

    def tr(self, out, in_, ident, reads, writes):
        self.op("pe", lambda e: e.transpose(out, in_, ident), reads, writes)

    def act(self, out, in_, func, reads, writes, bias=None, scale=None):
        kw = {}
        if bias is not None:
            kw["bias"] = bias
        if scale is not None:
            kw["scale"] = scale
        self.op("act", lambda e: e.activation(out=out, in_=in_, func=func, **kw), reads, writes)

    def tt(self, eng, out, in0, in1, op, reads, writes):
        self.op(eng, lambda e: e.tensor_tensor(out=out, in0=in0, in1=in1, op=op), reads, writes)

    def ts(self, eng, out, in0, s1, s2, op0, op1, reads, writes, accum_out=None):
        if op1 is None:
            self.op(eng, lambda e: e.tensor_scalar(out=out, in0=in0, scalar1=s1, scalar2=None, op0=op0),
                    reads, writes)
        elif accum_out is None:
            self.op(eng, lambda e: e.tensor_scalar(out=out, in0=in0, scalar1=s1, scalar2=s2, op0=op0, op1=op1),
                    reads, writes)
        else:
            self.op(eng, lambda e: e.tensor_scalar(out=out, in0=in0, scalar1=s1, scalar2=s2, op0=op0, op1=op1,
                                                   accum_out=accum_out), reads, writes)

    def stt(self, eng, out, in0, scalar, in1, op0, op1, reads, writes):
        self.op(eng, lambda e: e.scalar_tensor_tensor(out=out, in0=in0, scalar=scalar, in1=in1, op0=op0, op1=op1),
                reads, writes)

    def recip(self, out, in_, reads, writes):
        self.op("dve", lambda e: e.reciprocal(out=out, in_=in_), reads, writes)

    def memset(self, eng, ap, val, writes):
        self.op(eng, lambda e: e.memset(ap, val), (), writes)

    def copy(self, eng, out, in_, reads, writes):
        self.op(eng, lambda e: e.tensor_copy(out=out, in_=in_), reads, writes)


OPT = {}


class _Stop(Exception):
    pass


def build(nlayers=L, dbg=False, stop=None):
    try:
        return _build(nlayers, dbg, stop)
    except _Stop as e:
        nc, k, es = e.args
        k.wait_all_dma()
        k.flush()
        return nc, k


def _build(nlayers, dbg, stop):
    nc = bass.Bass("TRN2", target_bir_lowering=False)
    es = ExitStack()

    def din(name, shape, dt=F32):
        return nc.dram_tensor(name, list(shape), dt, kind="ExternalInput").ap()

    kind_dbg = "ExternalOutput" if dbg else "Internal"

    def dscr(name, shape, dt):
        return nc.dram_tensor(name, list(shape), dt, kind=kind_dbg).ap()

    xT_in = din("xT", [D, S])
    w_in = din("w_in", [L, D, DIN])
    w_br = din("w_branch", [L, D, D])
    w_out = din("w_out", [L, D, D])
    w_f1 = din("w_ffn_in", [L, D, 2 * DFF])
    w_f2 = din("w_ffn_out", [L, DFF, D])
    spar = din("spar", [L, 128, NSP])
    lrubd = din("lrubd", [L, 2, 4, 128, 128])
    abias = din("abias", [L, 128, 4, 5, 128])
    cs_d = din("cs", [128, 2, S])
    cmat = din("cmat", [4, 128, 128])
    amask = din("amask", [128, 5, 128])
    pow2 = din("pow2", [128, NBIS + 1])
    yT = nc.dram_tensor("yT", [D, S], F32, kind="ExternalOutput").ap()

    XT = dscr("XT", [D, S], F32)
    QA = dscr("QA", [256, S], BF16)
    KA = dscr("KA", [256, S], BF16)
    QB = dscr("QB", [256, S], BF16)
    KB = dscr("KB", [256, S], BF16)
    QI = dscr("QI", [256, S], BF16)
    KI = dscr("KI", [64, S], BF16)
    VAB = dscr("VAB", [S, 512], BF16)
    WI = dscr("WI", [S, 4], F32)
    YM = dscr("YM", [D, S], BF16)
    dtok = {n: Tok() for n in ("XT", "QA", "KA", "QB", "KB", "QI", "KI", "VAB", "WI", "YM", "xin", "yT")}
    YMt = [Tok() for _ in range(3)]

    k = K(nc, es)

    PS2 = [es.enter_context(nc.psum_tensor(f"ps2_{i}", [128, 1024], F32)) for i in range(3)]
    PSTs = [es.enter_context(nc.psum_tensor(f"pst{i}", [128, 1024], BF16)) for i in range(2)]

    class Bank:
        def __init__(self, t, c0):
            self.t, self.c0, self.tok = t, c0, Tok()

        def ap(self, p0=0, p1=128, a=0, b=512):
            return self.t[p0:p1, self.c0 + a:self.c0 + b]

    banks = []
    for t in PS2:
        banks.append(Bank(t, 0))
        banks.append(Bank(t, 512))
    pstoks = [Tok(), Tok()]

    uid = [0]

    def sbuf(name, shape, dt, stack=es):
        uid[0] += 1
        return stack.enter_context(nc.sbuf_tensor(f"{name}_{uid[0]}", list(shape), dt))

    cm = sbuf("cm", [128, 4, 128], BF16)
    cmt = Tok()
    epsc = sbuf("epsc", [128, 1], F32)
    epst = Tok()
    spt = sbuf("spt", [128, NSP], F32)
    sptok = Tok()
    clam = sbuf("clam", [128, 4], F32)
    clamtok = Tok()
    k.dma("pool", cm[:], cmat.rearrange("m p n -> p m n"), [], [cmt])
    k.memset("dve", epsc[:], EPS, [epst])
    ONES, ONESBLK, ROTM, IDENT = (cm[:, i, :] for i in range(4))

    def xview(ap2d, tb):
        return ap2d.rearrange("(c p) t -> p c t", p=128)[:, :, tb * 512:(tb + 1) * 512]

    def wview(w2d, c0, n):
        return w2d.rearrange("(c p) n -> p c n", p=128)[:, :, c0:c0 + n]

    def make_hT(X, Xtok, gcol, sq, sqtok, sd, sdtok, rstd, rstok, hdst, htok, bank):
        k.act(sq[:], X[:], AF.Square, [Xtok], [sqtok])
        for c in range(8):
            k.mm(bank.ap(), ONES, sq[:, c, :], c == 0, c == 7, [sqtok, cmt], [bank.tok])
        k.act(sd[:], bank.ap(), AF.Sqrt, [bank.tok, epst], [sdtok], bias=epsc[:, 0:1], scale=1.0 / D)
        k.recip(rstd[:], sd[:], [sdtok], [rstok])
        for c in range(8):
            k.stt("dve", hdst(c), X[:, c, :], spt[:, gcol + c:gcol + c + 1], rstd[:],
                  ALU.mult, ALU.mult, [Xtok, sptok, rstok], [htok])

    for l in range(nlayers):
        xsrc, xsrct = (xT_in, dtok["xin"]) if l == 0 else (XT, dtok["XT"])
        last = l == nlayers - 1
        k.ep = l
        k.dma("sp", spt[:], spar[l], [], [sptok])

        with ExitStack() as ph:
            hT = sbuf("hT", [128, 8, S], BF16, ph); hTt = Tok()
            with ExitStack() as ph1:
                xs = Ring([Slot(sbuf(f"xs{i}", [128, 8, 512], F32, ph1)) for i in range(2)])
                sq = sbuf("sq", [128, 8, 512], BF16, ph1); sqt = Tok()
                sd = sbuf("sd", [128, 512], F32, ph1); sdt = Tok()
                rstd = sbuf("rstd", [128, 512], F32, ph1); rst = Tok()
                for tb in range(NB):
                    X = xs.next()
                    k.dma("sp", X.t[:], xview(xsrc, tb), [xsrct], [X.tok])
                    make_hT(X.t, X.tok, SP_GMIX, sq, sqt, sd, sdt, rstd, rst,
                            lambda c, tb=tb: hT[:, c, tb * 512:(tb + 1) * 512], hTt, banks[tb % 6])
                k.flush()
                if stop == 'A1':
                    raise _Stop(nc, k, es)

            wr = Ring([Slot(sbuf(f"wr{i}", [128, 8, 128], BF16, ph)) for i in range(3)])
            ob = Ring([Slot(sbuf(f"ob{i}", [128, S], BF16, ph)) for i in range(2)])
            csr = Ring([Slot(sbuf(f"csr{i}", [128, 2, 512], F32, ph)) for i in range(3)])
            sqb = Ring([Slot(sbuf(f"sqb{i}", [128, 512], BF16, ph)) for i in range(3)])
            sd2 = Ring([Slot(sbuf(f"sd2{i}", [128, 512], F32, ph)) for i in range(3)])
            rs2 = Ring([Slot(sbuf(f"rs2{i}", [128, 512], F32, ph)) for i in range(3)])
            qnb = Ring([Slot(sbuf(f"qnb{i}", [128, 512], BF16, ph)) for i in range(3)])
            t1r = Ring([Slot(sbuf(f"t1r{i}", [128, 512], F32, ph)) for i in range(3)])
            t2r = Ring([Slot(sbuf(f"t2r{i}", [128, 512], F32, ph)) for i in range(3)])
            bk = Ring(banks)

            def load_w(c0, m):
                W = wr.next()
                k.dma("pool", W.t[:, :, 0:m], wview(w_in[l], c0, m), [], [W.tok])
                return W

            def proj_mm(W, m, tb):
                b = bk.next()
                for c in range(8):
                    k.mm(b.ap(0, m), W.t[:, c, 0:m], hT[:, c, tb * 512:(tb + 1) * 512], c == 0, c == 7,
                         [W.tok, hTt], [b.tok])
                return b

            chunks = []
            for ch in range(2):
                chunks.append((C_AQ + ch * 128, 128, QA, dtok["QA"], ch * 128, SP_GAQ, True, False))
                chunks.append((C_AK + ch * 128, 128, KA, dtok["KA"], ch * 128, SP_GAK, True, False))
                chunks.append((C_BQ + ch * 128, 128, QB, dtok["QB"], ch * 128, SP_GBQ, True, True))
                chunks.append((C_BK + ch * 128, 128, KB, dtok["KB"], ch * 128, SP_GBK, True, True))
                chunks.append((C_IQ + ch * 128, 128, QI, dtok["QI"], ch * 128, 0, False, True))
            chunks.append((C_IK, 64, KI, dtok["KI"], 0, SP_GIK, True, True))
            items = [(ci, tb) for ci in range(len(chunks)) for tb in range(NB)]
            ist, cst = {}, {}

            def rope_start(st, m, tb):
                QN = st["QN"]
                CS = csr.next()
                k.dma("sp", CS.t[:], cs_d[:, :, tb * 512:(tb + 1) * 512], [], [CS.tok])
                b3 = bk.next()
                k.mm(b3.ap(0, m), cm[0:m, 2, 0:m], QN.t[0:m, :], True, True, [QN.tok, cmt], [b3.tok])
                st["CS"], st["b3"] = CS, b3

            def S1(i):
                ci, tb = items[i]
                c0, m = chunks[ci][0:2]
                if tb == 0:
                    cst[ci] = (load_w(c0, m), ob.next())
                ist[i] = {"b": proj_mm(cst[ci][0], m, tb)}

            def S2(i):
                ci, tb = items[i]
                c0, m, dst, dstt, row0, gcol, norm, rope = chunks[ci]
                st = ist[i]
                b = st["b"]
                if norm:
                    SQ = sqb.next()
                    k.act(SQ.t[0:m, :], b.ap(0, m), AF.Square, [b.tok], [SQ.tok])
                    b2 = bk.next()
                    k.mm(b2.ap(0, m), cm[0:m, 1, 0:m], SQ.t[0:m, :], True, True, [SQ.tok, cmt], [b2.tok])
                    st["b2"] = b2
                else:
                    QN = qnb.next()
                    k.act(QN.t[0:m, :], b.ap(0, m), AF.Copy, [b.tok], [QN.tok])
                    st["QN"] = QN
                    rope_start(st, m, tb)

            def S3(i):
                ci, tb = items[i]
                c0, m, dst, dstt, row0, gcol, norm, rope = chunks[ci]
                st = ist[i]
                if not norm:
                    return
                b, b2 = st["b"], st["b2"]
                O = cst[ci][1]
                SD = sd2.next(); RS = rs2.next()
                k.act(SD.t[0:m, :], b2.ap(0, m), AF.Sqrt, [b2.tok, epst], [SD.tok],
                      bias=epsc[0:m, 0:1], scale=1.0 / 64)
                k.recip(RS.t[0:m, :], SD.t[0:m, :], [SD.tok], [RS.tok])
                if not rope:
                    k.stt("dve", O.t[0:m, tb * 512:(tb + 1) * 512], b.ap(0, m), spt[0:m, gcol:gcol + 1], RS.t[0:m, :],
                          ALU.mult, ALU.mult, [b.tok, sptok, RS.tok], [O.tok])
                    return
                QN = qnb.next()
                k.stt("dve", QN.t[0:m, :], b.ap(0, m), spt[0:m, gcol:gcol + 1], RS.t[0:m, :],
                      ALU.mult, ALU.mult, [b.tok, sptok, RS.tok], [QN.tok])
                st["QN"] = QN
                rope_start(st, m, tb)

            def S4(i):
                ci, tb = items[i]
                c0, m, dst, dstt, row0, gcol, norm, rope = chunks[ci]
                st = ist.pop(i)
                O = cst[ci][1]
                if rope:
                    QN, CS, b3 = st["QN"], st["CS"], st["b3"]
                    T1 = t1r.next(); T2 = t2r.next()
                    k.tt("dve", T1.t[0:m, :], QN.t[0:m, :], CS.t[0:m, 0, :], ALU.mult, [QN.tok, CS.tok], [T1.tok])
                    k.tt("dve", T2.t[0:m, :], b3.ap(0, m), CS.t[0:m, 1, :], ALU.mult, [b3.tok, CS.tok], [T2.tok])
                    k.tt("pool", O.t[0:m, tb * 512:(tb + 1) * 512], T1.t[0:m, :], T2.t[0:m, :], ALU.add,
                         [T1.tok, T2.tok], [O.tok])
                if tb == NB - 1:
                    k.dma("sp", dst[row0:row0 + m, :], O.t[0:m, :], [O.tok], [dstt])

            for s_ in range(len(items) + 3):
                for stage, off in ((S4, 3), (S3, 2), (S2, 1), (S1, 0)):
                    i = s_ - off
                    if 0 <= i < len(items):
                        stage(i)

            wv = sbuf("wv", [128, 8, 512], BF16, ph); wvt = Tok()
            wiw = sbuf("wiw", [128, 8, 4], BF16, ph); wiwt = Tok()
            k.dma("pool", wv[:, :, 0:256], wview(w_in[l], C_AV, 256), [], [wvt])
            k.dma("pool", wv[:, :, 256:512], wview(w_in[l], C_BV, 256), [], [wvt])
            k.dma("pool", wiw[:], wview(w_in[l], C_IW, 4), [], [wiwt])
            vb = Ring([Slot(sbuf(f"vb{i}", [128, 512], BF16, ph)) for i in range(2)])
            wib = sbuf("wib", [128, NT, 4], F32, ph); wibt = Tok()
            for tt_ in range(NT):
                b = bk.next()
                for c in range(8):
                    k.mm(b.ap(), hT[:, c, tt_ * 128:(tt_ + 1) * 128], wv[:, c, :], c == 0, c == 7,
                         [hTt, wvt], [b.tok])
                V = vb.next()
                k.act(V.t[:], b.ap(), AF.Copy, [b.tok], [V.tok])
                k.dma("sp", VAB[tt_ * 128:(tt_ + 1) * 128, :], V.t[:], [V.tok], [dtok["VAB"]])
                b = bk.next()
                for c in range(8):
                    k.mm(b.ap(0, 128, 0, 4), hT[:, c, tt_ * 128:(tt_ + 1) * 128], wiw[:, c, :], c == 0, c == 7,
                         [hTt, wiwt], [b.tok])
                k.ts("dve", wib[:, tt_, :], b.ap(0, 128, 0, 4), 0.0625, None, ALU.mult, None, [b.tok], [wibt])
            k.dma("sp", WI.rearrange("(t p) c -> p t c", p=128), wib[:], [wibt], [dtok["WI"]])

            k.act(clam[:], spt[:, SP_LAM:SP_LAM + 4], AF.Exp, [sptok], [clamtok], scale=-1.0)
            k.act(clam[:], clam[:], AF.Ln, [clamtok], [clamtok], bias=1.0)
            k.ts("dve", clam[:], clam[:], -8.0, None, ALU.mult, None, [clamtok], [clamtok])
            cxb = sbuf("cxb", [128, 3 + S], F32, ph)
            cxt = [Tok() for _ in range(NB + 1)]
            k.memset("dve", cxb[:, 0:3], 0.0, [cxt[NB]])
            wbd = Ring([Slot(sbuf(f"wbd{i}", [128, 2, 128], BF16, ph)) for i in range(2)])
            f32r = {n: Ring([Slot(sbuf(f"c{n}{i}", [128, 512], F32, ph)) for i in range(2)])
                    for n in ("gy", "u", "r", "i", "a", "m", "bb", "hs")}
            ubr = Ring([Slot(sbuf(f"ub{i}", [128, 512], BF16, ph)) for i in range(2)])
            for cc in range(4):
                WX = load_w(C_CX + cc * 128, 128)
                WY = load_w(C_CY + cc * 128, 128)
                BD = wbd.next()
                k.dma("pool", BD.t[:], lrubd[l, :, cc].rearrange("m p n -> p m n"), [], [BD.tok])
                O = ob.next()
                prev_hs = None
                for tb in range(NB):
                    b = proj_mm(WX, 128, tb)
                    k.act(cxb[:, 3 + tb * 512:3 + (tb + 1) * 512], b.ap(), AF.Copy, [b.tok], [cxt[tb]])
                    b = proj_mm(WY, 128, tb)
                    GY = f32r["gy"].next()
                    k.act(GY.t[:], b.ap(), AF.Gelu, [b.tok], [GY.tok])
                    U = f32r["u"].next()
                    rd = [cxt[tb], cxt[tb - 1] if tb > 0 else cxt[NB], sptok]
                    cw = lambda j: spt[:, SP_CW + j * 4 + cc:SP_CW + j * 4 + cc + 1]
                    k.ts("dve", U.t[:], cxb[:, tb * 512:tb * 512 + 512], cw(0),
                         spt[:, SP_CB + cc:SP_CB + cc + 1], ALU.mult, ALU.add, rd, [U.tok])
                    for j in range(1, 4):
                        k.stt("dve", U.t[:], cxb[:, tb * 512 + j:tb * 512 + j + 512], cw(j), U.t[:],
                              ALU.mult, ALU.add, rd + [U.tok], [U.tok])
                    UB = ubr.next()
                    k.act(UB.t[:], U.t[:], AF.Copy, [U.tok], [UB.tok])
                    bR = bk.next()
                    k.mm(bR.ap(), BD.t[:, 0, :], UB.t[:], True, True, [BD.tok, UB.tok], [bR.tok])
                    bI = bk.next()
                    k.mm(bI.ap(), BD.t[:, 1, :], UB.t[:], True, True, [BD.tok, UB.tok], [bI.tok])
                    R = f32r["r"].next(); I_ = f32r["i"].next(); A = f32r["a"].next(); M = f32r["m"].next()
                    k.act(R.t[:], bR.ap(), AF.Sigmoid, [bR.tok, sptok], [R.tok], bias=spt[:, SP_BA + cc:SP_BA + cc + 1])
                    k.act(I_.t[:], bI.ap(), AF.Sigmoid, [bI.tok, sptok], [I_.tok], bias=spt[:, SP_BX + cc:SP_BX + cc + 1])
                    k.act(A.t[:], R.t[:], AF.Exp, [R.tok, clamtok], [A.tok], scale=clam[:, cc:cc + 1])
                    k.act(M.t[:], A.t[:], AF.Square, [A.tok], [M.tok])
                    k.act(M.t[:], M.t[:], AF.Sqrt, [M.tok], [M.tok], bias=1.0, scale=-1.0)
                    BB = f32r["bb"].next()
                    k.tt("pool", BB.t[:], I_.t[:], U.t[:], ALU.mult, [I_.tok, U.tok], [BB.tok])
                    k.tt("pool", BB.t[:], BB.t[:], M.t[:], ALU.mult, [BB.tok, M.tok], [BB.tok])
                    HS = f32r["hs"].next()
                    if prev_hs is None:
                        k.op("dve", lambda e, HS=HS, A=A, BB=BB: e.tensor_tensor_scan(
                            out=HS.t[:], data0=A.t[:], data1=BB.t[:], initial=0.0, op0=ALU.mult, op1=ALU.add),
                            [A.tok, BB.tok], [HS.tok])
                    else:
                        k.op("dve", lambda e, HS=HS, A=A, BB=BB, PH=prev_hs: e.tensor_tensor_scan(
                            out=HS.t[:], data0=A.t[:], data1=BB.t[:], initial=PH.t[:, 511:512],
                            op0=ALU.mult, op1=ALU.add), [A.tok, BB.tok, prev_hs.tok], [HS.tok])
                    prev_hs = HS
                    k.tt("pool", O.t[:, tb * 512:(tb + 1) * 512], HS.t[:], GY.t[:], ALU.mult,
                         [HS.tok, GY.tok], [O.tok])
                k.dma("sp", YM[512 + cc * 128:512 + (cc + 1) * 128, :], O.t[:], [O.tok], [YMt[2]])
            k.flush()
            if stop == 'A':
                raise _Stop(nc, k, es)

        def finalize_attn(t, acc, rdr, ytr, yor, row0, ymtok, pst_i):
            RD = rdr.next()
            a3 = acc.t[:, acc.c0:acc.c0 + 260].rearrange("p (h e) -> p h e", e=65)
            k.recip(RD.t[:], a3[:, :, 64:65], [acc.tok], [RD.tok])
            YT = ytr.next()
            for h in range(4):
                k.ts("dve", YT.t[:, h * 64:(h + 1) * 64], acc.ap(0, 128, h * 65, h * 65 + 64), RD.t[:, h, :], None,
                     ALU.mult, None, [acc.tok, RD.tok], [YT.tok])
            half = pst_i[0] % 2
            pst_i[0] += 1
            for c in range(2):
                k.tr(PSTs[half][:, c * 128:(c + 1) * 128], YT.t[:, c * 128:(c + 1) * 128], IDENT,
                     [YT.tok, cmt], [pstoks[half]])
            YO = yor.next()
            k.act(YO.t[:], PSTs[half][:, 0:256], AF.Copy, [pstoks[half]], [YO.tok])
            k.dma("sp", YM[row0:row0 + 256, t * 128:(t + 1) * 128].rearrange("(c p) q -> p c q", p=128),
                  YO.t[:].rearrange("p (c q) -> p c q", c=2), [YO.tok], [ymtok])

        with ExitStack() as ph:
            KAs = sbuf("KAs", [128, 2, S], BF16, ph); kat = Tok()
            QAs = sbuf("QAs", [128, 2, S], BF16, ph); qat = Tok()
            VA4 = sbuf("VA4", [128, NT, 4, 65], BF16, ph); vat = Tok()
            EB = sbuf("EB", [128, 4, 5, 128], F32, ph); ebt = Tok()
            AM = sbuf("AM", [128, 5, 128], F32, ph); amt = Tok()
            k.dma("sp", KAs[:], KA.rearrange("(c p) t -> p c t", p=128), [dtok["KA"]], [kat])
            k.dma("sp", QAs[:], QA.rearrange("(c p) t -> p c t", p=128), [dtok["QA"]], [qat])
            for h in range(4):
                k.dma("sp", VA4[:, :, h, 0:64], VAB.rearrange("(t p) c -> p t c", p=128)[:, :, h * 64:(h + 1) * 64],
                      [dtok["VAB"]], [vat])
            k.memset("pool", VA4[:, :, :, 64:65], 1.0, [vat])
            k.dma("sp", EB[:], abias[l], [], [ebt])
            k.dma("sp", AM[:], amask, [], [amt])
            k.act(EB[:], EB[:], AF.Exp, [ebt], [ebt])
            for h in range(4):
                k.tt("dve", EB[:, h], EB[:, h], AM[:], ALU.mult, [ebt, amt], [ebt])
            Er = Ring([Slot(sbuf(f"Ea{i}", [128, 640], F32, ph)) for i in range(2)])
            Emr = Ring([Slot(sbuf(f"Ema{i}", [128, 640], BF16, ph)) for i in range(3)])
            rdr = Ring([Slot(sbuf(f"rda{i}", [128, 4, 1], F32, ph)) for i in range(2)])
            ytr = Ring([Slot(sbuf(f"yta{i}", [128, 256], BF16, ph)) for i in range(2)])
            yor = Ring([Slot(sbuf(f"yoa{i}", [128, 256], BF16, ph)) for i in range(2)])
            stb = Ring([(PS2[0], banks[0], banks[1]), (PS2[1], banks[2], banks[3])])
            accb = Ring([banks[4], banks[5]])
            pst_i = [0]

            def a_stage(t, h):
                ch, pb = h // 2, (h % 2) * 64
                js = [j for j in range(5) if t - 4 + j >= 0]
                j0 = js[0]
                PT_, ba, bb_ = stb.next()
                for j in js:
                    k.mm(PT_[:, j * 128:(j + 1) * 128], KAs[pb:pb + 64, ch, (t - 4 + j) * 128:(t - 3 + j) * 128],
                         QAs[pb:pb + 64, ch, t * 128:(t + 1) * 128], True, True,
                         [kat, qat], [ba.tok if j < 4 else bb_.tok])
                E = Er.next(); Em = Emr.next()
                k.act(E.t[:, j0 * 128:640], PT_[:, j0 * 128:640], AF.Exp, [ba.tok, bb_.tok], [E.tok], scale=0.125)
                k.tt("dve", Em.t[:, j0 * 128:640], E.t[:, j0 * 128:640],
                     EB[:, h, j0:5, :].rearrange("p j q -> p (j q)"), ALU.mult, [E.tok, ebt], [Em.tok])
                return Em, js

            items = [(t, h) for t in range(NT) for h in range(4)]
            pend = a_stage(*items[0])
            acc = None
            for i, (t, h) in enumerate(items):
                Em, js = pend
                if i + 1 < len(items):
                    pend = a_stage(*items[i + 1])
                if h == 0:
                    acc = accb.next()
                for j in js:
                    k.mm(acc.ap(0, 128, h * 65, h * 65 + 65), Em.t[:, j * 128:(j + 1) * 128], VA4[:, t - 4 + j, h, :],
                         j == js[0], j == 4, [vat, Em.tok], [acc.tok])
                if h == 3:
                    finalize_attn(t, acc, rdr, ytr, yor, 0, YMt[0], pst_i)
            k.flush()
            if stop == 'B1':
                raise _Stop(nc, k, es)

        with ExitStack() as ph:
            KI2 = sbuf("KI2", [128, S], BF16, ph); kit = Tok()
            QIs = sbuf("QIs", [128, 2, S], BF16, ph); qit = Tok()
            KBs = sbuf("KBs", [128, 2, S], BF16, ph); kbt = Tok()
            QBs = sbuf("QBs", [128, 2, S], BF16, ph); qbt = Tok()
            VB4 = sbuf("VB4", [128, NT, 4, 65], BF16, ph); vbt = Tok()
            WIs = sbuf("WIs", [128, NT, 4], F32, ph); wit = Tok()
            P2 = sbuf("P2", [128, NBIS + 1], F32, ph); p2t = Tok()
            k.dma("sp", KI2[0:64, :], KI, [dtok["KI"]], [kit])
            k.dma("sp", KI2[64:128, :], KI, [dtok["KI"]], [kit])
            k.dma("sp", QIs[:], QI.rearrange("(c p) t -> p c t", p=128), [dtok["QI"]], [qit])
            k.dma("sp", KBs[:], KB.rearrange("(c p) t -> p c t", p=128), [dtok["KB"]], [kbt])
            k.dma("sp", QBs[:], QB.rearrange("(c p) t -> p c t", p=128), [dtok["QB"]], [qbt])
            for h in range(4):
                k.dma("sp", VB4[:, :, h, 0:64],
                      VAB.rearrange("(t p) c -> p t c", p=128)[:, :, 256 + h * 64:256 + (h + 1) * 64],
                      [dtok["VAB"]], [vbt])
            k.memset("pool", VB4[:, :, :, 64:65], 1.0, [vbt])
            k.dma("sp", WIs[:], WI.rearrange("(t p) c -> p t c", p=128), [dtok["WI"]], [wit])
            k.dma("sp", P2[:], pow2, [], [p2t])
            scr = Ring([Slot(sbuf(f"sc{i}", [128, S], F32, ph)) for i in range(2)])
            rlr = Ring([Slot(sbuf(f"rl{i}", [128, 512], F32, ph)) for i in range(3)])
            mkr = Ring([Slot(sbuf(f"mk{i}", [128, S], BF16, ph)) for i in range(2)])
            mTall = Ring([Slot(sbuf(f"mTa{i}", [128, S], BF16, ph)) for i in range(2)])
            Er = Ring([Slot(sbuf(f"Eb{i}", [128, 512], BF16, ph)) for i in range(4)])
            Emr = Ring([Slot(sbuf(f"Emb{i}", [128, 512], BF16, ph)) for i in range(4)])
            junk = sbuf("junk", [128, S], BF16, ph); jt = Tok()
            smr = Ring([Slot(sbuf(f"sm{i}", [128, 8 + NBIS + 1], F32, ph)) for i in range(2)])
            rdr = Ring([Slot(sbuf(f"rdb{i}", [128, 4, 1], F32, ph)) for i in range(2)])
            ytr = Ring([Slot(sbuf(f"ytb{i}", [128, 256], BF16, ph)) for i in range(2)])
            yor = Ring([Slot(sbuf(f"yob{i}", [128, 256], BF16, ph)) for i in range(2)])
            dbk = Ring([banks[0], banks[1]])
            sbk = Ring([banks[2], banks[3], banks[4]])
            accb = Ring([banks[5]])
            pst_i = [0]

            def prep(t):
                nk = 128 * (t + 1)
                SC = scr.next()
                nblk = (nk + 511) // 512
                for kb_ in range(nblk):
                    w = min(512, nk - kb_ * 512)
                    cs_ = slice(kb_ * 512, kb_ * 512 + w)
                    for h in range(4):
                        ch, pb = h // 2, (h % 2) * 64
                        b = dbk.next()
                        k.mm(b.ap(0, 128, 0, w), QIs[pb:pb + 64, ch, t * 128:(t + 1) * 128], KI2[pb:pb + 64, cs_],
                             True, True, [qit, kit], [b.tok])
                        wsc = WIs[:, t, h:h + 1]
                        if h == 0:
                            k.ts("dve", SC.t[:, cs_], b.ap(0, 128, 0, w), 0.0, wsc, ALU.max, ALU.mult,
                                 [b.tok, wit], [SC.tok])
                        else:
                            RL = rlr.next()
                            k.act(RL.t[:, 0:w], b.ap(0, 128, 0, w), AF.Relu, [b.tok], [RL.tok])
                            k.act(RL.t[:, 0:w], RL.t[:, 0:w], AF.Copy, [RL.tok, wit], [RL.tok], scale=wsc)
                            k.tt("pool", SC.t[:, cs_], SC.t[:, cs_], RL.t[:, 0:w], ALU.add,
                                 [RL.tok, SC.tok], [SC.tok])
                SM = smr.next()
                mx, mn, rng, mid, cnt, dd, thr = (SM.t[:, i:i + 1] for i in range(7))
                steps = SM.t[:, 8:8 + NBIS + 1]
                bis = t >= 2 and not OPT.get('b2_nobis')
                if bis:
                    k.op("dve", lambda e: e.tensor_reduce(out=mx, in_=SC.t[:, 0:nk], axis=AX.X, op=ALU.max),
                         [SC.tok], [SM.tok])
                k.op("dve", lambda e: e.tensor_reduce(out=mn, in_=SC.t[:, 0:nk], axis=AX.X, op=ALU.min),
                     [SC.tok], [SM.tok])
                k.memset("dve", SC.t[0:64, nk - 64:nk], -1.0e30, [SC.tok])
                if bis:
                    k.tt("dve", rng, mx, mn, ALU.subtract, [SM.tok], [SM.tok])
                    k.ts("dve", steps, P2[:], rng, None, ALU.mult, None, [p2t, SM.tok], [SM.tok])
                    k.tt("dve", mid, mn, steps[:, 0:1], ALU.add, [SM.tok], [SM.tok])
                    for it in range(NBIS):
                        k.ts("dve", junk[:, 0:nk], SC.t[:, 0:nk], mid, 0.0, ALU.is_ge, ALU.add,
                             [SC.tok, SM.tok], [jt, SM.tok], accum_out=cnt)
                        k.ts("dve", dd, cnt, 255.5, 0.5, ALU.is_ge, ALU.subtract, [SM.tok], [SM.tok])
                        k.stt("dve", mid, dd, steps[:, it:it + 1], mid, ALU.mult, ALU.add, [SM.tok], [SM.tok])
                    k.tt("dve", thr, mid, steps[:, NBIS:NBIS + 1], ALU.subtract, [SM.tok], [SM.tok])
                else:
                    k.copy("dve", thr, mn, [SM.tok], [SM.tok])
                MK = mkr.next()
                k.ts("dve", MK.t[:, 0:nk], SC.t[:, 0:nk], thr, None, ALU.is_ge, None, [SC.tok, SM.tok], [MK.tok])
                return MK

            def prep_b(t, MK):
                ngrp = (t + 1 + 3) // 4
                MT = mTall.next()
                for g in range(ngrp):
                    jts = list(range(g * 4, min(t + 1, g * 4 + 4)))
                    half = pst_i[0] % 2
                    pst_i[0] += 1
                    for jj, jt_ in enumerate(jts):
                        k.tr(PSTs[half][:, jj * 128:(jj + 1) * 128],
                             MK.t[:, jt_ * 128:(jt_ + 1) * 128], IDENT, [MK.tok, cmt], [pstoks[half]])
                    n = len(jts) * 128
                    k.act(MT.t[:, g * 512:g * 512 + n], PSTs[half][:, 0:n], AF.Copy,
                          [pstoks[half]], [MT.tok])
                return MT

            def b_stage(t, h, g, MT):
                ch, pb = h // 2, (h % 2) * 64
                jts = list(range(g * 4, min(t + 1, g * 4 + 4)))
                n = len(jts) * 128
                b = sbk.next()
                for jj, jt_ in enumerate(jts):
                    k.mm(b.ap(0, 128, jj * 128, (jj + 1) * 128), KBs[pb:pb + 64, ch, jt_ * 128:(jt_ + 1) * 128],
                         QBs[pb:pb + 64, ch, t * 128:(t + 1) * 128], True, True, [kbt, qbt], [b.tok])
                E = Er.next(); Em = Emr.next()
                k.act(E.t[:, 0:n], b.ap(0, 128, 0, n), AF.Exp, [b.tok], [E.tok], scale=0.125)
                k.tt("pool", Em.t[:, 0:n], E.t[:, 0:n], MT.t[:, g * 512:g * 512 + n], ALU.mult,
                     [E.tok, MT.tok], [Em.tok])
                return Em, jts

            ntile = OPT.get('b2_tmax', NT)
            MTs = {0: prep_b(0, prep(0))}
            for t in range(ntile):
                MKn = prep(t + 1) if t + 1 < ntile else None
                if OPT.get('b2_noattn'):
                    continue
                MT = MTs.pop(t)
                ngrp = (t + 1 + 3) // 4
                items = [(h, g) for h in range(4) for g in range(ngrp)]
                pend = [b_stage(t, *it_, MT) for it_ in items[0:2]]
                acc = accb.next()
                for i, (h, g) in enumerate(items):
                    if i + 2 < len(items):
                        pend.append(b_stage(t, *items[i + 2], MT))
                    Em, jts = pend.pop(0)
                    for jj, jt_ in enumerate(jts):
                        k.mm(acc.ap(0, 128, h * 65, h * 65 + 65), Em.t[:, jj * 128:(jj + 1) * 128], VB4[:, jt_, h, :],
                             jt_ == 0, jt_ == t, [vbt, Em.tok], [acc.tok])
                if MKn is not None:
                    MTs[t + 1] = prep_b(t + 1, MKn)
                finalize_attn(t, acc, rdr, ytr, yor, 256, YMt[1], pst_i)
            k.flush()
            if stop == 'B2':
                raise _Stop(nc, k, es)

        with ExitStack() as ph:
            wg = sbuf("wg", [128, 8, 3072], BF16, ph); wgt = Tok()
            wb = sbuf("wb", [128, 8, 1024], BF16, ph); wbt = Tok()
            wo = sbuf("wo", [128, 8, 1024], BF16, ph); wot = Tok()
            for c in range(8):
                k.dma("pool", wg[:, c, :], w_in[l, c * 128:(c + 1) * 128, C_GT:C_GT + 3072], [], [wgt])
            k.dma("pool", wb[:], wview(w_br[l], 0, 1024), [], [wbt])
            k.dma("pool", wo[:], wview(w_out[l], 0, 1024), [], [wot])
            X = Slot(sbuf("xc", [128, 8, 512], F32, ph))
            sq = sbuf("sqc", [128, 8, 512], BF16, ph); sqt = Tok()
            sd = sbuf("sdc", [128, 512], F32, ph); sdt = Tok()
            rstd = sbuf("rstdc", [128, 512], F32, ph); rst = Tok()
            hTb = sbuf("hTb", [128, 8, 512], BF16, ph); hbt = Tok()
            ym = sbuf("ymc", [128, 8, 512], BF16, ph); ymt = Tok()
            mg = sbuf("mg", [128, 8, 512], BF16, ph); mgt = [Tok() for _ in range(8)]
            sgr = Ring([Slot(sbuf(f"sg{i}", [128, 512], F32, ph)) for i in range(3)])
            tmr = Ring([Slot(sbuf(f"tm{i}", [128, 512], F32, ph)) for i in range(3)])
            acr = Ring([Slot(sbuf(f"ac{i}", [128, 512], F32, ph)) for i in range(2)])
            xo = Ring([Slot(sbuf(f"xo{i}", [128, 512], F32, ph)) for i in range(3)])
            bk = Ring(banks)
            KR = [(0, 2), (2, 4), (4, 8)]
            for tb in range(NB):
                k.dma("sp", X.t[:], xview(xsrc, tb), [xsrct], [X.tok])
                k.dma("sp", ym[:], xview(YM, tb), YMt, [ymt])
                make_hT(X.t, X.tok, SP_GMIX, sq, sqt, sd, sdt, rstd, rst, lambda c: hTb[:, c, :], hbt, bk.next())
                for dc in range(8):
                    AC = acr.next()
                    for br in range(3):
                        bg = bk.next()
                        for c in range(8):
                            k.mm(bg.ap(), wg[:, c, br * 1024 + dc * 128:br * 1024 + (dc + 1) * 128], hTb[:, c, :],
                                 c == 0, c == 7, [wgt, hbt], [bg.tok])
                        SG = sgr.next()
                        bcol = SP_BG + br * 8 + dc
                        k.act(SG.t[:], bg.ap(), AF.Sigmoid, [bg.tok, sptok], [SG.tok], bias=spt[:, bcol:bcol + 1])
                        bb_ = bk.next()
                        k0, k1 = KR[br]
                        for c in range(k0, k1):
                            k.mm(bb_.ap(), wb[:, c, dc * 128:(dc + 1) * 128], ym[:, c, :], c == k0, c == k1 - 1,
                                 [wbt, ymt], [bb_.tok])
                        if br == 0:
                            k.tt("dve", AC.t[:], SG.t[:], bb_.ap(), ALU.mult, [SG.tok, bb_.tok], [AC.tok])
                        else:
                            TM = tmr.next()
                            k.tt("dve", TM.t[:], SG.t[:], bb_.ap(), ALU.mult, [SG.tok, bb_.tok], [TM.tok])
                            if br == 1:
                                k.tt("pool", AC.t[:], AC.t[:], TM.t[:], ALU.add, [AC.tok, TM.tok], [AC.tok])
                            else:
                                k.tt("pool", mg[:, dc, :], AC.t[:], TM.t[:], ALU.add, [AC.tok, TM.tok], [mgt[dc]])
                for oc in range(8):
                    bo = bk.next()
                    for c in range(8):
                        k.mm(bo.ap(), wo[:, c, oc * 128:(oc + 1) * 128], mg[:, c, :], c == 0, c == 7,
                             [wot, mgt[c]], [bo.tok])
                    XO = xo.next()
                    k.tt("dve", XO.t[:], X.t[:, oc, :], bo.ap(), ALU.add, [X.tok, bo.tok], [XO.tok])
                    k.dma("sp", XT[oc * 128:(oc + 1) * 128, tb * 512:(tb + 1) * 512], XO.t[:], [XO.tok], [dtok["XT"]])
            k.flush()
            if stop == 'C':
                raise _Stop(nc, k, es)

        with ExitStack() as ph:
            w1 = sbuf("w1", [128, 8, 2 * DFF], BF16, ph); w1t = Tok()
            w2 = sbuf("w2", [128, 22, 1024], BF16, ph); w2t = Tok()
            for c in range(8):
                k.dma("pool", w1[:, c, :], w_f1[l, c * 128:(c + 1) * 128, :], [], [w1t])
            for c in range(22):
                k.dma("pool", w2[:, c, :], w_f2[l, c * 128:(c + 1) * 128, :], [], [w2t])
            X = Slot(sbuf("xd", [128, 8, 512], F32, ph))
            sq = sbuf("sqd", [128, 8, 512], BF16, ph); sqt = Tok()
            sd = sbuf("sdd", [128, 512], F32, ph); sdt = Tok()
            rstd = sbuf("rstdd", [128, 512], F32, ph); rst = Tok()
            hTb = sbuf("hTd", [128, 8, 512], BF16, ph); hbt = Tok()
            av = sbuf("av", [128, 22, 512], BF16, ph); avt = [Tok() for _ in range(22)]
            sgr = Ring([Slot(sbuf(f"sl{i}", [128, 512], F32, ph)) for i in range(3)])
            xo = Ring([Slot(sbuf(f"xod{i}", [128, 512], F32, ph)) for i in range(3)])
            bk = Ring(banks)
            xdst, xdstt = (yT, dtok["yT"]) if last else (XT, dtok["XT"])
            for tb in range(NB):
                k.dma("sp", X.t[:], xview(XT, tb), [dtok["XT"]], [X.tok])
                make_hT(X.t, X.tok, SP_GFFN, sq, sqt, sd, sdt, rstd, rst, lambda c: hTb[:, c, :], hbt, bk.next())
                for fc in range(22):
                    bg = bk.next()
                    for c in range(8):
                        k.mm(bg.ap(), w1[:, c, fc * 128:(fc + 1) * 128], hTb[:, c, :], c == 0, c == 7,
                             [w1t, hbt], [bg.tok])
                    bu = bk.next()
                    for c in range(8):
                        k.mm(bu.ap(), w1[:, c, DFF + fc * 128:DFF + (fc + 1) * 128], hTb[:, c, :], c == 0, c == 7,
                             [w1t, hbt], [bu.tok])
                    SG = sgr.next()
                    k.act(SG.t[:], bg.ap(), AF.Silu, [bg.tok], [SG.tok])
                    k.tt("dve", av[:, fc, :], SG.t[:], bu.ap(), ALU.mult, [SG.tok, bu.tok], [avt[fc]])
                for oc in range(8):
                    bo = bk.next()
                    for c in range(22):
                        k.mm(bo.ap(), w2[:, c, oc * 128:(oc + 1) * 128], av[:, c, :], c == 0, c == 21,
                             [w2t, avt[c]], [bo.tok])
                    XO = xo.next()
                    k.tt("dve", XO.t[:], X.t[:, oc, :], bo.ap(), ALU.add, [X.tok, bo.tok], [XO.tok])
                    k.dma("sp", xdst[oc * 128:(oc + 1) * 128, tb * 512:(tb + 1) * 512], XO.t[:], [XO.tok], [xdstt])
            if last:
                k.wait_all_dma()
            k.flush()
            if stop == 'D':
                raise _Stop(nc, k, es)
    es.close()
    return nc, k


def host_consts():
    pos = np.arange(S, dtype=np.float32)
    inv = (1.0 / (np.float32(10000.0) ** (np.arange(0, 64, 2, dtype=np.float32) / np.float32(64)))).astype(np.float32)
    ang = pos[:, None] * inv[None, :]
    ang = np.concatenate([ang, ang], axis=-1)
    cosT = np.cos(ang).astype(np.float32).T
    sinT = np.sin(ang).astype(np.float32).T
    cs = np.zeros((128, 2, S), np.float32)
    cs[0:64, 0], cs[64:128, 0] = cosT, cosT
    cs[0:64, 1], cs[64:128, 1] = sinT, sinT
    ones = np.ones((128, 128), np.float32)
    onesblk = np.zeros((128, 128), np.float32)
    onesblk[0:64, 0:64] = 1.0
    onesblk[64:128, 64:128] = 1.0
    rotm = np.zeros((128, 128), np.float32)
    for hb in (0, 64):
        for m in range(32):
            rotm[hb + m + 32, hb + m] = -1.0
            rotm[hb + m, hb + m + 32] = 1.0
    ident = np.eye(128, dtype=np.float32)
    cmat = np.stack([ones, onesblk, rotm, ident]).astype(np.float32)
    kk = np.arange(128)[:, None, None]
    jj = np.arange(5)[None, :, None]
    qq = np.arange(128)[None, None, :]
    cq = (qq >= 64).astype(np.int64)
    ck = 2 * jj - 8 + (kk >= 64)
    amask = ((ck >= cq - 8) & (ck <= cq)).astype(np.float32)
    relidx = np.clip(128 * (4 - jj) + qq - kk, -128, 128) + 128
    pow2 = np.tile((2.0 ** -(np.arange(NBIS + 1) + 1.0)).astype(np.float32)[None, :], (128, 1))
    return cs, cmat, amask, relidx, pow2


def host_pack(inp):
    cs, cmat, amask, relidx, pow2 = host_consts()
    f = lambda a: np.ascontiguousarray(np.asarray(a, dtype=np.float32))
    spar = np.zeros((L, 128, NSP), np.float32)
    p = np.arange(128)
    for l in range(L):
        spar[l, :, SP_GMIX:SP_GMIX + 8] = f(inp["g_mix"])[l].reshape(8, 128).T
        spar[l, :, SP_GFFN:SP_GFFN + 8] = f(inp["g_ffn"])[l].reshape(8, 128).T
        spar[l, :, SP_BG:SP_BG + 24] = f(inp["b_gate"])[l].reshape(24, 128).T
        spar[l, :, SP_GAQ] = f(inp["qk_gain_a"])[l, 0][p % 64]
        spar[l, :, SP_GAK] = f(inp["qk_gain_a"])[l, 1][p % 64]
        spar[l, :, SP_GBQ] = f(inp["qk_gain_b"])[l, 0][p % 64]
        spar[l, :, SP_GBK] = f(inp["qk_gain_b"])[l, 1][p % 64]
        spar[l, :, SP_GIK] = f(inp["g_idx_k"])[l][p % 64]
        cw = f(inp["conv_w"])[l]
        for j in range(4):
            spar[l, :, SP_CW + j * 4:SP_CW + j * 4 + 4] = cw[j].reshape(4, 128).T
        spar[l, :, SP_CB:SP_CB + 4] = f(inp["conv_b"])[l].reshape(4, 128).T
        spar[l, :, SP_BA:SP_BA + 4] = f(inp["lru_ba"])[l].reshape(4, 128).T
        spar[l, :, SP_BX:SP_BX + 4] = f(inp["lru_bx"])[l].reshape(4, 128).T
        spar[l, :, SP_LAM:SP_LAM + 4] = f(inp["lru_lambda"])[l].reshape(4, 128).T
    lrubd = np.zeros((L, 2, 4, 128, 128), np.float32)
    for m, nm in enumerate(("lru_wa", "lru_wx")):
        wsrc = f(inp[nm])
        for cc in range(4):
            lrubd[:, m, cc, 0:64, 0:64] = wsrc[:, 2 * cc]
            lrubd[:, m, cc, 64:128, 64:128] = wsrc[:, 2 * cc + 1]
    rb = f(inp["rel_bias"])
    ab = rb[:, :, relidx]
    abias = np.ascontiguousarray(ab.transpose(0, 2, 1, 3, 4))
    shared = {"w_in": f(inp["w_in"]), "w_branch": f(inp["w_branch"]), "w_out": f(inp["w_out"]),
              "w_ffn_in": f(inp["w_ffn_in"]), "w_ffn_out": f(inp["w_ffn_out"]),
              "spar": spar, "lrubd": lrubd, "abias": abias, "cs": cs, "cmat": cmat,
              "amask": amask, "pow2": pow2}
    return shared


_CACHE = {}


def kernel(**inputs):
    x = np.asarray(inputs["x"], dtype=np.float32)
    shared = host_pack(inputs)
    if "nc" not in _CACHE:
        _CACHE["nc"] = build()[0]
    nc = _CACHE["nc"]
    in_maps = []
    for b in range(8):
        m = dict(shared)
        m["xT"] = np.ascontiguousarray(x[b].T)
        in_maps.append(m)
    res = run_bass_kernel_spmd(nc, in_maps, core_ids=list(range(8)))
    out = np.stack([np.ascontiguousarray(r["yT"].T) for r in res.results], axis=0)
    return out.astype(np.float32)
```
